# Optimizing a Trainium2 kernel written in Bass

```python
import math
import jax, jax.numpy as jnp
from jax import lax
import numpy as np

D_MODEL = 1024
BATCH = 4
SEQ = 8192
DEPTH = 2

GRID_W = 64
CTX_LEN = 256
MIX_W = D_MODEL
FOURIER_GROUPS = 4
FOURIER_W = MIX_W // 4
POOL_WINDOWS = (2, 4, 8, 16)
POOL_W = MIX_W // 4
POOL_GROUP = POOL_W // len(POOL_WINDOWS)
HEAD_DIM = 64
ATT_HEADS = (MIX_W // 4) // HEAD_DIM
ATT_KV_HEADS = 2
WINDOW = 128
ATT_BLOCK = 128
ROPE_BASE = 10000.0
RET_HEADS = 4
RET_DK = (MIX_W // 4) // RET_HEADS
RET_DV = RET_DK
RET_CHUNK = 128
N_EXPERTS = 32
TOP_K = 4
D_EXPERT = D_MODEL
SWIGLU_LIMIT = 7.0
SWIGLU_ALPHA = 1.702
MOE_BLOCK = 128
DEEPNORM_ALPHA = (2 * DEPTH) ** 0.25
DEEPNORM_BETA = (8 * DEPTH) ** -0.25
LN_EPS = 1e-6
NEG_INF = -1e30
IN_SIZES = (FOURIER_W, POOL_W, ATT_HEADS * HEAD_DIM, ATT_KV_HEADS * HEAD_DIM, ATT_KV_HEADS * HEAD_DIM,
            RET_HEADS * RET_DK, RET_HEADS * RET_DK, RET_HEADS * RET_DV, RET_HEADS * RET_DV)
IN_W = sum(IN_SIZES)

kernel_name = 'hybrid_parallel_groups_moe_dit'


def layer_norm(x, g=None, b=None):
    xf = x.astype(jnp.float32)
    mu = jnp.mean(xf, axis=-1, keepdims=True)
    var = jnp.mean(jnp.square(xf - mu), axis=-1, keepdims=True)
    y = (xf - mu) * lax.rsqrt(var + LN_EPS)
    if g is not None:
        y = y * g.astype(jnp.float32) + b.astype(jnp.float32)
    return y.astype(x.dtype)


def axial_rope(x, row, col):
    half = x.shape[-1] // 2
    inv = 1.0 / (ROPE_BASE ** (jnp.arange(0, half, 2, dtype=jnp.float32) / half))

    def rot(xp, pos):
        ang = pos.astype(jnp.float32)[:, None] * inv[None, :]
        cos = jnp.cos(ang)[None, :, None, :].astype(xp.dtype)
        sin = jnp.sin(ang)[None, :, None, :].astype(xp.dtype)
        x1, x2 = jnp.split(xp, 2, axis=-1)
        return jnp.concatenate([x1 * cos - x2 * sin, x2 * cos + x1 * sin], axis=-1)

    return jnp.concatenate([rot(x[..., :half], row), rot(x[..., half:], col)], axis=-1)


def fourier_mix(a):
    B, N, _ = a.shape
    af = a.reshape(B, N, FOURIER_GROUPS, FOURIER_W // FOURIER_GROUPS).astype(jnp.float32)
    y = jnp.fft.fftn(af, axes=(1, 3), norm='ortho').real
    return y.astype(a.dtype).reshape(B, N, FOURIER_W)


def centred_window_mean(x, w):
    N = x.shape[1]
    cs = jnp.concatenate([jnp.zeros_like(x[:, :1]), jnp.cumsum(x, axis=1)], axis=1)
    t = jnp.arange(N)
    lo = jnp.clip(t - w // 2, 0, N)
    hi = jnp.clip(t + w // 2, 0, N)
    cnt = (hi - lo).astype(x.dtype)
    return (cs[:, hi] - cs[:, lo]) / cnt[None, :, None]


def pool_mix(b, pool_w, pool_scale):
    B, N, _ = b.shape
    bg = b.reshape(B, N, len(POOL_WINDOWS), POOL_GROUP).astype(jnp.float32)
    pooled = jnp.stack([centred_window_mean(bg[:, :, i], w) - bg[:, :, i] for i, w in enumerate(POOL_WINDOWS)], axis=2)
    y = jnp.einsum('bngc,gcd->bngd', pooled.astype(b.dtype), pool_w)
    return y.reshape(B, N, POOL_W) * pool_scale


def banded_attention(q, k, v, k_ctx, v_ctx, sinks):
    B, N, _, _ = q.shape
    L = k_ctx.shape[1]
    nb = N // ATT_BLOCK
    G = ATT_HEADS // ATT_KV_HEADS
    scale = HEAD_DIM ** -0.5
    qb = q.reshape(B, nb, ATT_BLOCK, ATT_KV_HEADS, G, HEAD_DIM)

    def band(t):
        tp = jnp.pad(t, ((0, 0), (ATT_BLOCK, ATT_BLOCK), (0, 0), (0, 0)))
        tp = tp.reshape(B, nb + 2, ATT_BLOCK, ATT_KV_HEADS, HEAD_DIM)
        return jnp.concatenate([tp[:, :-2], tp[:, 1:-1], tp[:, 2:]], axis=2)

    kb, vb = band(k), band(v)
    s_loc = jnp.einsum('bnqkgd,bnskd->bnkgqs', qb, kb).astype(jnp.float32) * scale
    s_ctx = jnp.einsum('bnqkgd,blkd->bnkgql', qb, k_ctx).astype(jnp.float32) * scale
    blk = jnp.arange(nb)[:, None]
    qpos = blk * ATT_BLOCK + jnp.arange(ATT_BLOCK)[None]
    kpos = (blk - 1) * ATT_BLOCK + jnp.arange(3 * ATT_BLOCK)[None]
    valid = ((jnp.abs(qpos[:, :, None] - kpos[:, None, :]) <= WINDOW)
             & (kpos >= 0)[:, None, :] & (kpos < N)[:, None, :])
    s_loc = jnp.where(valid[None, :, None, None], s_loc, NEG_INF)
    sink = jnp.broadcast_to(sinks.astype(jnp.float32).reshape(ATT_KV_HEADS, G)[None, None, :, :, None, None],
                            s_loc.shape[:-1] + (1,))
    p = jax.nn.softmax(jnp.concatenate([s_loc, s_ctx, sink], axis=-1), axis=-1).astype(v.dtype)
    nloc = 3 * ATT_BLOCK
    o = (jnp.einsum('bnkgqs,bnskd->bnqkgd', p[..., :nloc], vb)
         + jnp.einsum('bnkgql,blkd->bnqkgd', p[..., nloc:nloc + L], v_ctx))
    return o.reshape(B, N, ATT_HEADS * HEAD_DIM)


def context_attention(q, k, v, sinks):
    B, L, _, _ = q.shape
    G = ATT_HEADS // ATT_KV_HEADS
    qg = q.reshape(B, L, ATT_KV_HEADS, G, HEAD_DIM)
    s = jnp.einsum('blkgd,bmkd->bkglm', qg, k).astype(jnp.float32) * (HEAD_DIM ** -0.5)
    sink = jnp.broadcast_to(sinks.astype(jnp.float32).reshape(ATT_KV_HEADS, G)[None, :, :, None, None],
                            s.shape[:-1] + (1,))
    p = jax.nn.softmax(jnp.concatenate([s, sink], axis=-1), axis=-1).astype(v.dtype)
    o = jnp.einsum('bkglm,bmkd->blkgd', p[..., :L], v)
    return o.reshape(B, L, ATT_HEADS * HEAD_DIM)


def retention_chunkwise(q, k, v, log_g, state0):
    B, N, H, dk = q.shape
    dv = v.shape[-1]
    nc = N // RET_CHUNK
    qc = q.reshape(B, nc, RET_CHUNK, H, dk)
    kc = k.reshape(B, nc, RET_CHUNK, H, dk)
    vc = v.reshape(B, nc, RET_CHUNK, H, dv)
    i = jnp.arange(RET_CHUNK, dtype=jnp.float32)
    diff = i[:, None] - i[None, :]
    dmask = jnp.where(diff >= 0, jnp.exp(jnp.maximum(diff, 0.0)[None] * log_g[:, None, None]), 0.0)
    scores = jnp.einsum('bnqhd,bnshd->bnhqs', qc, kc) * dmask[None, None]
    o_inner = jnp.einsum('bnhqs,bnshv->bnqhv', scores, vc)
    zeta = jnp.exp((RET_CHUNK - 1.0 - i)[:, None] * log_g[None, :])
    kv = jnp.einsum('bnshd,sh,bnshv->bnhdv', kc, zeta, vc)
    chunk_decay = jnp.exp(RET_CHUNK * log_g)[None, :, None, None]

    def step(R, kv_n):
        return chunk_decay * R + kv_n, R

    R_final, R_prev = lax.scan(step, state0, jnp.moveaxis(kv, 1, 0))
    R_prev = jnp.moveaxis(R_prev, 0, 1)
    xi = jnp.exp((i + 1.0)[:, None] * log_g[None, :])
    o_cross = jnp.einsum('bnqhd,bnhdv->bnqhv', qc, R_prev) * xi[None, None, :, :, None]
    return (o_inner + o_cross).reshape(B, N, H, dv), R_final


def bidirectional_retention(q_l, k_l, v_l, q_c, k_c, v_c, ret_decay):
    def heads(t):
        return t.reshape(t.shape[0], t.shape[1], RET_HEADS, -1).astype(jnp.float32)

    q_l, k_l, v_l, q_c, k_c, v_c = [heads(t) for t in (q_l, k_l, v_l, q_c, k_c, v_c)]
    k_l = k_l * (RET_DK ** -0.5)
    k_c = k_c * (RET_DK ** -0.5)
    log_g = -jnp.exp(ret_decay.astype(jnp.float32))
    B = q_l.shape[0]
    state0 = jnp.zeros((B, RET_HEADS, RET_DK, RET_DV), jnp.float32)

    def flip(t):
        return jnp.flip(t, axis=1)

    o_cf, s_f = retention_chunkwise(q_c, k_c, v_c, log_g[0], state0)
    o_lf, _ = retention_chunkwise(q_l, k_l, v_l, log_g[0], s_f)
    o_cb, s_b = retention_chunkwise(flip(q_c), flip(k_c), flip(v_c), log_g[1], state0)
    o_lb, _ = retention_chunkwise(flip(q_l), flip(k_l), flip(v_l), log_g[1], s_b)
    return o_lf + flip(o_lb), o_cf + flip(o_cb)


def retention_out(o, g):
    B, N = o.shape[:2]
    gn = layer_norm(o).reshape(B, N, RET_HEADS * RET_DV).astype(g.dtype)
    return jax.nn.silu(g) * gn


def token_mixers(h_lat, h_ctx, w_in, pool_w, pool_scale, sinks, ret_decay, w_out, row, col, need_ctx):
    B, N, _ = h_lat.shape
    L = h_ctx.shape[1]
    split_pts = np.cumsum(IN_SIZES)[:-1].tolist()
    a_l, b_l, q_l, k_l, v_l, rq_l, rk_l, rv_l, rg_l = jnp.split(h_lat @ w_in, split_pts, axis=-1)
    a_c, b_c, q_c, k_c, v_c, rq_c, rk_c, rv_c, rg_c = jnp.split(h_ctx @ w_in, split_pts, axis=-1)
    q_l = axial_rope(q_l.reshape(B, N, ATT_HEADS, HEAD_DIM), row, col)
    k_l = axial_rope(k_l.reshape(B, N, ATT_KV_HEADS, HEAD_DIM), row, col)
    v_l = v_l.reshape(B, N, ATT_KV_HEADS, HEAD_DIM)
    k_c = k_c.reshape(B, L, ATT_KV_HEADS, HEAD_DIM)
    v_c = v_c.reshape(B, L, ATT_KV_HEADS, HEAD_DIM)
    att_l = banded_attention(q_l, k_l, v_l, k_c, v_c, sinks)
    ret_l, ret_c = bidirectional_retention(rq_l, rk_l, rv_l, rq_c, rk_c, rv_c, ret_decay)
    y_lat = jnp.concatenate([fourier_mix(a_l), pool_mix(b_l, pool_w, pool_scale), att_l,
                             retention_out(ret_l, rg_l)], axis=-1) @ w_out
    if not need_ctx:
        return y_lat, None
    att_c = context_attention(q_c.reshape(B, L, ATT_HEADS, HEAD_DIM), k_c, v_c, sinks)
    y_ctx = jnp.concatenate([fourier_mix(a_c), pool_mix(b_c, pool_w, pool_scale), att_c,
                             retention_out(ret_c, rg_c)], axis=-1) @ w_out
    return y_lat, y_ctx


def clamped_swiglu(u):
    glu = jnp.minimum(u[..., ::2], SWIGLU_LIMIT)
    lin = jnp.clip(u[..., 1::2], -SWIGLU_LIMIT, SWIGLU_LIMIT)
    return glu * jax.nn.sigmoid(SWIGLU_ALPHA * glu) * (lin + 1.0)


def moe_ffn(h, router_w, router_b, w1, b1, w2, b2):
    T, D = h.shape
    logits = (h @ router_w + router_b).astype(jnp.float32)
    top_v, top_i = lax.top_k(logits, TOP_K)
    gates = jax.nn.softmax(top_v, axis=-1)
    A = T * TOP_K
    flat_e = top_i.reshape(-1)
    flat_g = gates.reshape(-1).astype(h.dtype)
    order = jnp.argsort(flat_e)
    sorted_e = flat_e[order]
    counts = jnp.bincount(flat_e, length=N_EXPERTS)
    padded = (counts + MOE_BLOCK - 1) // MOE_BLOCK * MOE_BLOCK
    pad_end = jnp.cumsum(padded)
    pad_start = pad_end - padded
    grp_start = jnp.cumsum(counts) - counts
    dest = pad_start[sorted_e] + jnp.arange(A) - grp_start[sorted_e]
    nblk = -(-A // MOE_BLOCK) + N_EXPERTS
    P = nblk * MOE_BLOCK
    row_tok = jnp.full((P,), T, jnp.int32).at[dest].set((order // TOP_K).astype(jnp.int32))
    row_gate = jnp.zeros((P,), h.dtype).at[dest].set(flat_g[order])
    blk_e = jnp.minimum(jnp.searchsorted(pad_end, jnp.arange(nblk) * MOE_BLOCK, side='right'), N_EXPERTS - 1)
    h_pad = jnp.concatenate([h, jnp.zeros((1, D), h.dtype)], axis=0)

    def expert_block(args):
        rows, e = args
        u = h_pad[rows] @ w1[e] + b1[e]
        return clamped_swiglu(u) @ w2[e] + b2[e]

    y = lax.map(expert_block, (row_tok.reshape(nblk, MOE_BLOCK), blk_e))
    y = y.reshape(P, D) * row_gate[:, None]
    return jnp.zeros((T + 1, D), y.dtype).at[row_tok].add(y)[:T]


def setup_inputs(seed: int = 0) -> dict:
    key = jax.random.key(seed)
    ks = jax.random.split(key, 20)
    nrm = jax.random.normal
    f32 = jnp.float32
    base_decay = -(5.0 + jnp.arange(RET_HEADS, dtype=f32)) * math.log(2.0)
    return {
        'x': nrm(ks[0], (BATCH, SEQ, D_MODEL), f32),
        'c': nrm(ks[1], (BATCH, D_MODEL), f32),
        'ctx': nrm(ks[2], (BATCH, CTX_LEN, D_MODEL), f32),
        'c_ctx': nrm(ks[3], (D_MODEL,), f32),
        'w_mod': nrm(ks[4], (DEPTH, D_MODEL, 6 * D_MODEL), f32) * D_MODEL ** -0.5,
        'b_mod': 0.02 * nrm(ks[5], (DEPTH, 6 * D_MODEL), f32),
        'w_in': nrm(ks[6], (DEPTH, D_MODEL, IN_W), f32) * D_MODEL ** -0.5,
        'pool_w': nrm(ks[7], (DEPTH, len(POOL_WINDOWS), POOL_GROUP, POOL_GROUP), f32) * POOL_GROUP ** -0.5,
        'pool_scale': 1.0 + 0.1 * nrm(ks[8], (DEPTH, POOL_W), f32),
        'attn_sink': 0.5 * nrm(ks[9], (DEPTH, ATT_HEADS), f32),
        'ret_decay': jnp.log(-base_decay)[None, None, :] * 0.0 + jnp.log(jnp.exp(base_decay))[None, None, :] + 0.1 * nrm(ks[10], (DEPTH, 2, RET_HEADS), f32),
        'w_out': nrm(ks[11], (DEPTH, MIX_W, D_MODEL), f32) * MIX_W ** -0.5 * DEEPNORM_BETA,
        'ln_g': 1.0 + 0.02 * nrm(ks[12], (DEPTH, 2, D_MODEL), f32),
        'ln_b': 0.02 * nrm(ks[13], (DEPTH, 2, D_MODEL), f32),
        'router_w': nrm(ks[14], (DEPTH, D_MODEL, N_EXPERTS), f32) * D_MODEL ** -0.5,
        'router_b': 0.01 * nrm(ks[15], (DEPTH, N_EXPERTS), f32),
        'exp_w1': nrm(ks[16], (DEPTH, N_EXPERTS, D_MODEL, 2 * D_EXPERT), f32) * D_MODEL ** -0.5,
        'exp_b1': 0.02 * nrm(ks[17], (DEPTH, N_EXPERTS, 2 * D_EXPERT), f32),
        'exp_w2': nrm(ks[18], (DEPTH, N_EXPERTS, D_EXPERT, D_MODEL), f32) * D_EXPERT ** -0.5 * DEEPNORM_BETA,
        'exp_b2': 0.02 * nrm(ks[19], (DEPTH, N_EXPERTS, D_MODEL), f32),
    }


def reference(x, c, ctx, c_ctx, w_mod, b_mod, w_in, pool_w, pool_scale, attn_sink, ret_decay, w_out,
              ln_g, ln_b, router_w, router_b, exp_w1, exp_b1, exp_w2, exp_b2):
    B, N, D = x.shape
    L = ctx.shape[1]
    ROWS = N // GRID_W
    row = jnp.broadcast_to(jnp.arange(ROWS)[:, None], (ROWS, GRID_W)).reshape(-1)
    col = jnp.broadcast_to(jnp.arange(GRID_W)[None, :], (ROWS, GRID_W)).reshape(-1)
    x_lat, x_ctx = x, ctx
    for l in range(DEPTH):
        need_ctx = l < DEPTH - 1
        sh1, sc1, g1, sh2, sc2, g2 = [m[:, None, :] for m in
                                      jnp.split(jax.nn.silu(c) @ w_mod[l] + b_mod[l], 6, axis=-1)]
        csh1, csc1, cg1, csh2, csc2, cg2 = jnp.split(jax.nn.silu(c_ctx) @ w_mod[l] + b_mod[l], 6, axis=-1)
        h_lat = layer_norm(x_lat) * (1.0 + sc1) + sh1
        h_ctx = layer_norm(x_ctx) * (1.0 + csc1) + csh1
        y_lat, y_ctx = token_mixers(h_lat, h_ctx, w_in[l], pool_w[l], pool_scale[l], attn_sink[l],
                                    ret_decay[l], w_out[l], row, col, need_ctx)
        x_lat = layer_norm(DEEPNORM_ALPHA * x_lat + g1 * y_lat, ln_g[l, 0], ln_b[l, 0])
        h2_lat = (layer_norm(x_lat) * (1.0 + sc2) + sh2).reshape(B * N, D)
        if need_ctx:
            x_ctx = layer_norm(DEEPNORM_ALPHA * x_ctx + cg1 * y_ctx, ln_g[l, 0], ln_b[l, 0])
            h2_ctx = (layer_norm(x_ctx) * (1.0 + csc2) + csh2).reshape(B * L, D)
            f = moe_ffn(jnp.concatenate([h2_ctx, h2_lat], axis=0), router_w[l], router_b[l],
                        exp_w1[l], exp_b1[l], exp_w2[l], exp_b2[l])
            f_ctx = f[:B * L].reshape(B, L, D)
            f_lat = f[B * L:].reshape(B, N, D)
            x_ctx = layer_norm(DEEPNORM_ALPHA * x_ctx + cg2 * f_ctx, ln_g[l, 1], ln_b[l, 1])
        else:
            f_lat = moe_ffn(h2_lat, router_w[l], router_b[l], exp_w1[l], exp_b1[l],
                            exp_w2[l], exp_b2[l]).reshape(B, N, D)
        x_lat = layer_norm(DEEPNORM_ALPHA * x_lat + g2 * f_lat, ln_g[l, 1], ln_b[l, 1])
    return x_lat
```

```python
import numpy as np
import os
from contextlib import ExitStack
import concourse.bass as bass
import concourse.mybir as mybir
from concourse.bass_utils import run_bass_kernel_spmd

F32 = mybir.dt.float32
I32 = mybir.dt.int32
AF = mybir.ActivationFunctionType
ALU = mybir.AluOpType
AX = mybir.AxisListType


class Buf:
    __slots__ = ("w", "r")

    def __init__(self):
        self.w = None
        self.r = {}


class Prog:
    ENGS = ("pe", "act", "pool", "dve", "sp")

    def __init__(self, nc, es, ring=12):
        self.nc = nc
        self.es = es
        self.eng = {"pe": nc.tensor, "act": nc.scalar, "pool": nc.gpsimd, "dve": nc.vector, "sp": nc.sync}
        self.sem = {}
        self.epoch = 0
        self.nsem = 0
        self._new_epoch_sems()
        self.ring = {}
        self.rr = {}
        for q in ("sp", "pool", "act"):
            self.ring[q] = []
            for i in range(ring):
                key = ("dma", q, i)
                self.sem[key] = self._mk(f"d_{q}_{i}")
                self.ring[q].append([key, 0])
            self.rr[q] = 0

    def _mk(self, name):
        self.nsem += 1
        return self.es.enter_context(self.nc.semaphore(name))

    def _new_epoch_sems(self):
        self.cnt = {}
        self.waited = {e: {} for e in self.ENGS}
        for e in self.ENGS:
            key = ("eng", e, self.epoch)
            self.sem[key] = self._mk(f"e_{e}_{self.epoch}")
            self.cnt[e] = 0

    def ekey(self, e):
        return ("eng", e, self.epoch)

    def _deps(self, reads, writes):
        deps = []
        for b in reads:
            if b.w is not None:
                deps.append(b.w)
        for b in writes:
            if b.w is not None:
                deps.append(b.w)
            deps.extend(b.r.values())
        return deps

    def _waits(self, e, deps):
        for (s, v) in deps:
            if s[0] == "eng":
                if s[2] != self.epoch:
                    continue
                if s[1] == "pe" and e == "pe":
                    continue
            if self.waited[e].get(s, 0) >= v:
                continue
            self.waited[e][s] = v
            self.eng[e].wait_ge(self.sem[s], v)

    def _mark(self, tok, reads, writes):
        for b in reads:
            b.r[tok[0]] = tok
        for b in writes:
            b.w = tok
            b.r = {}

    def op(self, e, fn, reads=(), writes=()):
        self._waits(e, self._deps(reads, writes))
        self.cnt[e] += 1
        k = self.ekey(e)
        tok = (k, self.cnt[e])
        fn(self.eng[e]).then_inc(self.sem[k], 1)
        self._mark(tok, reads, writes)
        return tok

    def dma(self, q, fn, reads=(), writes=(), serial=False):
        ring = self.ring[q]
        i = self.rr[q]
        self.rr[q] = (i + 1) % len(ring)
        key, n = ring[i]
        deps = self._deps(reads, writes)
        if serial and getattr(self, "last_serial", None) is not None:
            deps.append(self.last_serial)
        if n > 0:
            deps.append((key, 16 * n))
        self._waits(q, deps)
        ring[i][1] = n + 1
        tok = (key, 16 * (n + 1))
        fn(self.eng[q]).then_inc(self.sem[key], 16)
        self._mark(tok, reads, writes)
        if serial:
            self.last_serial = tok
        return tok

    def barrier(self, new_epoch=True):
        toks = [(self.ekey(e), self.cnt[e]) for e in self.ENGS if self.cnt[e] > 0]
        for q in self.ring:
            for key, n in self.ring[q]:
                if n > 0:
                    toks.append((key, 16 * n))
        for e in self.ENGS:
            saved = dict(self.waited[e])
            for (s, v) in toks:
                if s[0] == "eng" and s[1] == e:
                    pass
                if self.waited[e].get(s, 0) >= v:
                    continue
                self.waited[e][s] = v
                self.eng[e].wait_ge(self.sem[s], v)
        if new_epoch:
            self.epoch += 1
            dma_waited = {e: {s: v for s, v in self.waited[e].items() if s[0] == "dma"} for e in self.ENGS}
            self._new_epoch_sems()
            self.waited = dma_waited

    def finish(self):
        self.barrier(new_epoch=False)


D = 1024
CTX = 256
ALPHA = 4 ** 0.25
EPS = 1e-6
NEG = -1e30
NEXP = 32


def host_consts(N):
    f64 = np.float64
    c = {}
    c["ident"] = np.eye(128, dtype=np.float32)
    cc = np.arange(64)
    ang = 2 * np.pi * np.outer(cc, cc) / 64
    BC = np.zeros((128, 128), f64); BS = np.zeros((128, 128), f64)
    for g in range(2):
        BC[g * 64:(g + 1) * 64, g * 64:(g + 1) * 64] = np.cos(ang)
        BS[g * 64:(g + 1) * 64, g * 64:(g + 1) * 64] = np.sin(ang)
    c["BC"] = BC.astype(np.float32); c["BS"] = BS.astype(np.float32)
    for nm, Nn in (("L", N), ("C", CTX)):
        N1 = Nn // 128
        a1 = 2 * np.pi * np.outer(np.arange(N1), np.arange(N1)) / N1
        C1, S1 = np.cos(a1), np.sin(a1)
        c["FAP" + nm] = np.concatenate([C1, S1], 1).astype(np.float32)
        c["FAQ" + nm] = np.concatenate([-S1, C1], 1).astype(np.float32)
        at = 2 * np.pi * np.outer(np.arange(128), np.arange(N1)) / Nn
        c["TWC" + nm] = np.cos(at).astype(np.float32)
        c["TWS" + nm] = np.sin(at).astype(np.float32)
        a2 = 2 * np.pi * np.outer(np.arange(128), np.arange(128)) / 128
        sc = 1.0 / np.sqrt(Nn * 64.0)
        c["FBC" + nm] = (np.cos(a2) * sc).astype(np.float32)
        c["FBS" + nm] = (-np.sin(a2) * sc).astype(np.float32)
    PB = np.zeros((128, 20, 128), f64)
    for g, w in enumerate((2, 4, 8, 16)):
        h = w // 2
        for t in range(128):
            for s in range(t - h, t + h):
                if 0 <= s < 128:
                    PB[s, g * 5 + 0, t] += 1.0 / w
                    cnt_first = min(t + h, 10 ** 9) - max(t - h, 0)
                    PB[s, g * 5 + 1, t] += 1.0 / cnt_first
                    cnt_last = min(t + h, 128) - (t - h)
                    PB[s, g * 5 + 2, t] += 1.0 / cnt_last
                elif s < 0:
                    PB[s + 128, g * 5 + 3, t] += 1.0 / w
                else:
                    PB[s - 128, g * 5 + 4, t] += 1.0 / w
            for k in range(3):
                PB[t, g * 5 + k, t] -= 1.0
    c["PB"] = PB.astype(np.float32)
    inv = 1.0 / (10000.0 ** (np.arange(0, 32, 2, dtype=np.float32) / np.float32(32)))
    t = np.arange(N)
    row = (t // 64).astype(np.float32); col = (t % 64).astype(np.float32)
    angr = (row[:, None] * inv[None, :]).astype(np.float32)
    angc = (col[:, None] * inv[None, :]).astype(np.float32)
    c["ROPC"] = np.concatenate([np.cos(angr.astype(f64)), np.cos(angc.astype(f64))], 1).astype(np.float32)
    c["ROPS"] = np.concatenate([np.sin(angr.astype(f64)), np.sin(angc.astype(f64))], 1).astype(np.float32)
    r = np.arange(128)
    c["MASKP"] = np.where(r[None, :] >= r[:, None], 0.0, NEG).astype(np.float32)
    c["MASKN"] = np.where(r[None, :] <= r[:, None], 0.0, NEG).astype(np.float32)
    posz = np.zeros((128, 4, 2), np.float32)
    posz[:, :, 0] = (127 - r)[:, None]; posz[:, :, 1] = r[:, None]
    c["POSZ"] = posz
    posxi = np.zeros((128, 128), np.float32)
    posxi[0:64, :] = (r + 1)[None, :]; posxi[64:128, :] = (128 - r)[None, :]
    c["POSXI"] = posxi
    dq = r[None, :] - r[:, None]
    c["PF"] = np.maximum(dq, 0).astype(np.float32); c["MF"] = (dq >= 0).astype(np.float32)
    c["PBK"] = np.maximum(-dq, 0).astype(np.float32); c["MB"] = (dq <= 0).astype(np.float32)
    c["UT"] = (r[:, None] < r[None, :]).astype(np.float32)
    c["ONES"] = np.ones((128, 128), np.float32)
    kp = (np.arange(8)[None, :] * 128 + r[:, None]).astype(np.float32)
    c["KP"] = kp
    return c


def build_program(N, debug=False):
    NL = N // 128
    NT = NL + 2
    T = NT * 128
    NBLK = (T * 4) // 128 + NEXP
    PR = NBLK * 128
    nc = bass.Bass("TRN2", target_bir_lowering=False)
    dr = lambda name, shape, dt=F32, kind="ExternalInput": nc.dram_tensor(name, shape, dt, kind=kind).ap()
    xin = dr("xin", [T, D])
    cc_in = dr("cc", [128, 8, 2])
    w_mod = dr("w_mod", [2, D, 6 * D]); b_mod = dr("b_mod", [2, 6 * D])
    w_in = dr("w_in", [2, D, 2048]); pool_w = dr("pool_w", [2, 4, 64, 64]); pool_scale = dr("pool_scale", [2, 256])
    attn_sink = dr("attn_sink", [2, 4]); ret_decay = dr("ret_decay", [2, 2, 4]); w_out = dr("w_out", [2, D, D])
    ln_g = dr("ln_g", [2, 2, D]); ln_b = dr("ln_b", [2, 2, D])
    router_w = dr("router_w", [2, D, NEXP]); router_b = dr("router_b", [2, NEXP])
    exp_w1 = dr("exp_w1", [2, NEXP, D, 2048]); exp_b1 = dr("exp_b1", [2, NEXP, 2048])
    exp_w2 = dr("exp_w2", [2, NEXP, D, D]); exp_b2 = dr("exp_b2", [2, NEXP, D])
    hc = host_consts(N)
    jv = (np.arange(NBLK, dtype=np.float32) * 128)[None, :].repeat(128, 0)
    hc["JV"] = jv
    cst = {k: dr("c_" + k, list(v.shape)) for k, v in hc.items()}
    out = dr("out", [N, D], kind="ExternalOutput")
    ik = "ExternalOutput" if debug else "Internal"
    X = dr("X", [T, D], kind=ik)
    U = dr("U", [T, 2048], kind=ik)
    PTD = dr("PTD", [256, T], kind=ik); QTD = dr("QTD", [256, T], kind=ik)
    M = dr("M", [T, D], kind=ik)
    H2 = dr("H2", [T, D], kind=ik)
    MODS = dr("MODS", [2, 6 * D], kind=ik)
    XS = dr("XS", [PR, D], kind=ik); YS = dr("YS", [PR, D], kind=ik)
    DBG = dr("DBG", [128, 4096], kind=ik); DBGI = dr("DBGI", [128, 4096], I32, kind=ik)

    with ExitStack() as es0:
        P = Prog(nc, es0)
        op, dma = P.op, P.dma

        uid = [0]

        def pools(es):
            def sb(name, shape, dt=F32):
                uid[0] += 1
                return (es.enter_context(nc.sbuf_tensor(f"{name}_{uid[0]}", shape, dt)), Buf())

            def ps(name, shape, dt=F32):
                uid[0] += 1
                free = ((shape[1] + 511) // 512) * 512
                t = es.enter_context(nc.psum_tensor(f"{name}_{uid[0]}", [shape[0], free], dt))
                return (t[:, 0:shape[1]], Buf())
            return sb, ps

        def load(es, name, src, shape, q="sp"):
            t, b = pools(es)[0](name, shape)
            dma(q, lambda e: e.dma_start(out=t[:], in_=src), writes=[b])
            return t, b

        def bc(v):
            return v.partition_broadcast(128)

        sb0, _ = pools(es0)
        ID, bID = load(es0, "ID", cst["ident"], [128, 128])
        LOG, bLOG = sb0("LOG", [128, NT, NEXP])

        def layer_norm(es, tag, src, bsrc, dst, bdst, tmp):
            st, bst = tmp["st"]; mv, bmv = tmp["mv"]; rs, brs = tmp["rs"]
            for j in range(2):
                op("dve", lambda e: e.bn_stats(out=st[:, j, :], in_=src[:, j * 512:(j + 1) * 512]), reads=[bsrc], writes=[bst])
            op("dve", lambda e: e.bn_aggr(out=mv[:], in_=st[:].rearrange("p a b -> p (a b)")), reads=[bst], writes=[bmv])
            op("act", lambda e: e.activation(out=rs[:], in_=mv[:, 1:2], func=AF.Sqrt, bias=EPS, scale=1.0), reads=[bmv], writes=[brs])
            op("dve", lambda e: e.reciprocal(out=rs[:], in_=rs[:]), reads=[brs], writes=[brs])
            op("dve", lambda e: e.tensor_scalar(out=dst[:], in0=src[:], scalar1=mv[:, 0:1], scalar2=rs[:, 0:1], op0=ALU.subtract, op1=ALU.mult),
               reads=[bsrc, bmv, brs], writes=[bdst])

        def ln_tmp(es, tag):
            sb, _ = pools(es)
            return {"st": sb("st" + tag, [128, 2, 6]), "mv": sb("mv" + tag, [128, 2]), "rs": sb("rs" + tag, [128, 1])}

        def transpose8(src, bsrc, pT, bpT, dstT, bdstT, eng="act"):
            for k in range(8):
                op("pe", lambda e: e.transpose(out=pT[:, k * 128:(k + 1) * 128], in_=src[:, k * 128:(k + 1) * 128], identity=ID[:]),
                   reads=[bsrc, bID], writes=[bpT])
            if eng == "act":
                op("act", lambda e: e.copy(out=dstT[:].rearrange("p a b -> p (a b)"), in_=pT[:]), reads=[bpT], writes=[bdstT])
            else:
                op("dve", lambda e: e.tensor_copy(out=dstT[:].rearrange("p a b -> p (a b)"), in_=pT[:]), reads=[bpT], writes=[bdstT])

        def mul_add(dst, bdst, src, bsrc, A, bA, B, bB):
            op("dve", lambda e: e.tensor_tensor(out=dst[:], in0=src[:], in1=A[:], op=ALU.mult), reads=[bsrc, bA], writes=[bdst])
            op("dve", lambda e: e.tensor_tensor(out=dst[:], in0=dst[:], in1=B[:], op=ALU.add), reads=[bdst, bB], writes=[bdst])

        for l in range(2):
            need_ctx = (l == 0)
            xsrc = xin if l == 0 else X
            with ExitStack() as es:
                sb, ps = pools(es)
                CC, bCC = load(es, "CC", cc_in, [128, 8, 2])
                op("act", lambda e: e.activation(out=CC[:], in_=CC[:], func=AF.Silu), reads=[bCC], writes=[bCC])
                BM, bBM = sb("BM", [2, 6 * D])
                dma("sp", lambda e: e.dma_start(out=BM[:], in_=b_mod[l].partition_broadcast(2)), writes=[bBM])
                MO, bMO = sb("MO", [2, 6 * D])
                WM = [sb(f"WM{i}", [128, 3072]) for i in range(2)]
                pM, bpM = ps("pM", [2, 3072])
                it = 0
                for half in range(2):
                    for kc in range(8):
                        W, bW = WM[it % 2]; it += 1
                        dma("sp", lambda e: e.dma_start(out=W[:], in_=w_mod[l, kc * 128:(kc + 1) * 128, half * 3072:(half + 1) * 3072]), writes=[bW])
                        for n in range(6):
                            op("pe", lambda e: e.matmul(pM[:, n * 512:(n + 1) * 512], lhsT=CC[:, kc, :], rhs=W[:, n * 512:(n + 1) * 512],
                                                        start=(kc == 0), stop=(kc == 7)), reads=[bCC, bW], writes=[bpM])
                    op("dve", lambda e: e.tensor_tensor(out=MO[:, half * 3072:(half + 1) * 3072], in0=pM[:], in1=BM[:, half * 3072:(half + 1) * 3072], op=ALU.add),
                       reads=[bpM, bBM], writes=[bMO])
                for s in (1, 4):
                    op("dve", lambda e: e.tensor_scalar(out=MO[:, s * D:(s + 1) * D], in0=MO[:, s * D:(s + 1) * D], scalar1=1.0, scalar2=None, op0=ALU.add),
                       reads=[bMO], writes=[bMO])
                dma("sp", lambda e: e.dma_start(out=MODS, in_=MO[:]), reads=[bMO])
            P.barrier()

            def modv(es, name, which, slot):
                return load(es, name, bc(MODS[which, slot * D:(slot + 1) * D]), [128, D])

            with ExitStack() as es:
                sb, ps = pools(es)
                WI, bWI = load(es, "WI", w_in[l].rearrange("(k p) n -> p k n", p=128), [128, 8, 2048])
                SC = [modv(es, "SC1L", 0, 1), modv(es, "SC1C", 1, 1)]
                SH = [modv(es, "SH1L", 0, 0), modv(es, "SH1C", 1, 0)]
                BCt, bBC = load(es, "BCt", cst["BC"], [128, 128]); BSt, bBS = load(es, "BSt", cst["BS"], [128, 128])
                xt = [sb(f"xt{i}", [128, D]) for i in range(2)]
                hts1 = [sb(f"ht{i}", [128, D]) for i in range(2)]; hTs1 = [sb(f"hT{i}", [128, 8, 128]) for i in range(2)]
                ut = [sb(f"ut{i}", [128, 2048]) for i in range(2)]
                aT, baT = sb("aT", [128, 2, 128]); pq = [sb(f"pq{i}", [128, 4, 128]) for i in range(2)]
                tmp = ln_tmp(es, "1")
                pT, bpT = ps("pT", [128, 1024]); pU, bpU = ps("pU", [128, 2048])
                bpUn = [Buf() for _ in range(4)]; bUtn = [[Buf() for _ in range(4)] for _ in range(2)]
                pA, bpA = ps("pA", [128, 512]); pPQ, bpPQ = ps("pPQ", [128, 512])
                for i in range(NT):
                    ctxk = 1 if i < 2 else 0
                    Xt, bX = xt[i % 2]; Ut, bUt = ut[i % 2]; PQ, bPQ = pq[i % 2]; ht, bht = hts1[i % 2]; hT, bhT = hTs1[i % 2]
                    dma("sp", lambda e: e.dma_start(out=Xt[:], in_=xsrc[i * 128:(i + 1) * 128, :]), writes=[bX])
                    layer_norm(es, "1", Xt, bX, ht, bht, tmp)
                    mul_add(ht, bht, ht, bht, SC[ctxk][0], SC[ctxk][1], SH[ctxk][0], SH[ctxk][1])
                    transpose8(ht, bht, pT, bpT, hT, bhT)
                    for n in range(4):
                        for k in range(8):
                            op("pe", lambda e: e.matmul(pU[:, n * 512:(n + 1) * 512], lhsT=hT[:, k, :], rhs=WI[:, k, n * 512:(n + 1) * 512],
                                                        start=(k == 0), stop=(k == 7)), reads=[bhT, bWI], writes=[bpUn[n]])
                        if n % 2 == 0:
                            op("act", lambda e: e.copy(out=Ut[:, n * 512:(n + 1) * 512], in_=pU[:, n * 512:(n + 1) * 512]), reads=[bpUn[n]], writes=[bUtn[i % 2][n]])
                        else:
                            op("dve", lambda e: e.tensor_copy(out=Ut[:, n * 512:(n + 1) * 512], in_=pU[:, n * 512:(n + 1) * 512]), reads=[bpUn[n]], writes=[bUtn[i % 2][n]])
                    dma("pool", lambda e: e.dma_start(out=U[i * 128:(i + 1) * 128, :], in_=Ut[:]), reads=bUtn[i % 2])
                    for kc in range(2):
                        op("pe", lambda e: e.transpose(out=pA[:, kc * 128:(kc + 1) * 128], in_=Ut[:, kc * 128:(kc + 1) * 128], identity=ID[:]),
                           reads=[bUtn[i % 2][0], bID], writes=[bpA])
                    op("dve", lambda e: e.tensor_copy(out=aT[:].rearrange("p a b -> p (a b)"), in_=pA[:, 0:256]), reads=[bpA], writes=[baT])
                    for kc in range(2):
                        op("pe", lambda e: e.matmul(pPQ[:, kc * 128:(kc + 1) * 128], lhsT=BCt[:], rhs=aT[:, kc, :], start=True, stop=True),
                           reads=[baT, bBC], writes=[bpPQ])
                        op("pe", lambda e: e.matmul(pPQ[:, (2 + kc) * 128:(3 + kc) * 128], lhsT=BSt[:], rhs=aT[:, kc, :], start=True, stop=True),
                           reads=[baT, bBS], writes=[bpPQ])
                    op("dve", lambda e: e.tensor_copy(out=PQ[:].rearrange("p a b -> p (a b)"), in_=pPQ[:]), reads=[bpPQ], writes=[bPQ])
                    dma("pool", lambda e: e.dma_start(out=PTD[:, i * 128:(i + 1) * 128].rearrange("(k p) t -> p k t", p=128), in_=PQ[:, 0:2, :]), reads=[bPQ])
                    dma("pool", lambda e: e.dma_start(out=QTD[:, i * 128:(i + 1) * 128].rearrange("(k p) t -> p k t", p=128), in_=PQ[:, 2:4, :]), reads=[bPQ])
            P.barrier()
            if debug and l == 0 and debug == "p1":
                break
            seqs = [("L", 2, NL)] + ([("C", 0, 2)] if need_ctx else [])
            for (nm, t0, N1) in seqs:
                off = t0 * 128
                with ExitStack() as es:
                    sb, ps = pools(es)
                    FAP, bFAP = load(es, "FAP", cst["FAP" + nm], [N1, 2 * N1]); FAQ, bFAQ = load(es, "FAQ", cst["FAQ" + nm], [N1, 2 * N1])
                    TWC, bTWC = load(es, "TWC", cst["TWC" + nm], [128, N1]); TWS, bTWS = load(es, "TWS", cst["TWS" + nm], [128, N1])
                    FBC, bFBC = load(es, "FBC", cst["FBC" + nm], [128, 128]); FBS, bFBS = load(es, "FBS", cst["FBS" + nm], [128, 128])
                    PTs, bPTs = sb("PTs", [N1, 64, 128]); QTs, bQTs = sb("QTs", [N1, 64, 128])
                    At, bAt = sb("At", [128, 64, N1]); Bt, bBt = sb("Bt", [128, 64, N1])
                    cpa = min(64, 512 // (2 * N1)); cpb = min(64, 512 // N1)
                    tt = [sb(f"ft{i}", [128, cpa, N1]) for i in range(4)]
                    Yt, bYt = sb("Yt", [128, N1, 64])
                    pZ = [ps(f"pZ{i}", [128, 512]) for i in range(2)]; pY = [ps(f"pY{i}", [128, 512]) for i in range(2)]
                    for g in range(4):
                        dma("sp", lambda e: e.dma_start(out=PTs[:], in_=PTD[g * 64:(g + 1) * 64, off:off + 128 * N1].rearrange("c (a b) -> a c b", b=128)), writes=[bPTs])
                        dma("sp", lambda e: e.dma_start(out=QTs[:], in_=QTD[g * 64:(g + 1) * 64, off:off + 128 * N1].rearrange("c (a b) -> a c b", b=128)), writes=[bQTs])
                        for ci0, c0 in enumerate(range(0, 64, cpa)):
                            Z, bZ = pZ[ci0 % 2]
                            for ci in range(cpa):
                                c = c0 + ci
                                op("pe", lambda e: e.matmul(Z[:, ci * 2 * N1:(ci + 1) * 2 * N1], lhsT=PTs[:, c, :], rhs=FAP[:], start=True, stop=False), reads=[bPTs, bFAP], writes=[bZ])
                                op("pe", lambda e: e.matmul(Z[:, ci * 2 * N1:(ci + 1) * 2 * N1], lhsT=QTs[:, c, :], rhs=FAQ[:], start=False, stop=True), reads=[bQTs, bFAQ], writes=[bZ])
                            Zv = Z[:, 0:cpa * 2 * N1].rearrange("p (c t k) -> p c t k", t=2, k=N1)
                            Wv = Zv[:, :, 0, :]; Vv = Zv[:, :, 1, :]
                            cb = TWC[:].unsqueeze(1).to_broadcast([128, cpa, N1]); sbb = TWS[:].unsqueeze(1).to_broadcast([128, cpa, N1])
                            (t1, b1), (t2, b2), (t3, b3), (t4, b4) = tt
                            op("dve", lambda e: e.tensor_tensor(out=t1[:], in0=Wv, in1=cb, op=ALU.mult), reads=[bZ, bTWC], writes=[b1])
                            op("dve", lambda e: e.tensor_tensor(out=t2[:], in0=Vv, in1=sbb, op=ALU.mult), reads=[bZ, bTWS], writes=[b2])
                            op("dve", lambda e: e.tensor_tensor(out=t3[:], in0=Wv, in1=sbb, op=ALU.mult), reads=[bZ, bTWS], writes=[b3])
                            op("dve", lambda e: e.tensor_tensor(out=t4[:], in0=Vv, in1=cb, op=ALU.mult), reads=[bZ, bTWC], writes=[b4])
                            op("pool", lambda e: e.tensor_tensor(out=At[:, c0:c0 + cpa, :], in0=t1[:], in1=t2[:], op=ALU.subtract), reads=[b1, b2], writes=[bAt])
                            op("pool", lambda e: e.tensor_tensor(out=Bt[:, c0:c0 + cpa, :], in0=t3[:], in1=t4[:], op=ALU.add), reads=[b3, b4], writes=[bBt])
                        for ci0, c0 in enumerate(range(0, 64, cpb)):
                            Y, bY = pY[ci0 % 2]
                            ncol = cpb * N1
                            op("pe", lambda e: e.matmul(Y[:, 0:ncol], lhsT=FBC[:], rhs=At[:, c0:c0 + cpb, :].rearrange("p c k -> p (c k)"), start=True, stop=False), reads=[bAt, bFBC], writes=[bY])
                            op("pe", lambda e: e.matmul(Y[:, 0:ncol], lhsT=FBS[:], rhs=Bt[:, c0:c0 + cpb, :].rearrange("p c k -> p (c k)"), start=False, stop=True), reads=[bBt, bFBS], writes=[bY])
                            op("act", lambda e: e.copy(out=Yt[:, :, c0:c0 + cpb].rearrange("p k c -> p c k"), in_=Y[:, 0:ncol].rearrange("p (c k) -> p c k", k=N1)), reads=[bY], writes=[bYt])
                        dma("pool", lambda e: e.dma_start(out=M[off:off + 128 * N1, g * 64:(g + 1) * 64].rearrange("(a b) c -> a b c", b=N1), in_=Yt[:]), reads=[bYt])
                P.barrier(new_epoch=False)
            if debug == "p2a":
                break
            with ExitStack() as es:
                sb, ps = pools(es)
                PBt, bPBt = load(es, "PBt", cst["PB"], [128, 20, 128])
                PW, bPW = load(es, "PW", pool_w[l].rearrange("g c d -> c g d"), [64, 4, 64])
                PSC, bPSC = load(es, "PSC", bc(pool_scale[l]), [128, 256])
                bt = [[sb(f"bt{i}_{j}", [128, 256]) for j in range(3)] for i in range(2)]
                pooledT, bpooledT = sb("pooledT", [64, 4, 128])
                ots = [sb(f"pot{i}", [128, 256]) for i in range(2)]
                pP, bpP = ps("pP", [64, 512]); pO, bpO = ps("pO", [128, 256])
                it = 0
                for (nm, t0, nt) in seqs:
                    for i in range(nt):
                        B3 = bt[it % 2]; ot, bot = ots[it % 2]; it += 1
                        for j, ti in enumerate((i - 1, i, i + 1)):
                            if 0 <= ti < nt:
                                dma("sp", lambda e: e.dma_start(out=B3[j][0][:], in_=U[(t0 + ti) * 128:(t0 + ti + 1) * 128, 256:512]), writes=[B3[j][1]])
                        kind = 1 if i == 0 else (2 if i == nt - 1 else 0)
                        terms = [(1, kind)] + ([(0, 3)] if i > 0 else []) + ([(2, 4)] if i < nt - 1 else [])
                        for g in range(4):
                            for ti_, (j, kd) in enumerate(terms):
                                op("pe", lambda e: e.matmul(pP[:, g * 128:(g + 1) * 128], lhsT=B3[j][0][:, g * 64:(g + 1) * 64], rhs=PBt[:, g * 5 + kd, :],
                                                            start=(ti_ == 0), stop=(ti_ == len(terms) - 1)), reads=[B3[j][1], bPBt], writes=[bpP])
                        op("act", lambda e: e.copy(out=pooledT[:].rearrange("p a b -> p (a b)"), in_=pP[:]), reads=[bpP], writes=[bpooledT])
                        for g in range(4):
                            op("pe", lambda e: e.matmul(pO[:, g * 64:(g + 1) * 64], lhsT=pooledT[:, g, :], rhs=PW[:, g, :], start=True, stop=True), reads=[bpooledT, bPW], writes=[bpO])
                        op("dve", lambda e: e.tensor_tensor(out=ot[:], in0=pO[:], in1=PSC[:], op=ALU.mult), reads=[bpO, bPSC], writes=[bot])
                        dma("pool", lambda e: e.dma_start(out=M[(t0 + i) * 128:(t0 + i + 1) * 128, 256:512], in_=ot[:]), reads=[bot])
            P.barrier(new_epoch=False)
            if debug == "p2b":
                break
            with ExitStack() as es:
                sb, ps = pools(es)
                MP = load(es, "MP", cst["MASKP"], [128, 128]); MN = load(es, "MN", cst["MASKN"], [128, 128])
                SINK, bSINK = load(es, "SINK", bc(attn_sink[l]), [128, 4])
                NSINK, bNSINK = sb("NSINK", [128, 4])
                op("dve", lambda e: e.tensor_scalar(out=NSINK[:], in0=SINK[:], scalar1=-1.0, scalar2=None, op0=ALU.mult), reads=[bSINK], writes=[bNSINK])
                RING = 4
                QKV = [sb(f"QKV{i}", [128, 512]) for i in range(RING)]
                qT = [sb(f"qT{i}", [64, 4, 128]) for i in range(RING)]
                kT = [sb(f"kT{i}", [64, 2, 128]) for i in range(RING)]
                cQKV = [sb(f"cQKV{i}", [128, 512]) for i in range(2)]
                cqT = [sb(f"cqT{i}", [64, 4, 128]) for i in range(2)]
                ckT = [sb(f"ckT{i}", [64, 2, 128]) for i in range(2)]
                RC = [sb(f"RC{i}", [128, 32]) for i in range(2)]; RS = [sb(f"RS{i}", [128, 32]) for i in range(2)]
                rtmp, brtmp = sb("rtmp", [128, 6, 2, 16]); rt2, brt2 = sb("rt2", [128, 6, 16]); rt3, brt3 = sb("rt3", [128, 6, 16])
                scs = [sb(f"sc{i}", [128, 2, 5, 128]) for i in range(2)]; prs = [sb(f"pr{i}", [128, 2, 640]) for i in range(2)]; pTss = [sb(f"pTs{i}", [128, 2, 640]) for i in range(2)]
                rmaxs = [sb(f"rmax{i}", [128, 2]) for i in range(2)]; negms = [sb(f"negm{i}", [128, 2]) for i in range(2)]
                rsums = [sb(f"rsum{i}", [128, 2]) for i in range(2)]; esks = [sb(f"esk{i}", [128, 2]) for i in range(2)]
                aos = [sb(f"ao{i}", [128, 256]) for i in range(2)]
                if l == 0:
                    ZT, bZT = sb("ZT", [128, 8192])
                    op("pool", lambda e: e.memset(ZT[:], 0.0), writes=[bZT])
                pS, bpS = ps("pS", [128, 1536]); pPT, bpPT = ps("pPT", [128, 1536]); pO, bpO = ps("pOa", [128, 256])
                pQK, bpQK = pPT[0:64, 0:1024], bpPT

                def prep(ti, QKVt, qTt, kTt, rope_slot):
                    Q, bQ = QKVt
                    dma("sp", lambda e: e.dma_start(out=Q[:], in_=U[ti * 128:(ti + 1) * 128, 512:1024]), writes=[bQ])
                    if rope_slot is not None:
                        (C, bC), (S, bS) = RC[rope_slot], RS[rope_slot]
                        r0 = (ti - 2) * 128
                        dma("sp", lambda e: e.dma_start(out=C[:], in_=cst["ROPC"][r0:r0 + 128, :]), writes=[bC])
                        dma("sp", lambda e: e.dma_start(out=S[:], in_=cst["ROPS"][r0:r0 + 128, :]), writes=[bS])
                        for hf in range(2):
                            xv = Q[:, 0:384].rearrange("p (h f t d) -> p h f t d", h=6, f=2, t=2)[:, :, hf, :, :]
                            cb = C[:, hf * 16:(hf + 1) * 16].unsqueeze(1).unsqueeze(1).to_broadcast([128, 6, 2, 16])
                            sbb = S[:, hf * 16:(hf + 1) * 16].unsqueeze(1).to_broadcast([128, 6, 16])
                            op("dve", lambda e: e.tensor_tensor(out=rtmp[:], in0=xv, in1=cb, op=ALU.mult), reads=[bQ, bC], writes=[brtmp])
                            op("dve", lambda e: e.tensor_tensor(out=rt2[:], in0=xv[:, :, 1, :], in1=sbb, op=ALU.mult), reads=[bQ, bS], writes=[brt2])
                            op("dve", lambda e: e.tensor_tensor(out=rt3[:], in0=xv[:, :, 0, :], in1=sbb, op=ALU.mult), reads=[bQ, bS], writes=[brt3])
                            op("dve", lambda e: e.tensor_tensor(out=xv[:, :, 0, :], in0=rtmp[:, :, 0, :], in1=rt2[:], op=ALU.subtract), reads=[brtmp, brt2], writes=[bQ])
                            op("dve", lambda e: e.tensor_tensor(out=xv[:, :, 1, :], in0=rtmp[:, :, 1, :], in1=rt3[:], op=ALU.add), reads=[brtmp, brt3], writes=[bQ])
                    for h in range(6):
                        op("pe", lambda e: e.transpose(out=pQK[:, h * 128:(h + 1) * 128], in_=Q[:, h * 64:(h + 1) * 64], identity=ID[:]), reads=[bQ, bID], writes=[bpQK])
                    op("act", lambda e: e.copy(out=qTt[0][:].rearrange("p a b -> p (a b)"), in_=pQK[:, 0:512]), reads=[bpQK], writes=[qTt[1]])
                    op("act", lambda e: e.copy(out=kTt[0][:].rearrange("p a b -> p (a b)"), in_=pQK[:, 512:768]), reads=[bpQK], writes=[kTt[1]])

                def attend(qTt, blocks, ti, ao_t):
                    ao, bao = ao_t
                    nb = len(blocks); Wd = nb * 128

                    def stA(hp):
                        (sc, bsc), (pr, bpr) = scs[hp % 2], prs[hp % 2]
                        (rmax, brmax), (negm, bnegm), (rsum, brsum), (esk, besk) = rmaxs[hp % 2], negms[hp % 2], rsums[hp % 2], esks[hp % 2]
                        for g in range(2):
                            for bi, (kTt, Vt, mask) in enumerate(blocks):
                                op("pe", lambda e: e.matmul(pS[:, g * 640 + bi * 128:g * 640 + (bi + 1) * 128], lhsT=qTt[0][:, 2 * hp + g, :], rhs=kTt[0][:, hp, :], start=True, stop=True),
                                   reads=[qTt[1], kTt[1]], writes=[bpS])
                        pSv = pS[:, 0:1280].rearrange("p (g b c) -> p g b c", g=2, b=5)
                        for bi, (kTt, Vt, mask) in enumerate(blocks):
                            if mask is not None:
                                op("dve", lambda e: e.tensor_tensor(out=sc[:, :, bi, :], in0=pSv[:, :, bi, :], in1=mask[0][:].unsqueeze(1).to_broadcast([128, 2, 128]), op=ALU.add),
                                   reads=[bpS, mask[1]], writes=[bsc])
                            else:
                                op("act", lambda e: e.copy(out=sc[:, :, bi, :], in_=pSv[:, :, bi, :]), reads=[bpS], writes=[bsc])
                        for g in range(2):
                            scg = sc[:, g, 0:nb, :].rearrange("p b c -> p (b c)")
                            op("dve", lambda e: e.reduce_max(out=rmax[:, g:g + 1], in_=scg, axis=AX.X), reads=[bsc], writes=[brmax])
                        op("dve", lambda e: e.scalar_tensor_tensor(out=negm[:], in0=rmax[:], scalar=-0.125, in1=NSINK[:, 2 * hp:2 * hp + 2], op0=ALU.mult, op1=ALU.min),
                           reads=[brmax, bNSINK], writes=[bnegm])
                        for g in range(2):
                            scg = sc[:, g, 0:nb, :].rearrange("p b c -> p (b c)")
                            op("act", lambda e: e.activation(out=pr[:, g, 0:Wd], in_=scg, func=AF.Exp, bias=negm[:, g:g + 1], scale=0.125), reads=[bsc, bnegm], writes=[bpr])
                        for g in range(2):
                            op("dve", lambda e: e.reduce_sum(out=rsum[:, g:g + 1], in_=pr[:, g, 0:Wd], axis=AX.X), reads=[bpr], writes=[brsum])
                        op("dve", lambda e: e.tensor_tensor(out=esk[:], in0=SINK[:, 2 * hp:2 * hp + 2], in1=negm[:], op=ALU.add), reads=[bSINK, bnegm], writes=[besk])
                        op("act", lambda e: e.activation(out=esk[:], in_=esk[:], func=AF.Exp), reads=[besk], writes=[besk])
                        op("dve", lambda e: e.tensor_tensor(out=rsum[:], in0=rsum[:], in1=esk[:], op=ALU.add), reads=[brsum, besk], writes=[brsum])
                        op("dve", lambda e: e.reciprocal(out=rsum[:], in_=rsum[:]), reads=[brsum], writes=[brsum])

                    def stB(hp):
                        (pr, bpr), (pTs, bpTs), (rsum, brsum) = prs[hp % 2], pTss[hp % 2], rsums[hp % 2]
                        for g in range(2):
                            for bi in range(nb):
                                op("pe", lambda e: e.transpose(out=pPT[:, g * 640 + bi * 128:g * 640 + (bi + 1) * 128], in_=pr[:, g, bi * 128:(bi + 1) * 128], identity=ID[:]),
                                   reads=[bpr, bID], writes=[bpPT])
                        op("act", lambda e: e.copy(out=pTs[:, :, 0:Wd], in_=pPT[:, 0:1280].rearrange("p (g w) -> p g w", g=2)[:, :, 0:Wd]), reads=[bpPT], writes=[bpTs])
                        for g in range(2):
                            hq = 2 * hp + g
                            for bi, (kTt, Vt, mask) in enumerate(blocks):
                                op("pe", lambda e: e.matmul(pO[:, hq * 64:(hq + 1) * 64], lhsT=pTs[:, g, bi * 128:(bi + 1) * 128], rhs=Vt[0][:, 384 + hp * 64:384 + (hp + 1) * 64],
                                                            start=(bi == 0), stop=(bi == nb - 1)), reads=[bpTs, Vt[1]], writes=[bpO])
                        op("dve", lambda e: e.tensor_tensor(out=ao[:, hp * 128:(hp + 1) * 128].rearrange("p (g d) -> p g d", g=2),
                                                            in0=pO[:, hp * 128:(hp + 1) * 128].rearrange("p (g d) -> p g d", g=2),
                                                            in1=rsum[:].unsqueeze(2).to_broadcast([128, 2, 64]), op=ALU.mult), reads=[bpO, brsum], writes=[bao])

                    stA(0); stA(1); stB(0); stB(1)
                    dma("pool", lambda e: e.dma_start(out=M[ti * 128:(ti + 1) * 128, 512:768], in_=ao[:]), reads=[bao])

                for t in range(2):
                    prep(t, cQKV[t], cqT[t], ckT[t], None)
                cblocks = [(ckT[0], cQKV[0], None), (ckT[1], cQKV[1], None)]
                if need_ctx:
                    for t in range(2):
                        attend(cqT[t], cblocks, t, aos[t % 2])
                prep(2, QKV[0], qT[0], kT[0], 0)
                for i in range(NL):
                    if i + 1 < NL:
                        s = (i + 1) % RING
                        prep(i + 3, QKV[s], qT[s], kT[s], (i + 1) % 2)
                    blocks = []
                    if i > 0:
                        blocks.append((kT[(i - 1) % RING], QKV[(i - 1) % RING], MP))
                    blocks.append((kT[i % RING], QKV[i % RING], None))
                    if i + 1 < NL:
                        blocks.append((kT[(i + 1) % RING], QKV[(i + 1) % RING], MN))
                    blocks += cblocks
                    attend(qT[i % RING], blocks, i + 2, aos[i % 2])
                    if l == 0:
                        a0, a1 = (i * NBLK) // NL, ((i + 1) * NBLK) // NL
                        for a in range(a0, a1, 8):
                            ae = min(a1, a + 8)
                            dma("sp", lambda e: e.dma_start(out=XS.rearrange("(a p) d -> p a d", p=128)[:, a:ae, :], in_=ZT[:, 0:(ae - a) * D].rearrange("p (a d) -> p a d", d=D)), reads=[bZT])
            P.barrier(new_epoch=False)
            if debug == "p2c":
                break
            with ExitStack() as es:
                sb, ps = pools(es)
                POSZ, bPOSZ = load(es, "POSZ", cst["POSZ"], [128, 4, 2]); POSXI, bPOSXI = load(es, "POSXI", cst["POSXI"], [128, 128])
                PFt, bPF = load(es, "PFt", cst["PF"], [128, 128]); MFt, bMF = load(es, "MFt", cst["MF"], [128, 128])
                PBKt, bPBK = load(es, "PBKt", cst["PBK"], [128, 128]); MBt, bMB = load(es, "MBt", cst["MB"], [128, 128])
                LGX, bLGX = sb("LGX", [128, 4]); LG, bLG = sb("LG", [128, 2, 4])
                dma("sp", lambda e: e.dma_start(out=LGX[0:64, :], in_=ret_decay[l, 0].partition_broadcast(64)), writes=[bLGX])
                dma("sp", lambda e: e.dma_start(out=LGX[64:128, :], in_=ret_decay[l, 1].partition_broadcast(64)), writes=[bLGX])
                dma("sp", lambda e: e.dma_start(out=LG[:].rearrange("p a b -> p (a b)"), in_=ret_decay[l].rearrange("a b -> (a b)").partition_broadcast(128)), writes=[bLG])
                for (Tt, bT) in ((LGX, bLGX), (LG, bLG)):
                    op("act", lambda e: e.activation(out=Tt[:], in_=Tt[:], func=AF.Exp), reads=[bT], writes=[bT])
                    op("dve", lambda e: e.tensor_scalar(out=Tt[:], in0=Tt[:], scalar1=-1.0, scalar2=None, op0=ALU.mult), reads=[bT], writes=[bT])
                ZETA, bZETA = sb("ZETA", [128, 4, 2]); XI, bXI = sb("XI", [128, 4, 128]); GC, bGC = sb("GC", [128, 4]); DS, bDS = sb("DS", [128, 4, 128])
                e1, be1 = sb("e1", [128, 128]); e2, be2 = sb("e2", [128, 128])
                op("dve", lambda e: e.tensor_tensor(out=ZETA[:], in0=POSZ[:], in1=LG[:].rearrange("p d h -> p h d"), op=ALU.mult), reads=[bPOSZ, bLG], writes=[bZETA])
                op("act", lambda e: e.activation(out=ZETA[:], in_=ZETA[:], func=AF.Exp), reads=[bZETA], writes=[bZETA])
                op("dve", lambda e: e.tensor_scalar(out=ZETA[:], in0=ZETA[:], scalar1=0.125, scalar2=None, op0=ALU.mult), reads=[bZETA], writes=[bZETA])
                op("act", lambda e: e.activation(out=GC[:], in_=LGX[:], func=AF.Exp, scale=128.0), reads=[bLGX], writes=[bGC])
                for h in range(4):
                    op("act", lambda e: e.activation(out=XI[:, h, :], in_=POSXI[:], func=AF.Exp, scale=LGX[:, h:h + 1]), reads=[bPOSXI, bLGX], writes=[bXI])
                    op("act", lambda e: e.activation(out=e1[:], in_=PFt[:], func=AF.Exp, scale=LG[:, 0, h:h + 1]), reads=[bPF, bLG], writes=[be1])
                    op("dve", lambda e: e.tensor_tensor(out=e1[:], in0=e1[:], in1=MFt[:], op=ALU.mult), reads=[be1, bMF], writes=[be1])
                    op("act", lambda e: e.activation(out=e2[:], in_=PBKt[:], func=AF.Exp, scale=LG[:, 1, h:h + 1]), reads=[bPBK, bLG], writes=[be2])
                    op("dve", lambda e: e.tensor_tensor(out=e2[:], in0=e2[:], in1=MBt[:], op=ALU.mult), reads=[be2, bMB], writes=[be2])
                    op("dve", lambda e: e.tensor_tensor(out=e1[:], in0=e1[:], in1=e2[:], op=ALU.add), reads=[be1, be2], writes=[be1])
                    op("dve", lambda e: e.tensor_scalar(out=DS[:, h, :], in0=e1[:], scalar1=0.125, scalar2=None, op0=ALU.mult), reads=[be1], writes=[bDS])
                RB, _ = sb("RB", [64, NT, 4, 64]); bRB = [Buf() for _ in range(NT)]
                RBB, _ = sb("RBB", [64, NT, 4, 64])
                GCB, bGCB = sb("GCB", [64, 4]); XIB, bXIB = sb("XIB", [64, 4, 128])
                PXB, bPXB = load(es, "PXB", cst["POSXI"][64:128, :], [64, 128])
                op("act", lambda e: e.activation(out=GCB[:], in_=LG[0:64, 1, :], func=AF.Exp, scale=128.0), reads=[bLG], writes=[bGCB])
                for h in range(4):
                    op("act", lambda e: e.activation(out=XIB[:, h, :], in_=PXB[:], func=AF.Exp, scale=LG[0:64, 1, h:h + 1]), reads=[bPXB, bLG], writes=[bXIB])
                QXBs = [sb(f"QXB{i}", [64, 128]) for i in range(2)]; kTrs = [sb(f"kTr{i}", [64, 128]) for i in range(2)]
                Rts = [sb(f"Rt{i}", [128, 1024]) for i in range(2)]
                KZ, bKZ = sb("KZ", [128, 4, 2, 64]); RQ2, bRQ2 = sb("RQ2", [128, 4, 2, 64])
                QXs = [sb(f"QX{i}", [128, 128]) for i in range(2)]; qkTs = [sb(f"qkT{i}", [64, 256]) for i in range(2)]; SDs = [sb(f"SD{i}", [128, 128]) for i in range(2)]
                s1, bs1 = sb("s1", [128, 4]); s2, bs2 = sb("s2", [128, 4]); msq, bmsq = sb("msq", [128, 4])
                sq, bsq = sb("sq", [128, 256]); gn, bgn = sb("gn", [128, 4, 64]); sg, bsg = sb("sg", [128, 256])
                st4, bst4 = sb("st4", [128, 4, 6]); mv4, bmv4 = sb("mv4", [128, 4, 2])
                ros = [sb(f"ro{i}", [128, 256]) for i in range(2)]
                pKV, bpKV = ps("pKV", [64, 512]); pTR = [ps(f"pTR{i}", [128, 256]) for i in range(2)]
                pST = [ps(f"pST{i}", [128, 128]) for i in range(2)]; pOr, bpOr = ps("pOr", [128, 256])
                op("pool", lambda e: e.memset(RBB[:, 1, :, :], 0.0), writes=[bRB[1]])
                op("pool", lambda e: e.memset(RB[:, 0, :, :], 0.0), writes=[bRB[0]])
                order_b = [1, 0] + list(range(NT - 1, 1, -1))
                it = 0

                def kv_states(Rt, bRt):
                    op("dve", lambda e: e.tensor_tensor(out=KZ[:], in0=Rt[:, 256:512].rearrange("p (h d) -> p h d", h=4).unsqueeze(2).to_broadcast([128, 4, 2, 64]),
                                                        in1=ZETA[:].unsqueeze(3).to_broadcast([128, 4, 2, 64]), op=ALU.mult), reads=[bRt, bZETA], writes=[bKZ])
                    for h in range(4):
                        for a in range(2):
                            op("pe", lambda e: e.matmul(pKV[0:64, (a * 4 + h) * 64:(a * 4 + h + 1) * 64], lhsT=KZ[:, h, a, :], rhs=Rt[:, 512 + h * 64:512 + (h + 1) * 64],
                                                        start=True, stop=True), reads=[bKZ, bRt], writes=[bpKV])

                for idx, ti in enumerate(order_b):
                    Rt, bRt = Rts[it % 2]; it += 1
                    dma("sp", lambda e: e.dma_start(out=Rt[:, 256:768], in_=U[ti * 128:(ti + 1) * 128, 1280:1792]), writes=[bRt])
                    kv_states(Rt, bRt)
                    if idx + 1 < NT:
                        nxt = order_b[idx + 1]
                        for h in range(4):
                            op("dve", lambda e: e.scalar_tensor_tensor(out=RBB[:, nxt, h, :], in0=RBB[:, ti, h, :], scalar=GCB[:, h:h + 1],
                                                                       in1=pKV[0:64, (4 + h) * 64:(5 + h) * 64], op0=ALU.mult, op1=ALU.add),
                               reads=[bRB[ti], bGCB, bpKV], writes=[bRB[nxt]])
                for ti in range(NT):
                    want_out = ((ti >= 2) or need_ctx) and not os.environ.get('RET_SKIP_OUT')
                    Rt, bRt = Rts[it % 2]; ro, bro = ros[it % 2]; it += 1
                    dma("sp", lambda e: e.dma_start(out=Rt[:], in_=U[ti * 128:(ti + 1) * 128, 1024:2048]), writes=[bRt])
                    kv_states(Rt, bRt)
                    if want_out:
                        for h in range(4):
                            TR, bTR = pTR[h % 2]; ST, bST = pST[h % 2]
                            (QX, bQX), (qkT, bqkT), (SD, bSD), (QXB, bQXB), (kTr, bkTr) = QXs[h % 2], qkTs[h % 2], SDs[h % 2], QXBs[h % 2], kTrs[h % 2]
                            op("pe", lambda e: e.transpose(out=TR[0:64, 0:128], in_=Rt[:, h * 64:(h + 1) * 64], identity=ID[:]), reads=[bRt, bID], writes=[bTR])
                            op("pe", lambda e: e.transpose(out=TR[0:64, 128:256], in_=Rt[:, 256 + h * 64:256 + (h + 1) * 64], identity=ID[:]), reads=[bRt, bID], writes=[bTR])
                            op("act", lambda e: e.copy(out=qkT[:], in_=TR[0:64, :]), reads=[bTR], writes=[bqkT])
                            op("act", lambda e: e.copy(out=kTr[:], in_=TR[0:64, 128:256]), reads=[bTR], writes=[bkTr])
                            op("dve", lambda e: e.tensor_tensor(out=QX[0:64, :], in0=qkT[:, 0:128], in1=XI[0:64, h, :], op=ALU.mult), reads=[bqkT, bXI], writes=[bQX])
                            op("dve", lambda e: e.tensor_tensor(out=QXB[:], in0=qkT[:, 0:128], in1=XIB[:, h, :], op=ALU.mult), reads=[bqkT, bXIB], writes=[bQXB])
                            op("pe", lambda e: e.matmul(ST[:], lhsT=kTr[:], rhs=qkT[:, 0:128], start=True, stop=True), reads=[bqkT, bkTr], writes=[bST])
                            op("dve", lambda e: e.tensor_tensor(out=SD[:], in0=ST[:], in1=DS[:, h, :], op=ALU.mult), reads=[bST, bDS], writes=[bSD])
                            op("pe", lambda e: e.matmul(pOr[:, h * 64:(h + 1) * 64], lhsT=SD[:], rhs=Rt[:, 512 + h * 64:512 + (h + 1) * 64], start=True, stop=False), reads=[bSD, bRt], writes=[bpOr])
                            op("pe", lambda e: e.matmul(pOr[:, h * 64:(h + 1) * 64], lhsT=QX[0:64, :], rhs=RB[:, ti, h, :], start=False, stop=False), reads=[bQX, bRB[ti]], writes=[bpOr])
                            op("pe", lambda e: e.matmul(pOr[:, h * 64:(h + 1) * 64], lhsT=QXB[:], rhs=RBB[:, ti, h, :], start=False, stop=True), reads=[bQXB, bRB[ti]], writes=[bpOr])
                    if ti + 1 < NT:
                        for h in range(4):
                            op("dve", lambda e: e.scalar_tensor_tensor(out=RB[:, ti + 1, h, :], in0=RB[:, ti, h, :], scalar=GC[0:64, h:h + 1],
                                                                       in1=pKV[0:64, h * 64:(h + 1) * 64], op0=ALU.mult, op1=ALU.add),
                               reads=[bRB[ti], bGC, bpKV], writes=[bRB[ti + 1]])
                    if want_out and os.environ.get('RET_NO_GN'):
                        op("act", lambda e: e.copy(out=ro[:], in_=pOr[:]), reads=[bpOr], writes=[bro])
                        dma("pool", lambda e: e.dma_start(out=M[ti * 128:(ti + 1) * 128, 768:1024], in_=ro[:]), reads=[bro])
                    elif want_out:
                        op("act", lambda e: e.copy(out=sq[:], in_=pOr[:]), reads=[bpOr], writes=[bsq])
                        for h in range(4):
                            op("dve", lambda e: e.bn_stats(out=st4[:, h, :], in_=sq[:, h * 64:(h + 1) * 64]), reads=[bsq], writes=[bst4])
                        for h in range(4):
                            op("dve", lambda e: e.bn_aggr(out=mv4[:, h, :], in_=st4[:, h, :]), reads=[bst4], writes=[bmv4])
                        for h in range(4):
                            op("act", lambda e: e.activation(out=s2[:, h:h + 1], in_=mv4[:, h, 1:2], func=AF.Sqrt, bias=EPS, scale=1.0), reads=[bmv4], writes=[bs2])
                        op("dve", lambda e: e.reciprocal(out=s1[:], in_=s2[:]), reads=[bs2], writes=[bs1])
                        for h in range(4):
                            op("dve", lambda e: e.tensor_scalar(out=gn[:, h, :], in0=sq[:, h * 64:(h + 1) * 64], scalar1=mv4[:, h, 0:1], scalar2=s1[:, h:h + 1], op0=ALU.subtract, op1=ALU.mult),
                               reads=[bsq, bmv4, bs1], writes=[bgn])
                        op("act", lambda e: e.activation(out=sg[:], in_=Rt[:, 768:1024], func=AF.Silu), reads=[bRt], writes=[bsg])
                        op("dve", lambda e: e.tensor_tensor(out=ro[:], in0=gn[:].rearrange("p h d -> p (h d)"), in1=sg[:], op=ALU.mult), reads=[bgn, bsg], writes=[bro])
                        dma("pool", lambda e: e.dma_start(out=M[ti * 128:(ti + 1) * 128, 768:1024], in_=ro[:]), reads=[bro])
            P.barrier()
            if debug == "p2":
                break
            tiles3 = list(range(NT)) if need_ctx else list(range(2, NT))
            with ExitStack() as es:
                sb, ps = pools(es)
                WO, bWO = load(es, "WO", w_out[l].rearrange("(k p) n -> p k n", p=128), [128, 8, D])
                RW, bRW = load(es, "RW", router_w[l].rearrange("(k p) n -> p k n", p=128), [128, 8, NEXP])
                RBi, bRBi = load(es, "RBi", bc(router_b[l]), [128, NEXP])
                G1 = [modv(es, "G1L", 0, 2), modv(es, "G1C", 1, 2)]
                SC2 = [modv(es, "SC2L", 0, 4), modv(es, "SC2C", 1, 4)]
                SH2 = [modv(es, "SH2L", 0, 3), modv(es, "SH2C", 1, 3)]
                LNG = load(es, "LNG", bc(ln_g[l, 0]), [128, D]); LNB = load(es, "LNB", bc(ln_b[l, 0]), [128, D])
                mts = [sb(f"mt{i}", [128, D]) for i in range(2)]; xts = [sb(f"x3t{i}", [128, D]) for i in range(2)]
                mTs = [sb(f"mT{i}", [128, 8, 128]) for i in range(2)]; t3s = [sb(f"t3{i}", [128, D]) for i in range(2)]; xns = [sb(f"xn{i}", [128, D]) for i in range(2)]
                h2s = [sb(f"h2t{i}", [128, D]) for i in range(2)]; h2Ts = [sb(f"h2T{i}", [128, 8, 128]) for i in range(2)]
                bpYn = [Buf() for _ in range(2)]
                tmp = ln_tmp(es, "3")
                pT, bpT = ps("pT3", [128, 1024]); pY, bpY = ps("pY3", [128, 1024]); pL, bpL = ps("pL", [128, NEXP])
                pT2, bpT2 = ps("pT3b", [128, 1024])

                def front(ii):
                    i = tiles3[ii]
                    ck = 1 if i < 2 else 0
                    Mt, bMt = mts[ii % 2]; Xt, bXt = xts[ii % 2]; h2, bh2 = h2s[ii % 2]
                    (mT, bmT), (t3, bt3), (xn, bxn) = mTs[ii % 2], t3s[ii % 2], xns[ii % 2]
                    dma("sp", lambda e: e.dma_start(out=Mt[:], in_=M[i * 128:(i + 1) * 128, :]), writes=[bMt])
                    dma("sp", lambda e: e.dma_start(out=Xt[:], in_=xsrc[i * 128:(i + 1) * 128, :]), writes=[bXt])
                    transpose8(Mt, bMt, pT, bpT, mT, bmT)
                    for n in range(2):
                        for k in range(8):
                            op("pe", lambda e: e.matmul(pY[:, n * 512:(n + 1) * 512], lhsT=mT[:, k, :], rhs=WO[:, k, n * 512:(n + 1) * 512], start=(k == 0), stop=(k == 7)),
                               reads=[bmT, bWO], writes=[bpYn[n]])
                        op("dve", lambda e: e.tensor_tensor(out=t3[:, n * 512:(n + 1) * 512], in0=pY[:, n * 512:(n + 1) * 512], in1=G1[ck][0][:, n * 512:(n + 1) * 512], op=ALU.mult),
                           reads=[bpYn[n], G1[ck][1]], writes=[bt3])
                    op("dve", lambda e: e.scalar_tensor_tensor(out=t3[:], in0=Xt[:], scalar=ALPHA, in1=t3[:], op0=ALU.mult, op1=ALU.add), reads=[bXt, bt3], writes=[bt3])
                    layer_norm(es, "3", t3, bt3, xn, bxn, tmp)
                    mul_add(xn, bxn, xn, bxn, LNG[0], LNG[1], LNB[0], LNB[1])
                    dma("pool", lambda e: e.dma_start(out=X[i * 128:(i + 1) * 128, :], in_=xn[:]), reads=[bxn])
                    layer_norm(es, "3", xn, bxn, h2, bh2, tmp)
                    mul_add(h2, bh2, h2, bh2, SC2[ck][0], SC2[ck][1], SH2[ck][0], SH2[ck][1])
                    dma("pool", lambda e: e.dma_start(out=H2[i * 128:(i + 1) * 128, :], in_=h2[:]), reads=[bh2])

                def back(ii):
                    i = tiles3[ii]
                    h2, bh2 = h2s[ii % 2]; h2T, bh2T = h2Ts[ii % 2]
                    transpose8(h2, bh2, pT2, bpT2, h2T, bh2T)
                    for k in range(8):
                        op("pe", lambda e: e.matmul(pL[:], lhsT=h2T[:, k, :], rhs=RW[:, k, :], start=(k == 0), stop=(k == 7)), reads=[bh2T, bRW], writes=[bpL])
                    op("dve", lambda e: e.tensor_tensor(out=LOG[:, i, :], in0=pL[:], in1=RBi[:], op=ALU.add), reads=[bpL, bRBi], writes=[bLOG])

                front(0)
                for ii in range(len(tiles3)):
                    if ii + 1 < len(tiles3):
                        front(ii + 1)
                    back(ii)
            P.barrier()
            if debug == "p3":
                break
            tiles4 = tiles3
            with ExitStack() as es:
                sb, ps = pools(es)
                UTt, bUT = load(es, "UTt", cst["UT"], [128, 128]); ONES, bONES = load(es, "ONESt", cst["ONES"], [128, 128])
                JV, bJV = load(es, "JVt", cst["JV"], [128, NBLK]); KP, bKP = load(es, "KPt", cst["KP"], [128, 8])
                SL4i, bSL4i = sb("SL4i", [128, NT, 4], I32); G4, bG4 = sb("G4", [128, NT, 4])
                IDX, bIDX = sb("IDX", [128, NBLK, 8], I32); BEi, bBEi = sb("BEi", [128, NBLK], I32)
                with ExitStack() as es2:
                    sb2, ps2 = pools(es2)
                    MASK, bMASK = sb2("MASK", [128, NT, NEXP]); GT, bGT = sb2("GT", [128, NT, NEXP]); POS, bPOS = sb2("POS", [128, NT, NEXP])
                    BASE, bBASE = sb2("BASE", [128, NEXP]); t8, bt8 = sb2("t8", [128, 8]); ng, bng = sb2("ng", [128, 1]); ss, bss = sb2("ss", [128, 1])
                    ee, bee = sb2("ee", [128, NEXP]); oh, boh = sb2("oh", [128, NEXP])
                    PADD, bPADD = sb2("PADD", [128, NEXP]); PEND, bPEND = sb2("PEND", [128, NEXP]); PST, bPST = sb2("PST", [128, NEXP])
                    SLOT, bSLOT = sb2("SLOT", [128, NEXP]); s4f, bs4f = sb2("s4f", [128, 8])
                    BE, bBE = sb2("BE", [128, NBLK]); IDXf, bIDXf = sb2("IDXf", [128, NBLK, 8]); ONE32, bONE32 = sb2("ONE32", [128, NEXP])
                    pPos, bpPos = ps2("pPos", [128, NEXP]); pCS, bpCS = ps2("pCS", [128, NEXP])
                    op("pool", lambda e: e.memset(BASE[:], 0.0), writes=[bBASE])
                    op("pool", lambda e: e.memset(ONE32[:], 1.0), writes=[bONE32])
                    for i in tiles4:
                        lg = LOG[:, i, :]
                        op("dve", lambda e: e.max(out=t8[:], in_=lg), reads=[bLOG], writes=[bt8])
                        op("dve", lambda e: e.tensor_scalar(out=MASK[:, i, :], in0=lg, scalar1=t8[:, 3:4], scalar2=None, op0=ALU.is_ge), reads=[bLOG, bt8], writes=[bMASK])
                        op("dve", lambda e: e.tensor_scalar(out=ng[:], in0=t8[:, 0:1], scalar1=-1.0, scalar2=None, op0=ALU.mult), reads=[bt8], writes=[bng])
                        op("act", lambda e: e.activation(out=ee[:], in_=lg, func=AF.Exp, bias=ng[:, 0:1], scale=1.0), reads=[bLOG, bng], writes=[bee])
                        op("dve", lambda e: e.tensor_tensor(out=ee[:], in0=ee[:], in1=MASK[:, i, :], op=ALU.mult), reads=[bee, bMASK], writes=[bee])
                        op("dve", lambda e: e.reduce_sum(out=ss[:], in_=ee[:], axis=AX.X), reads=[bee], writes=[bss])
                        op("dve", lambda e: e.reciprocal(out=ss[:], in_=ss[:]), reads=[bss], writes=[bss])
                        op("dve", lambda e: e.tensor_scalar(out=GT[:, i, :], in0=ee[:], scalar1=ss[:, 0:1], scalar2=None, op0=ALU.mult), reads=[bee, bss], writes=[bGT])
                        op("pe", lambda e: e.matmul(pPos[:], lhsT=UTt[:], rhs=MASK[:, i, :], start=True, stop=True), reads=[bUT, bMASK], writes=[bpPos])
                        op("pe", lambda e: e.matmul(pCS[:], lhsT=ONES[:], rhs=MASK[:, i, :], start=True, stop=True), reads=[bONES, bMASK], writes=[bpCS])
                        op("dve", lambda e: e.tensor_tensor(out=POS[:, i, :], in0=pPos[:], in1=BASE[:], op=ALU.add), reads=[bpPos, bBASE], writes=[bPOS])
                        op("dve", lambda e: e.tensor_tensor(out=BASE[:], in0=pCS[:], in1=BASE[:], op=ALU.add), reads=[bpCS, bBASE], writes=[bBASE])
                    PADi, bPADi = sb2("PADi", [128, NEXP], I32)
                    op("dve", lambda e: e.tensor_scalar(out=PADD[:], in0=BASE[:], scalar1=127.0, scalar2=None, op0=ALU.add), reads=[bBASE], writes=[bPADD])
                    op("dve", lambda e: e.tensor_copy(out=PADi[:], in_=PADD[:]), reads=[bPADD], writes=[bPADi])
                    op("dve", lambda e: e.tensor_single_scalar(out=PADi[:], in_=PADi[:], scalar=7, op=ALU.arith_shift_right), reads=[bPADi], writes=[bPADi])
                    op("dve", lambda e: e.tensor_single_scalar(out=PADi[:], in_=PADi[:], scalar=7, op=ALU.logical_shift_left), reads=[bPADi], writes=[bPADi])
                    op("dve", lambda e: e.tensor_copy(out=PADD[:], in_=PADi[:]), reads=[bPADi], writes=[bPADD])
                    op("dve", lambda e: e.tensor_tensor_scan(out=PEND[:], data0=ONE32[:], data1=PADD[:], initial=0.0, op0=ALU.mult, op1=ALU.add), reads=[bONE32, bPADD], writes=[bPEND])
                    op("dve", lambda e: e.tensor_tensor(out=PST[:], in0=PEND[:], in1=PADD[:], op=ALU.subtract), reads=[bPEND, bPADD], writes=[bPST])
                    op("dve", lambda e: e.tensor_scalar(out=PST[:], in0=PST[:], scalar1=1.0, scalar2=None, op0=ALU.add), reads=[bPST], writes=[bPST])
                    for i in tiles4:
                        op("dve", lambda e: e.tensor_tensor(out=SLOT[:], in0=POS[:, i, :], in1=PST[:], op=ALU.add), reads=[bPOS, bPST], writes=[bSLOT])
                        op("dve", lambda e: e.tensor_tensor(out=SLOT[:], in0=SLOT[:], in1=MASK[:, i, :], op=ALU.mult), reads=[bSLOT, bMASK], writes=[bSLOT])
                        op("dve", lambda e: e.tensor_scalar(out=SLOT[:], in0=SLOT[:], scalar1=-1.0, scalar2=None, op0=ALU.add), reads=[bSLOT], writes=[bSLOT])
                        op("dve", lambda e: e.max(out=s4f[:], in_=SLOT[:]), reads=[bSLOT], writes=[bs4f])
                        op("dve", lambda e: e.tensor_copy(out=SL4i[:, i, :], in_=s4f[:, 0:4]), reads=[bs4f], writes=[bSL4i])
                        for k in range(4):
                            op("dve", lambda e: e.tensor_scalar(out=oh[:], in0=SLOT[:], scalar1=s4f[:, k:k + 1], scalar2=None, op0=ALU.is_equal), reads=[bSLOT, bs4f], writes=[boh])
                            op("dve", lambda e: e.tensor_tensor(out=oh[:], in0=oh[:], in1=GT[:, i, :], op=ALU.mult), reads=[boh, bGT], writes=[boh])
                            op("dve", lambda e: e.reduce_sum(out=G4[:, i, k:k + 1], in_=oh[:], axis=AX.X), reads=[boh], writes=[bG4])
                    op("pool", lambda e: e.memset(BE[:], 0.0), writes=[bBE])
                    for ex in range(NEXP):
                        op("dve", lambda e: e.scalar_tensor_tensor(out=BE[:], in0=JV[:], scalar=PEND[:, ex:ex + 1], in1=BE[:], op0=ALU.is_ge, op1=ALU.add), reads=[bJV, bPEND, bBE], writes=[bBE])
                    op("dve", lambda e: e.tensor_scalar(out=BE[:], in0=BE[:], scalar1=float(NEXP - 1), scalar2=None, op0=ALU.min), reads=[bBE], writes=[bBE])
                    BE2, bBE2 = sb2("BE2", [128, NBLK]); SAME, bSAME = sb2("SAME", [128, NBLK])
                    op("dve", lambda e: e.tensor_copy(out=BE2[:], in_=BE[:]), reads=[bBE], writes=[bBE2])
                    op("pool", lambda e: e.memset(SAME[:], 0.0), writes=[bSAME])
                    op("dve", lambda e: e.tensor_tensor(out=SAME[:, 1:NBLK], in0=BE[:, 1:NBLK], in1=BE2[:, 0:NBLK - 1], op=ALU.is_equal), reads=[bBE, bBE2, bSAME], writes=[bSAME])
                    op("dve", lambda e: e.tensor_scalar(out=SAME[:], in0=SAME[:], scalar1=100000.0, scalar2=None, op0=ALU.mult), reads=[bSAME], writes=[bSAME])
                    op("dve", lambda e: e.tensor_tensor(out=BE2[:], in0=BE2[:], in1=SAME[:], op=ALU.add), reads=[bBE2, bSAME], writes=[bBE2])
                    op("dve", lambda e: e.tensor_copy(out=BEi[:], in_=BE2[:]), reads=[bBE2], writes=[bBEi])
                    op("dve", lambda e: e.scalar_tensor_tensor(out=BE[:], in0=BE[:], scalar=1024.0, in1=SAME[:], op0=ALU.mult, op1=ALU.add), reads=[bBE, bSAME], writes=[bBE])
                    op("dve", lambda e: e.tensor_tensor(out=IDXf[:], in0=BE[:].unsqueeze(2).to_broadcast([128, NBLK, 8]), in1=KP[:].unsqueeze(1).to_broadcast([128, NBLK, 8]), op=ALU.add),
                       reads=[bBE, bKP], writes=[bIDXf])
                    op("dve", lambda e: e.tensor_copy(out=IDX[:], in_=IDXf[:]), reads=[bIDXf], writes=[bIDX])
                P.barrier(new_epoch=False)
                if debug == "p4a":
                    dma("sp", lambda e: e.dma_start(out=DBG[:, 0:NT * 4], in_=G4[:].rearrange("p a b -> p (a b)")), reads=[bG4])
                    dma("sp", lambda e: e.dma_start(out=DBGI[:, 0:NT * 4], in_=SL4i[:].rearrange("p a b -> p (a b)")), reads=[bSL4i])
                    dma("sp", lambda e: e.dma_start(out=DBGI[:, NT * 4:NT * 4 + NBLK], in_=BEi[:]), reads=[bBEi])
                    P.barrier(new_epoch=False)
                    break
                with ExitStack() as es2:
                    sb2, ps2 = pools(es2)
                    hts = [sb2(f"h4t{i}", [128, D]) for i in range(3)]
                    for ii, i in enumerate(tiles4):
                        Ht, bHt = hts[ii % 3]
                        dma("sp", lambda e: e.dma_start(out=Ht[:], in_=H2[i * 128:(i + 1) * 128, :]), writes=[bHt])
                        for k in range(4):
                            dma("pool", lambda e: e.indirect_dma_start(out=XS[:, :], out_offset=bass.IndirectOffsetOnAxis(ap=SL4i[:, i, k:k + 1], axis=0), in_=Ht[:], in_offset=None), reads=[bHt, bSL4i])
                P.barrier(new_epoch=False)
                if debug == "p4b":
                    break
                with ExitStack() as es2:
                    sb2, ps2 = pools(es2)
                    W1, _ = sb2("W1", [128, 8, 2048]); W2, _ = sb2("W2", [128, 8, D])
                    bW1 = [Buf() for _ in range(8)]; bW2 = [Buf() for _ in range(8)]
                    B1, bB1 = sb2("B1", [128, 2048]); B2, bB2 = sb2("B2", [128, D])
                    xss = [sb2(f"xs{i}", [128, D]) for i in range(2)]; xTs = [sb2(f"xT4_{i}", [128, 8, 128]) for i in range(2)]
                    ubs = [sb2(f"ub{i}", [128, 1024]) for i in range(2)]; glus = [sb2(f"glu{i}", [128, 512]) for i in range(2)]
                    lins = [sb2(f"lin{i}", [128, 512]) for i in range(2)]; sigs = [sb2(f"sig{i}", [128, 512]) for i in range(2)]
                    aTs = [sb2(f"aT4_{i}", [128, 4, 128]) for i in range(2)]; ys = [sb2(f"ys{i}", [128, D]) for i in range(2)]
                    pT, bpT = ps2("pT4", [128, 1024]); pUs = [ps2(f"pU4_{i}", [128, 1024]) for i in range(2)]; pY, bpY = ps2("pY4", [128, 1024])
                    w1v = exp_w1.rearrange("l e k n -> (l e k) n"); w2v = exp_w2.rearrange("l e k n -> (l e k) n")
                    b1v = exp_b1.rearrange("l e n -> (l e) n"); b2v = exp_b2.rearrange("l e n -> (l e) n")
                    regW = nc.gpsimd.to_reg(NEXP * D - 1); regB = nc.gpsimd.to_reg(NEXP - 1)

                    def gW1(j):
                        for k in range(8):
                            dma("pool", lambda e: e.indirect_dma_start(out=W1[:, k, :], out_offset=None, in_=w1v, in_offset=bass.IndirectOffsetOnAxis(ap=IDX[:, j, k:k + 1], axis=0),
                                                                       element_offset=l * NEXP * D * 2048, bounds_check=regW, oob_is_err=False), reads=[bIDX], writes=[bW1[k]])

                    def gB1(j):
                        dma("pool", lambda e: e.indirect_dma_start(out=B1[:], out_offset=None, in_=b1v, in_offset=bass.IndirectOffsetOnAxis(ap=BEi[:, j:j + 1], axis=0),
                                                                   element_offset=l * NEXP * 2048, bounds_check=regB, oob_is_err=False), reads=[bBEi], writes=[bB1])

                    def gW2(j):
                        for k in range(8):
                            dma("pool", lambda e: e.indirect_dma_start(out=W2[:, k, :], out_offset=None, in_=w2v, in_offset=bass.IndirectOffsetOnAxis(ap=IDX[:, j, k:k + 1], axis=0),
                                                                       element_offset=l * NEXP * D * D, bounds_check=regW, oob_is_err=False), reads=[bIDX], writes=[bW2[k]])
                        dma("pool", lambda e: e.indirect_dma_start(out=B2[:], out_offset=None, in_=b2v, in_offset=bass.IndirectOffsetOnAxis(ap=BEi[:, j:j + 1], axis=0),
                                                                   element_offset=l * NEXP * D, bounds_check=regB, oob_is_err=False), reads=[bBEi], writes=[bB2])

                    def ldx(j):
                        Xs, bXs = xss[j % 2]
                        dma("sp", lambda e: e.dma_start(out=Xs[:], in_=XS[j * 128:(j + 1) * 128, :]), writes=[bXs])

                    def tx(j):
                        Xs, bXs = xss[j % 2]; xT, bxT = xTs[j % 2]
                        transpose8(Xs, bXs, pT, bpT, xT, bxT)

                    def swiglu(hf):
                        pU, bpU = pUs[hf]; ub, bub = ubs[hf]; glu, bglu = glus[hf]; lin, blin = lins[hf]; sig, bsig = sigs[hf]
                        op("dve", lambda e: e.tensor_tensor(out=ub[:], in0=pU[:], in1=B1[:, hf * 1024:(hf + 1) * 1024], op=ALU.add), reads=[bpU, bB1], writes=[bub])
                        ubv = ub[:].rearrange("p (f t) -> p f t", t=2)
                        op("dve", lambda e: e.tensor_scalar(out=glu[:], in0=ubv[:, :, 0], scalar1=7.0, scalar2=None, op0=ALU.min), reads=[bub], writes=[bglu])
                        op("dve", lambda e: e.tensor_scalar(out=lin[:], in0=ubv[:, :, 1], scalar1=7.0, scalar2=-7.0, op0=ALU.min, op1=ALU.max), reads=[bub], writes=[blin])
                        op("act", lambda e: e.activation(out=sig[:], in_=glu[:], func=AF.Sigmoid, scale=1.702), reads=[bglu], writes=[bsig])
                        op("dve", lambda e: e.tensor_tensor(out=glu[:], in0=glu[:], in1=sig[:], op=ALU.mult), reads=[bglu, bsig], writes=[bglu])
                        op("dve", lambda e: e.scalar_tensor_tensor(out=lin[:], in0=lin[:], scalar=1.0, in1=glu[:], op0=ALU.add, op1=ALU.mult), reads=[blin, bglu], writes=[blin])

                    def ta(hf):
                        lin, blin = lins[hf]; aT, baT = aTs[hf]
                        for k in range(4):
                            op("pe", lambda e: e.transpose(out=pT[:, k * 128:(k + 1) * 128], in_=lin[:, k * 128:(k + 1) * 128], identity=ID[:]), reads=[blin, bID], writes=[bpT])
                        op("act", lambda e: e.copy(out=aT[:].rearrange("p a b -> p (a b)"), in_=pT[:, 0:512]), reads=[bpT], writes=[baT])

                    NB = len(tiles4) * 4 + NEXP
                    gW1(0); gB1(0); gW2(0); ldx(0)
                    if NB > 1:
                        ldx(1)
                    tx(0)
                    for j in range(NB):
                        xT, bxT = xTs[j % 2]; Ys, bYs = ys[j % 2]
                        for hf in range(2):
                            pU, bpU = pUs[hf]
                            for n in range(2):
                                for k in range(8):
                                    op("pe", lambda e: e.matmul(pU[:, n * 512:(n + 1) * 512], lhsT=xT[:, k, :], rhs=W1[:, k, (hf * 2 + n) * 512:(hf * 2 + n + 1) * 512],
                                                                start=(k == 0), stop=(k == 7)), reads=[bxT, bW1[k]], writes=[bpU])
                        if j + 1 < NB:
                            gW1(j + 1)
                            tx(j + 1)
                        if j + 2 < NB:
                            ldx(j + 2)
                        swiglu(0)
                        ta(0)
                        for n in range(2):
                            for k in range(4):
                                op("pe", lambda e: e.matmul(pY[:, n * 512:(n + 1) * 512], lhsT=aTs[0][0][:, k, :], rhs=W2[:, k, n * 512:(n + 1) * 512], start=(k == 0), stop=False),
                                   reads=[aTs[0][1], bW2[k]], writes=[bpY])
                        swiglu(1)
                        if j + 1 < NB:
                            gB1(j + 1)
                        ta(1)
                        for n in range(2):
                            for k in range(4):
                                op("pe", lambda e: e.matmul(pY[:, n * 512:(n + 1) * 512], lhsT=aTs[1][0][:, k, :], rhs=W2[:, 4 + k, n * 512:(n + 1) * 512], start=False, stop=(k == 3)),
                                   reads=[aTs[1][1], bW2[4 + k]], writes=[bpY])
                        op("dve", lambda e: e.tensor_tensor(out=Ys[:], in0=pY[:], in1=B2[:], op=ALU.add), reads=[bpY, bB2], writes=[bYs])
                        if j + 1 < NB:
                            gW2(j + 1)
                        dma("sp", lambda e: e.dma_start(out=YS[j * 128:(j + 1) * 128, :], in_=Ys[:]), reads=[bYs])
                P.barrier(new_epoch=False)
                if debug == "p4c":
                    dma("sp", lambda e: e.dma_start(out=DBGI[:, 0:NT * 4], in_=SL4i[:].rearrange("p a b -> p (a b)")), reads=[bSL4i])
                    P.barrier(new_epoch=False)
                    break
                with ExitStack() as es2:
                    sb2, ps2 = pools(es2)
                    G2 = [modv(es2, "G2L", 0, 5), modv(es2, "G2C", 1, 5)]
                    LNG = load(es2, "LNG2", bc(ln_g[l, 1]), [128, D]); LNB = load(es2, "LNB2", bc(ln_b[l, 1]), [128, D])
                    ygs = [[sb2(f"yg{i}_{k}", [128, D]) for k in range(4)] for i in range(2)]
                    xts = [sb2(f"x5t{i}", [128, D]) for i in range(2)]
                    f, bf = sb2("f5", [128, D]); xo = [sb2(f"xo{i}", [128, D]) for i in range(2)]
                    tmp = ln_tmp(es2, "5")
                    for ii, i in enumerate(tiles4):
                        ck = 1 if i < 2 else 0
                        YG = ygs[ii % 2]; Xt, bXt = xts[ii % 2]; Xo, bXo = xo[ii % 2]
                        for k in range(4):
                            dma("pool", lambda e: e.indirect_dma_start(out=YG[k][0][:], out_offset=None, in_=YS[:, :], in_offset=bass.IndirectOffsetOnAxis(ap=SL4i[:, i, k:k + 1], axis=0)),
                                reads=[bSL4i], writes=[YG[k][1]])
                        dma("sp", lambda e: e.dma_start(out=Xt[:], in_=X[i * 128:(i + 1) * 128, :]), writes=[bXt])
                        op("dve", lambda e: e.tensor_scalar(out=f[:], in0=YG[0][0][:], scalar1=G4[:, i, 0:1], scalar2=None, op0=ALU.mult), reads=[YG[0][1], bG4], writes=[bf])
                        for k in range(1, 4):
                            op("dve", lambda e: e.scalar_tensor_tensor(out=f[:], in0=YG[k][0][:], scalar=G4[:, i, k:k + 1], in1=f[:], op0=ALU.mult, op1=ALU.add), reads=[YG[k][1], bG4, bf], writes=[bf])
                        op("dve", lambda e: e.tensor_tensor(out=f[:], in0=f[:], in1=G2[ck][0][:], op=ALU.mult), reads=[bf, G2[ck][1]], writes=[bf])
                        op("dve", lambda e: e.scalar_tensor_tensor(out=f[:], in0=Xt[:], scalar=ALPHA, in1=f[:], op0=ALU.mult, op1=ALU.add), reads=[bXt, bf], writes=[bf])
                        layer_norm(es2, "5", f, bf, Xo, bXo, tmp)
                        mul_add(Xo, bXo, Xo, bXo, LNG[0], LNG[1], LNB[0], LNB[1])
                        if l == 1:
                            dma("sp", lambda e: e.dma_start(out=out[(i - 2) * 128:(i - 1) * 128, :], in_=Xo[:]), reads=[bXo])
                        else:
                            dma("sp", lambda e: e.dma_start(out=X[i * 128:(i + 1) * 128, :], in_=Xo[:]), reads=[bXo])
            P.barrier()
        P.finish()
    return nc


def make_in_map(inp, b, N):
    f = lambda a: np.ascontiguousarray(np.asarray(a, dtype=np.float32))
    m = {}
    m["xin"] = f(np.concatenate([inp["ctx"][b], inp["x"][b]], 0))
    cc = np.stack([np.asarray(inp["c"][b]).reshape(8, 128).T, np.asarray(inp["c_ctx"]).reshape(8, 128).T], -1)
    m["cc"] = f(cc)
    for k in ("w_mod", "b_mod", "w_in", "pool_w", "pool_scale", "attn_sink", "ret_decay", "w_out", "ln_g", "ln_b",
              "router_w", "router_b", "exp_w1", "exp_b1", "exp_w2", "exp_b2"):
        m[k] = f(inp[k])
    hc = host_consts(N)
    NT = N // 128 + 2
    NBLK = (NT * 128 * 4) // 128 + NEXP
    hc["JV"] = (np.arange(NBLK, dtype=np.float32) * 128)[None, :].repeat(128, 0)
    for k, v in hc.items():
        m["c_" + k] = f(v)
    return m


_CACHE = {}


def kernel(**inputs):
    inp = {k: np.asarray(v) for k, v in inputs.items()}
    B, N, _ = inp["x"].shape
    if N not in _CACHE:
        _CACHE[N] = build_program(N)
    nc = _CACHE[N]
    n_cores = 8
    maps = [make_in_map(inp, b, N) for b in range(B)]
    in_maps = [maps[i % B] for i in range(n_cores)]
    res = run_bass_kernel_spmd(nc, in_maps, core_ids=list(range(n_cores)))
    return np.stack([np.asarray(res.results[b]["out"], dtype=np.float32) for b in range(B)], 0)
```

```python
import numpy as np
import os
from contextlib import ExitStack
import concourse.bass as bass
import concourse.mybir as mybir
from concourse.bass_utils import run_bass_kernel_spmd

F32 = mybir.dt.float32
I32 = mybir.dt.int32
AF = mybir.ActivationFunctionType
ALU = mybir.AluOpType
AX = mybir.AxisListType


class Buf:
    __slots__ = ("w", "r")

    def __init__(self):
        self.w = None
        self.r = {}


class Prog:
    ENGS = ("pe", "act", "pool", "dve", "sp")

    def __init__(self, nc, es, ring=12):
        self.nc = nc
        self.es = es
        self.eng = {"pe": nc.tensor, "act": nc.scalar, "pool": nc.gpsimd, "dve": nc.vector, "sp": nc.sync}
        self.sem = {}
        self.epoch = 0
        self.nsem = 0
        self._new_epoch_sems()
        self.ring = {}
        self.rr = {}
        for q in ("sp", "pool", "act"):
            self.ring[q] = []
            for i in range(ring):
                key = ("dma", q, i)
                self.sem[key] = self._mk(f"d_{q}_{i}")
                self.ring[q].append([key, 0])
            self.rr[q] = 0

    def _mk(self, name):
        self.nsem += 1
        return self.es.enter_context(self.nc.semaphore(name))

    def _new_epoch_sems(self):
        self.cnt = {}
        self.waited = {e: {} for e in self.ENGS}
        for e in self.ENGS:
            key = ("eng", e, self.epoch)
            self.sem[key] = self._mk(f"e_{e}_{self.epoch}")
            self.cnt[e] = 0

    def ekey(self, e):
        return ("eng", e, self.epoch)

    def _deps(self, reads, writes):
        deps = []
        for b in reads:
            if b.w is not None:
                deps.append(b.w)
        for b in writes:
            if b.w is not None:
                deps.append(b.w)
            deps.extend(b.r.values())
        return deps

    def _waits(self, e, deps):
        for (s, v) in deps:
            if s[0] == "eng":
                if s[2] != self.epoch:
                    continue
                if s[1] == "pe" and e == "pe":
                    continue
            if self.waited[e].get(s, 0) >= v:
                continue
            self.waited[e][s] = v
            self.eng[e].wait_ge(self.sem[s], v)

    def _mark(self, tok, reads, writes):
        for b in reads:
            b.r[tok[0]] = tok
        for b in writes:
            b.w = tok
            b.r = {}

    def op(self, e, fn, reads=(), writes=()):
        self._waits(e, self._deps(reads, writes))
        self.cnt[e] += 1
        k = self.ekey(e)
        tok = (k, self.cnt[e])
        fn(self.eng[e]).then_inc(self.sem[k], 1)
        self._mark(tok, reads, writes)
        return tok

    def dma(self, q, fn, reads=(), writes=(), serial=False):
        ring = self.ring[q]
        i = self.rr[q]
        self.rr[q] = (i + 1) % len(ring)
        key, n = ring[i]
        deps = self._deps(reads, writes)
        if serial and getattr(self, "last_serial", None) is not None:
            deps.append(self.last_serial)
        if n > 0:
            deps.append((key, 16 * n))
        self._waits(q, deps)
        ring[i][1] = n + 1
        tok = (key, 16 * (n + 1))
        fn(self.eng[q]).then_inc(self.sem[key], 16)
        self._mark(tok, reads, writes)
        if serial:
            self.last_serial = tok
        return tok

    def barrier(self, new_epoch=True):
        toks = [(self.ekey(e), self.cnt[e]) for e in self.ENGS if self.cnt[e] > 0]
        for q in self.ring:
            for key, n in self.ring[q]:
                if n > 0:
                    toks.append((key, 16 * n))
        for e in self.ENGS:
            saved = dict(self.waited[e])
            for (s, v) in toks:
                if s[0] == "eng" and s[1] == e:
                    pass
                if self.waited[e].get(s, 0) >= v:
                    continue
                self.waited[e][s] = v
                self.eng[e].wait_ge(self.sem[s], v)
        if new_epoch:
            self.epoch += 1
            dma_waited = {e: {s: v for s, v in self.waited[e].items() if s[0] == "dma"} for e in self.ENGS}
            self._new_epoch_sems()
            self.waited = dma_waited

    def finish(self):
        self.barrier(new_epoch=False)


D = 1024
CTX = 256
ALPHA = 4 ** 0.25
EPS = 1e-6
NEG = -1e30
NEXP = 32


def host_consts(N):
    f64 = np.float64
    c = {}
    c["ident"] = np.eye(128, dtype=np.float32)
    cc = np.arange(64)
    ang = 2 * np.pi * np.outer(cc, cc) / 64
    BC = np.zeros((128, 128), f64); BS = np.zeros((128, 128), f64)
    for g in range(2):
        BC[g * 64:(g + 1) * 64, g * 64:(g + 1) * 64] = np.cos(ang)
        BS[g * 64:(g + 1) * 64, g * 64:(g + 1) * 64] = np.sin(ang)
    c["BC"] = BC.astype(np.float32); c["BS"] = BS.astype(np.float32)
    for nm, Nn in (("L", N), ("C", CTX)):
        N1 = Nn // 128
        a1 = 2 * np.pi * np.outer(np.arange(N1), np.arange(N1)) / N1
        C1, S1 = np.cos(a1), np.sin(a1)
        c["FAP" + nm] = np.concatenate([C1, S1], 1).astype(np.float32)
        c["FAQ" + nm] = np.concatenate([-S1, C1], 1).astype(np.float32)
        at = 2 * np.pi * np.outer(np.arange(128), np.arange(N1)) / Nn
        c["TWC" + nm] = np.cos(at).astype(np.float32)
        c["TWS" + nm] = np.sin(at).astype(np.float32)
        a2 = 2 * np.pi * np.outer(np.arange(128), np.arange(128)) / 128
        sc = 1.0 / np.sqrt(Nn * 64.0)
        c["FBC" + nm] = (np.cos(a2) * sc).astype(np.float32)
        c["FBS" + nm] = (-np.sin(a2) * sc).astype(np.float32)
    PB = np.zeros((128, 20, 128), f64)
    for g, w in enumerate((2, 4, 8, 16)):
        h = w // 2
        for t in range(128):
            for s in range(t - h, t + h):
                if 0 <= s < 128:
                    PB[s, g * 5 + 0, t] += 1.0 / w
                    cnt_first = min(t + h, 10 ** 9) - max(t - h, 0)
                    PB[s, g * 5 + 1, t] += 1.0 / cnt_first
                    cnt_last = min(t + h, 128) - (t - h)
                    PB[s, g * 5 + 2, t] += 1.0 / cnt_last
                elif s < 0:
                    PB[s + 128, g * 5 + 3, t] += 1.0 / w
                else:
                    PB[s - 128, g * 5 + 4, t] += 1.0 / w
            for k in range(3):
                PB[t, g * 5 + k, t] -= 1.0
    c["PB"] = PB.astype(np.float32)
    inv = 1.0 / (10000.0 ** (np.arange(0, 32, 2, dtype=np.float32) / np.float32(32)))
    t = np.arange(N)
    row = (t // 64).astype(np.float32); col = (t % 64).astype(np.float32)
    angr = (row[:, None] * inv[None, :]).astype(np.float32)
    angc = (col[:, None] * inv[None, :]).astype(np.float32)
    c["ROPC"] = np.concatenate([np.cos(angr.astype(f64)), np.cos(angc.astype(f64))], 1).astype(np.float32)
    c["ROPS"] = np.concatenate([np.sin(angr.astype(f64)), np.sin(angc.astype(f64))], 1).astype(np.float32)
    r = np.arange(128)
    c["MASKP"] = np.where(r[None, :] >= r[:, None], 0.0, NEG).astype(np.float32)
    c["MASKN"] = np.where(r[None, :] <= r[:, None], 0.0, NEG).astype(np.float32)
    posz = np.zeros((128, 4, 2), np.float32)
    posz[:, :, 0] = (127 - r)[:, None]; posz[:, :, 1] = r[:, None]
    c["POSZ"] = posz
    posxi = np.zeros((128, 128), np.float32)
    posxi[0:64, :] = (r + 1)[None, :]; posxi[64:128, :] = (128 - r)[None, :]
    c["POSXI"] = posxi
    dq = r[None, :] - r[:, None]
    c["PF"] = np.maximum(dq, 0).astype(np.float32); c["MF"] = (dq >= 0).astype(np.float32)
    c["PBK"] = np.maximum(-dq, 0).astype(np.float32); c["MB"] = (dq <= 0).astype(np.float32)
    c["UT"] = (r[:, None] < r[None, :]).astype(np.float32)
    c["ONES"] = np.ones((128, 128), np.float32)
    kp = (np.arange(8)[None, :] * 128 + r[:, None]).astype(np.float32)
    c["KP"] = kp
    return c


def build_program(N, debug=False):
    NL = N // 128
    NT = NL + 2
    T = NT * 128
    NBLK = (T * 4) // 128 + NEXP
    PR = NBLK * 128
    nc = bass.Bass("TRN2", target_bir_lowering=False)
    dr = lambda name, shape, dt=F32, kind="ExternalInput": nc.dram_tensor(name, shape, dt, kind=kind).ap()
    xin = dr("xin", [T, D])
    cc_in = dr("cc", [128, 8, 2])
    w_mod = dr("w_mod", [2, D, 6 * D]); b_mod = dr("b_mod", [2, 6 * D])
    w_in = dr("w_in", [2, D, 2048]); pool_w = dr("pool_w", [2, 4, 64, 64]); pool_scale = dr("pool_scale", [2, 256])
    attn_sink = dr("attn_sink", [2, 4]); ret_decay = dr("ret_decay", [2, 2, 4]); w_out = dr("w_out", [2, D, D])
    ln_g = dr("ln_g", [2, 2, D]); ln_b = dr("ln_b", [2, 2, D])
    router_w = dr("router_w", [2, D, NEXP]); router_b = dr("router_b", [2, NEXP])
    exp_w1 = dr("exp_w1", [2, NEXP, D, 2048]); exp_b1 = dr("exp_b1", [2, NEXP, 2048])
    exp_w2 = dr("exp_w2", [2, NEXP, D, D]); exp_b2 = dr("exp_b2", [2, NEXP, D])
    hc = host_consts(N)
    jv = (np.arange(NBLK, dtype=np.float32) * 128)[None, :].repeat(128, 0)
    hc["JV"] = jv
    cst = {k: dr("c_" + k, list(v.shape)) for k, v in hc.items()}
    out = dr("out", [N, D], kind="ExternalOutput")
    ik = "ExternalOutput" if debug else "Internal"
    X = dr("X", [T, D], kind=ik)
    U = dr("U", [T, 2048], kind=ik)
    PTD = dr("PTD", [256, T], kind=ik); QTD = dr("QTD", [256, T], kind=ik)
    M = dr("M", [T, D], kind=ik)
    H2 = dr("H2", [T, D], kind=ik)
    MODS = dr("MODS", [2, 6 * D], kind=ik)
    XS = dr("XS", [PR, D], kind=ik); YS = dr("YS", [PR, D], kind=ik)
    DBG = dr("DBG", [128, 4096], kind=ik); DBGI = dr("DBGI", [128, 4096], I32, kind=ik)

    with ExitStack() as es0:
        P = Prog(nc, es0)
        op, dma = P.op, P.dma

        uid = [0]

        def pools(es):
            def sb(name, shape, dt=F32):
                uid[0] += 1
                return (es.enter_context(nc.sbuf_tensor(f"{name}_{uid[0]}", shape, dt)), Buf())

            def ps(name, shape, dt=F32):
                uid[0] += 1
                free = ((shape[1] + 511) // 512) * 512
                t = es.enter_context(nc.psum_tensor(f"{name}_{uid[0]}", [shape[0], free], dt))
                return (t[:, 0:shape[1]], Buf())
            return sb, ps

        def load(es, name, src, shape, q="sp"):
            t, b = pools(es)[0](name, shape)
            dma(q, lambda e: e.dma_start(out=t[:], in_=src), writes=[b])
            return t, b

        def bc(v):
            return v.partition_broadcast(128)

        sb0, _ = pools(es0)
        ID, bID = load(es0, "ID", cst["ident"], [128, 128])
        LOG, bLOG = sb0("LOG", [128, NT, NEXP])

        def layer_norm(es, tag, src, bsrc, dst, bdst, tmp):
            st, bst = tmp["st"]; mv, bmv = tmp["mv"]; rs, brs = tmp["rs"]
            for j in range(2):
                op("dve", lambda e: e.bn_stats(out=st[:, j, :], in_=src[:, j * 512:(j + 1) * 512]), reads=[bsrc], writes=[bst])
            op("dve", lambda e: e.bn_aggr(out=mv[:], in_=st[:].rearrange("p a b -> p (a b)")), reads=[bst], writes=[bmv])
            op("act", lambda e: e.activation(out=rs[:], in_=mv[:, 1:2], func=AF.Sqrt, bias=EPS, scale=1.0), reads=[bmv], writes=[brs])
            op("dve", lambda e: e.reciprocal(out=rs[:], in_=rs[:]), reads=[brs], writes=[brs])
            op("dve", lambda e: e.tensor_scalar(out=dst[:], in0=src[:], scalar1=mv[:, 0:1], scalar2=rs[:, 0:1], op0=ALU.subtract, op1=ALU.mult),
               reads=[bsrc, bmv, brs], writes=[bdst])

        def ln_tmp(es, tag):
            sb, _ = pools(es)
            return {"st": sb("st" + tag, [128, 2, 6]), "mv": sb("mv" + tag, [128, 2]), "rs": sb("rs" + tag, [128, 1])}

        def transpose8(src, bsrc, pT, bpT, dstT, bdstT, eng="act"):
            for k in range(8):
                op("pe", lambda e: e.transpose(out=pT[:, k * 128:(k + 1) * 128], in_=src[:, k * 128:(k + 1) * 128], identity=ID[:]),
                   reads=[bsrc, bID], writes=[bpT])
            if eng == "act":
                op("act", lambda e: e.copy(out=dstT[:].rearrange("p a b -> p (a b)"), in_=pT[:]), reads=[bpT], writes=[bdstT])
            else:
                op("dve", lambda e: e.tensor_copy(out=dstT[:].rearrange("p a b -> p (a b)"), in_=pT[:]), reads=[bpT], writes=[bdstT])

        def mul_add(dst, bdst, src, bsrc, A, bA, B, bB):
            op("dve", lambda e: e.tensor_tensor(out=dst[:], in0=src[:], in1=A[:], op=ALU.mult), reads=[bsrc, bA], writes=[bdst])
            op("dve", lambda e: e.tensor_tensor(out=dst[:], in0=dst[:], in1=B[:], op=ALU.add), reads=[bdst, bB], writes=[bdst])

        for l in range(2):
            need_ctx = (l == 0)
            xsrc = xin if l == 0 else X
            with ExitStack() as es:
                sb, ps = pools(es)
                CC, bCC = load(es, "CC", cc_in, [128, 8, 2])
                op("act", lambda e: e.activation(out=CC[:], in_=CC[:], func=AF.Silu), reads=[bCC], writes=[bCC])
                BM, bBM = sb("BM", [2, 6 * D])
                dma("sp", lambda e: e.dma_start(out=BM[:], in_=b_mod[l].partition_broadcast(2)), writes=[bBM])
                MO, bMO = sb("MO", [2, 6 * D])
                WM = [sb(f"WM{i}", [128, 3072]) for i in range(2)]
                pM, bpM = ps("pM", [2, 3072])
                it = 0
                for half in range(2):
                    for kc in range(8):
                        W, bW = WM[it % 2]; it += 1
                        dma("sp", lambda e: e.dma_start(out=W[:], in_=w_mod[l, kc * 128:(kc + 1) * 128, half * 3072:(half + 1) * 3072]), writes=[bW])
                        for n in range(6):
                            op("pe", lambda e: e.matmul(pM[:, n * 512:(n + 1) * 512], lhsT=CC[:, kc, :], rhs=W[:, n * 512:(n + 1) * 512],
                                                        start=(kc == 0), stop=(kc == 7)), reads=[bCC, bW], writes=[bpM])
                    op("dve", lambda e: e.tensor_tensor(out=MO[:, half * 3072:(half + 1) * 3072], in0=pM[:], in1=BM[:, half * 3072:(half + 1) * 3072], op=ALU.add),
                       reads=[bpM, bBM], writes=[bMO])
                for s in (1, 4):
                    op("dve", lambda e: e.tensor_scalar(out=MO[:, s * D:(s + 1) * D], in0=MO[:, s * D:(s + 1) * D], scalar1=1.0, scalar2=None, op0=ALU.add),
                       reads=[bMO], writes=[bMO])
                dma("sp", lambda e: e.dma_start(out=MODS, in_=MO[:]), reads=[bMO])
            P.barrier()

            def modv(es, name, which, slot):
                return load(es, name, bc(MODS[which, slot * D:(slot + 1) * D]), [128, D])

            with ExitStack() as es:
                sb, ps = pools(es)
                WI, bWI = load(es, "WI", w_in[l].rearrange("(k p) n -> p k n", p=128), [128, 8, 2048])
                SC = [modv(es, "SC1L", 0, 1), modv(es, "SC1C", 1, 1)]
                SH = [modv(es, "SH1L", 0, 0), modv(es, "SH1C", 1, 0)]
                BCt, bBC = load(es, "BCt", cst["BC"], [128, 128]); BSt, bBS = load(es, "BSt", cst["BS"], [128, 128])
                xt = [sb(f"xt{i}", [128, D]) for i in range(2)]
                hts1 = [sb(f"ht{i}", [128, D]) for i in range(2)]; hTs1 = [sb(f"hT{i}", [128, 8, 128]) for i in range(2)]
                ut = [sb(f"ut{i}", [128, 2048]) for i in range(2)]
                aT, baT = sb("aT", [128, 2, 128]); pq = [sb(f"pq{i}", [128, 4, 128]) for i in range(2)]
                tmp = ln_tmp(es, "1")
                pT, bpT = ps("pT", [128, 1024]); pU, bpU = ps("pU", [128, 2048])
                bpUn = [Buf() for _ in range(4)]; bUtn = [[Buf() for _ in range(4)] for _ in range(2)]
                pA, bpA = ps("pA", [128, 512]); pPQ, bpPQ = ps("pPQ", [128, 512])
                def p1_front(i):
                    ctxk = 1 if i < 2 else 0
                    Xt, bX = xt[i % 2]; Ut, bUt = ut[i % 2]; PQ, bPQ = pq[i % 2]; ht, bht = hts1[i % 2]; hT, bhT = hTs1[i % 2]
                    dma("sp", lambda e: e.dma_start(out=Xt[:], in_=xsrc[i * 128:(i + 1) * 128, :]), writes=[bX])
                    layer_norm(es, "1", Xt, bX, ht, bht, tmp)
                    mul_add(ht, bht, ht, bht, SC[ctxk][0], SC[ctxk][1], SH[ctxk][0], SH[ctxk][1])
                    transpose8(ht, bht, pT, bpT, hT, bhT)
                def p1_mid(i):
                    ctxk = 1 if i < 2 else 0
                    Xt, bX = xt[i % 2]; Ut, bUt = ut[i % 2]; PQ, bPQ = pq[i % 2]; ht, bht = hts1[i % 2]; hT, bhT = hTs1[i % 2]
                    for n in range(4):
                        for k in range(8):
                            op("pe", lambda e: e.matmul(pU[:, n * 512:(n + 1) * 512], lhsT=hT[:, k, :], rhs=WI[:, k, n * 512:(n + 1) * 512],
                                                        start=(k == 0), stop=(k == 7)), reads=[bhT, bWI], writes=[bpUn[n]])
                        if n % 2 == 0:
                            op("act", lambda e: e.copy(out=Ut[:, n * 512:(n + 1) * 512], in_=pU[:, n * 512:(n + 1) * 512]), reads=[bpUn[n]], writes=[bUtn[i % 2][n]])
                        else:
                            op("dve", lambda e: e.tensor_copy(out=Ut[:, n * 512:(n + 1) * 512], in_=pU[:, n * 512:(n + 1) * 512]), reads=[bpUn[n]], writes=[bUtn[i % 2][n]])
                    dma("pool", lambda e: e.dma_start(out=U[i * 128:(i + 1) * 128, :], in_=Ut[:]), reads=bUtn[i % 2])
                def p1_dft(i):
                    ctxk = 1 if i < 2 else 0
                    Xt, bX = xt[i % 2]; Ut, bUt = ut[i % 2]; PQ, bPQ = pq[i % 2]; ht, bht = hts1[i % 2]; hT, bhT = hTs1[i % 2]
                    for kc in range(2):
                        op("pe", lambda e: e.transpose(out=pA[:, kc * 128:(kc + 1) * 128], in_=Ut[:, kc * 128:(kc + 1) * 128], identity=ID[:]),
                           reads=[bUtn[i % 2][0], bID], writes=[bpA])
                    op("dve", lambda e: e.tensor_copy(out=aT[:].rearrange("p a b -> p (a b)"), in_=pA[:, 0:256]), reads=[bpA], writes=[baT])
                    for kc in range(2):
                        op("pe", lambda e: e.matmul(pPQ[:, kc * 128:(kc + 1) * 128], lhsT=BCt[:], rhs=aT[:, kc, :], start=True, stop=True),
                           reads=[baT, bBC], writes=[bpPQ])
                        op("pe", lambda e: e.matmul(pPQ[:, (2 + kc) * 128:(3 + kc) * 128], lhsT=BSt[:], rhs=aT[:, kc, :], start=True, stop=True),
                           reads=[baT, bBS], writes=[bpPQ])
                    op("dve", lambda e: e.tensor_copy(out=PQ[:].rearrange("p a b -> p (a b)"), in_=pPQ[:]), reads=[bpPQ], writes=[bPQ])
                    dma("pool", lambda e: e.dma_start(out=PTD[:, i * 128:(i + 1) * 128].rearrange("(k p) t -> p k t", p=128), in_=PQ[:, 0:2, :]), reads=[bPQ])
                    dma("pool", lambda e: e.dma_start(out=QTD[:, i * 128:(i + 1) * 128].rearrange("(k p) t -> p k t", p=128), in_=PQ[:, 2:4, :]), reads=[bPQ])
                p1_front(0)
                for i in range(NT):
                    p1_mid(i)
                    if i + 1 < NT:
                        p1_front(i + 1)
                    p1_dft(i)
            P.barrier()
            if debug and l == 0 and debug == "p1":
                break
            seqs = [("L", 2, NL)] + ([("C", 0, 2)] if need_ctx else [])
            for (nm, t0, N1) in seqs:
                off = t0 * 128
                with ExitStack() as es:
                    sb, ps = pools(es)
                    FAP, bFAP = load(es, "FAP", cst["FAP" + nm], [N1, 2 * N1]); FAQ, bFAQ = load(es, "FAQ", cst["FAQ" + nm], [N1, 2 * N1])
                    TWC, bTWC = load(es, "TWC", cst["TWC" + nm], [128, N1]); TWS, bTWS = load(es, "TWS", cst["TWS" + nm], [128, N1])
                    FBC, bFBC = load(es, "FBC", cst["FBC" + nm], [128, 128]); FBS, bFBS = load(es, "FBS", cst["FBS" + nm], [128, 128])
                    PTs, bPTs = sb("PTs", [N1, 64, 128]); QTs, bQTs = sb("QTs", [N1, 64, 128])
                    At, bAt = sb("At", [128, 64, N1]); Bt, bBt = sb("Bt", [128, 64, N1])
                    cpa = min(64, 512 // (2 * N1)); cpb = min(64, 512 // N1)
                    tt = [sb(f"ft{i}", [128, cpa, N1]) for i in range(4)]
                    Yt, bYt = sb("Yt", [128, N1, 64])
                    pZ = [ps(f"pZ{i}", [128, 512]) for i in range(2)]; pY = [ps(f"pY{i}", [128, 512]) for i in range(2)]
                    for g in range(4):
                        dma("sp", lambda e: e.dma_start(out=PTs[:], in_=PTD[g * 64:(g + 1) * 64, off:off + 128 * N1].rearrange("c (a b) -> a c b", b=128)), writes=[bPTs])
                        dma("sp", lambda e: e.dma_start(out=QTs[:], in_=QTD[g * 64:(g + 1) * 64, off:off + 128 * N1].rearrange("c (a b) -> a c b", b=128)), writes=[bQTs])
                        for ci0, c0 in enumerate(range(0, 64, cpa)):
                            Z, bZ = pZ[ci0 % 2]
                            for ci in range(cpa):
                                c = c0 + ci
                                op("pe", lambda e: e.matmul(Z[:, ci * 2 * N1:(ci + 1) * 2 * N1], lhsT=PTs[:, c, :], rhs=FAP[:], start=True, stop=False), reads=[bPTs, bFAP], writes=[bZ])
                                op("pe", lambda e: e.matmul(Z[:, ci * 2 * N1:(ci + 1) * 2 * N1], lhsT=QTs[:, c, :], rhs=FAQ[:], start=False, stop=True), reads=[bQTs, bFAQ], writes=[bZ])
                            Zv = Z[:, 0:cpa * 2 * N1].rearrange("p (c t k) -> p c t k", t=2, k=N1)
                            Wv = Zv[:, :, 0, :]; Vv = Zv[:, :, 1, :]
                            cb = TWC[:].unsqueeze(1).to_broadcast([128, cpa, N1]); sbb = TWS[:].unsqueeze(1).to_broadcast([128, cpa, N1])
                            (t1, b1), (t2, b2), (t3, b3), (t4, b4) = tt
                            op("dve", lambda e: e.tensor_tensor(out=t1[:], in0=Wv, in1=cb, op=ALU.mult), reads=[bZ, bTWC], writes=[b1])
                            op("dve", lambda e: e.tensor_tensor(out=t2[:], in0=Vv, in1=sbb, op=ALU.mult), reads=[bZ, bTWS], writes=[b2])
                            op("dve", lambda e: e.tensor_tensor(out=t3[:], in0=Wv, in1=sbb, op=ALU.mult), reads=[bZ, bTWS], writes=[b3])
                            op("dve", lambda e: e.tensor_tensor(out=t4[:], in0=Vv, in1=cb, op=ALU.mult), reads=[bZ, bTWC], writes=[b4])
                            op("pool", lambda e: e.tensor_tensor(out=At[:, c0:c0 + cpa, :], in0=t1[:], in1=t2[:], op=ALU.subtract), reads=[b1, b2], writes=[bAt])
                            op("pool", lambda e: e.tensor_tensor(out=Bt[:, c0:c0 + cpa, :], in0=t3[:], in1=t4[:], op=ALU.add), reads=[b3, b4], writes=[bBt])
                        for ci0, c0 in enumerate(range(0, 64, cpb)):
                            Y, bY = pY[ci0 % 2]
                            ncol = cpb * N1
                            op("pe", lambda e: e.matmul(Y[:, 0:ncol], lhsT=FBC[:], rhs=At[:, c0:c0 + cpb, :].rearrange("p c k -> p (c k)"), start=True, stop=False), reads=[bAt, bFBC], writes=[bY])
                            op("pe", lambda e: e.matmul(Y[:, 0:ncol], lhsT=FBS[:], rhs=Bt[:, c0:c0 + cpb, :].rearrange("p c k -> p (c k)"), start=False, stop=True), reads=[bBt, bFBS], writes=[bY])
                            op("act", lambda e: e.copy(out=Yt[:, :, c0:c0 + cpb].rearrange("p k c -> p c k"), in_=Y[:, 0:ncol].rearrange("p (c k) -> p c k", k=N1)), reads=[bY], writes=[bYt])
                        dma("pool", lambda e: e.dma_start(out=M[off:off + 128 * N1, g * 64:(g + 1) * 64].rearrange("(a b) c -> a b c", b=N1), in_=Yt[:]), reads=[bYt])
                P.barrier(new_epoch=False)
            if debug == "p2a":
                break
            with ExitStack() as es:
                sb, ps = pools(es)
                PBt, bPBt = load(es, "PBt", cst["PB"], [128, 20, 128])
                PW, bPW = load(es, "PW", pool_w[l].rearrange("g c d -> c g d"), [64, 4, 64])
                PSC, bPSC = load(es, "PSC", bc(pool_scale[l]), [128, 256])
                bt = [[sb(f"bt{i}_{j}", [128, 256]) for j in range(3)] for i in range(2)]
                pooledT, bpooledT = sb("pooledT", [64, 4, 128])
                ots = [sb(f"pot{i}", [128, 256]) for i in range(2)]
                pP, bpP = ps("pP", [64, 512]); pO, bpO = ps("pO", [128, 256])
                it = 0
                for (nm, t0, nt) in seqs:
                    for i in range(nt):
                        B3 = bt[it % 2]; ot, bot = ots[it % 2]; it += 1
                        for j, ti in enumerate((i - 1, i, i + 1)):
                            if 0 <= ti < nt:
                                dma("sp", lambda e: e.dma_start(out=B3[j][0][:], in_=U[(t0 + ti) * 128:(t0 + ti + 1) * 128, 256:512]), writes=[B3[j][1]])
                        kind = 1 if i == 0 else (2 if i == nt - 1 else 0)
                        terms = [(1, kind)] + ([(0, 3)] if i > 0 else []) + ([(2, 4)] if i < nt - 1 else [])
                        for g in range(4):
                            for ti_, (j, kd) in enumerate(terms):
                                op("pe", lambda e: e.matmul(pP[:, g * 128:(g + 1) * 128], lhsT=B3[j][0][:, g * 64:(g + 1) * 64], rhs=PBt[:, g * 5 + kd, :],
                                                            start=(ti_ == 0), stop=(ti_ == len(terms) - 1)), reads=[B3[j][1], bPBt], writes=[bpP])
                        op("act", lambda e: e.copy(out=pooledT[:].rearrange("p a b -> p (a b)"), in_=pP[:]), reads=[bpP], writes=[bpooledT])
                        for g in range(4):
                            op("pe", lambda e: e.matmul(pO[:, g * 64:(g + 1) * 64], lhsT=pooledT[:, g, :], rhs=PW[:, g, :], start=True, stop=True), reads=[bpooledT, bPW], writes=[bpO])
                        op("dve", lambda e: e.tensor_tensor(out=ot[:], in0=pO[:], in1=PSC[:], op=ALU.mult), reads=[bpO, bPSC], writes=[bot])
                        dma("pool", lambda e: e.dma_start(out=M[(t0 + i) * 128:(t0 + i + 1) * 128, 256:512], in_=ot[:]), reads=[bot])
            P.barrier(new_epoch=False)
            if debug == "p2b":
                break
            with ExitStack() as es:
                sb, ps = pools(es)
                MP = load(es, "MP", cst["MASKP"], [128, 128]); MN = load(es, "MN", cst["MASKN"], [128, 128])
                SINK, bSINK = load(es, "SINK", bc(attn_sink[l]), [128, 4])
                NSINK, bNSINK = sb("NSINK", [128, 4])
                op("dve", lambda e: e.tensor_scalar(out=NSINK[:], in0=SINK[:], scalar1=-1.0, scalar2=None, op0=ALU.mult), reads=[bSINK], writes=[bNSINK])
                RING = 4
                QKV = [sb(f"QKV{i}", [128, 512]) for i in range(RING)]
                qT = [sb(f"qT{i}", [64, 4, 128]) for i in range(RING)]
                kT = [sb(f"kT{i}", [64, 2, 128]) for i in range(RING)]
                cQKV = [sb(f"cQKV{i}", [128, 512]) for i in range(2)]
                cqT = [sb(f"cqT{i}", [64, 4, 128]) for i in range(2)]
                ckT = [sb(f"ckT{i}", [64, 2, 128]) for i in range(2)]
                RC = [sb(f"RC{i}", [128, 32]) for i in range(2)]; RS = [sb(f"RS{i}", [128, 32]) for i in range(2)]
                rtmp, brtmp = sb("rtmp", [128, 6, 2, 16]); rt2, brt2 = sb("rt2", [128, 6, 16]); rt3, brt3 = sb("rt3", [128, 6, 16])
                scs = [sb(f"sc{i}", [128, 2, 5, 128]) for i in range(2)]; prs = [sb(f"pr{i}", [128, 2, 640]) for i in range(2)]; pTss = [sb(f"pTs{i}", [128, 2, 640]) for i in range(2)]
                rmaxs = [sb(f"rmax{i}", [128, 2]) for i in range(2)]; negms = [sb(f"negm{i}", [128, 2]) for i in range(2)]
                rsums = [sb(f"rsum{i}", [128, 2]) for i in range(2)]; esks = [sb(f"esk{i}", [128, 2]) for i in range(2)]
                aos = [sb(f"ao{i}", [128, 256]) for i in range(2)]
                if l == 0:
                    ZT, bZT = sb("ZT", [128, 8192])
                    op("pool", lambda e: e.memset(ZT[:], 0.0), writes=[bZT])
                pS, bpS = ps("pS", [128, 1536]); pPT, bpPT = ps("pPT", [128, 1536]); pO, bpO = ps("pOa", [128, 256])
                pQK, bpQK = pPT[0:64, 0:1024], bpPT

                def prep(ti, QKVt, qTt, kTt, rope_slot):
                    Q, bQ = QKVt
                    dma("sp", lambda e: e.dma_start(out=Q[:], in_=U[ti * 128:(ti + 1) * 128, 512:1024]), writes=[bQ])
                    if rope_slot is not None:
                        (C, bC), (S, bS) = RC[rope_slot], RS[rope_slot]
                        r0 = (ti - 2) * 128
                        dma("sp", lambda e: e.dma_start(out=C[:], in_=cst["ROPC"][r0:r0 + 128, :]), writes=[bC])
                        dma("sp", lambda e: e.dma_start(out=S[:], in_=cst["ROPS"][r0:r0 + 128, :]), writes=[bS])
                        for hf in range(2):
                            xv = Q[:, 0:384].rearrange("p (h f t d) -> p h f t d", h=6, f=2, t=2)[:, :, hf, :, :]
                            cb = C[:, hf * 16:(hf + 1) * 16].unsqueeze(1).unsqueeze(1).to_broadcast([128, 6, 2, 16])
                            sbb = S[:, hf * 16:(hf + 1) * 16].unsqueeze(1).to_broadcast([128, 6, 16])
                            op("dve", lambda e: e.tensor_tensor(out=rtmp[:], in0=xv, in1=cb, op=ALU.mult), reads=[bQ, bC], writes=[brtmp])
                            op("dve", lambda e: e.tensor_tensor(out=rt2[:], in0=xv[:, :, 1, :], in1=sbb, op=ALU.mult), reads=[bQ, bS], writes=[brt2])
                            op("dve", lambda e: e.tensor_tensor(out=rt3[:], in0=xv[:, :, 0, :], in1=sbb, op=ALU.mult), reads=[bQ, bS], writes=[brt3])
                            op("dve", lambda e: e.tensor_tensor(out=xv[:, :, 0, :], in0=rtmp[:, :, 0, :], in1=rt2[:], op=ALU.subtract), reads=[brtmp, brt2], writes=[bQ])
                            op("dve", lambda e: e.tensor_tensor(out=xv[:, :, 1, :], in0=rtmp[:, :, 1, :], in1=rt3[:], op=ALU.add), reads=[brtmp, brt3], writes=[bQ])
                    for h in range(6):
                        op("pe", lambda e: e.transpose(out=pQK[:, h * 128:(h + 1) * 128], in_=Q[:, h * 64:(h + 1) * 64], identity=ID[:]), reads=[bQ, bID], writes=[bpQK])
                    op("act", lambda e: e.copy(out=qTt[0][:].rearrange("p a b -> p (a b)"), in_=pQK[:, 0:512]), reads=[bpQK], writes=[qTt[1]])
                    op("act", lambda e: e.copy(out=kTt[0][:].rearrange("p a b -> p (a b)"), in_=pQK[:, 512:768]), reads=[bpQK], writes=[kTt[1]])

                def attend(qTt, blocks, ti, ao_t):
                    ao, bao = ao_t
                    nb = len(blocks); Wd = nb * 128

                    def stA(hp):
                        (sc, bsc), (pr, bpr) = scs[hp % 2], prs[hp % 2]
                        (rmax, brmax), (negm, bnegm), (rsum, brsum), (esk, besk) = rmaxs[hp % 2], negms[hp % 2], rsums[hp % 2], esks[hp % 2]
                        for g in range(2):
                            for bi, (kTt, Vt, mask) in enumerate(blocks):
                                op("pe", lambda e: e.matmul(pS[:, g * 640 + bi * 128:g * 640 + (bi + 1) * 128], lhsT=qTt[0][:, 2 * hp + g, :], rhs=kTt[0][:, hp, :], start=True, stop=True),
                                   reads=[qTt[1], kTt[1]], writes=[bpS])
                        pSv = pS[:, 0:1280].rearrange("p (g b c) -> p g b c", g=2, b=5)
                        for bi, (kTt, Vt, mask) in enumerate(blocks):
                            if mask is not None:
                                op("dve", lambda e: e.tensor_tensor(out=sc[:, :, bi, :], in0=pSv[:, :, bi, :], in1=mask[0][:].unsqueeze(1).to_broadcast([128, 2, 128]), op=ALU.add),
                                   reads=[bpS, mask[1]], writes=[bsc])
                            else:
                                op("act", lambda e: e.copy(out=sc[:, :, bi, :], in_=pSv[:, :, bi, :]), reads=[bpS], writes=[bsc])
                        for g in range(2):
                            scg = sc[:, g, 0:nb, :].rearrange("p b c -> p (b c)")
                            op("dve", lambda e: e.reduce_max(out=rmax[:, g:g + 1], in_=scg, axis=AX.X), reads=[bsc], writes=[brmax])
                        op("dve", lambda e: e.scalar_tensor_tensor(out=negm[:], in0=rmax[:], scalar=-0.125, in1=NSINK[:, 2 * hp:2 * hp + 2], op0=ALU.mult, op1=ALU.min),
                           reads=[brmax, bNSINK], writes=[bnegm])
                        for g in range(2):
                            scg = sc[:, g, 0:nb, :].rearrange("p b c -> p (b c)")
                            op("act", lambda e: e.activation(out=pr[:, g, 0:Wd], in_=scg, func=AF.Exp, bias=negm[:, g:g + 1], scale=0.125), reads=[bsc, bnegm], writes=[bpr])
                        for g in range(2):
                            op("dve", lambda e: e.reduce_sum(out=rsum[:, g:g + 1], in_=pr[:, g, 0:Wd], axis=AX.X), reads=[bpr], writes=[brsum])
                        op("dve", lambda e: e.tensor_tensor(out=esk[:], in0=SINK[:, 2 * hp:2 * hp + 2], in1=negm[:], op=ALU.add), reads=[bSINK, bnegm], writes=[besk])
                        op("act", lambda e: e.activation(out=esk[:], in_=esk[:], func=AF.Exp), reads=[besk], writes=[besk])
                        op("dve", lambda e: e.tensor_tensor(out=rsum[:], in0=rsum[:], in1=esk[:], op=ALU.add), reads=[brsum, besk], writes=[brsum])
                        op("dve", lambda e: e.reciprocal(out=rsum[:], in_=rsum[:]), reads=[brsum], writes=[brsum])

                    def stB(hp):
                        (pr, bpr), (pTs, bpTs), (rsum, brsum) = prs[hp % 2], pTss[hp % 2], rsums[hp % 2]
                        for g in range(2):
                            for bi in range(nb):
                                op("pe", lambda e: e.transpose(out=pPT[:, g * 640 + bi * 128:g * 640 + (bi + 1) * 128], in_=pr[:, g, bi * 128:(bi + 1) * 128], identity=ID[:]),
                                   reads=[bpr, bID], writes=[bpPT])
                        op("act", lambda e: e.copy(out=pTs[:, :, 0:Wd], in_=pPT[:, 0:1280].rearrange("p (g w) -> p g w", g=2)[:, :, 0:Wd]), reads=[bpPT], writes=[bpTs])
                        for g in range(2):
                            hq = 2 * hp + g
                            for bi, (kTt, Vt, mask) in enumerate(blocks):
                                op("pe", lambda e: e.matmul(pO[:, hq * 64:(hq + 1) * 64], lhsT=pTs[:, g, bi * 128:(bi + 1) * 128], rhs=Vt[0][:, 384 + hp * 64:384 + (hp + 1) * 64],
                                                            start=(bi == 0), stop=(bi == nb - 1)), reads=[bpTs, Vt[1]], writes=[bpO])
                        op("dve", lambda e: e.tensor_tensor(out=ao[:, hp * 128:(hp + 1) * 128].rearrange("p (g d) -> p g d", g=2),
                                                            in0=pO[:, hp * 128:(hp + 1) * 128].rearrange("p (g d) -> p g d", g=2),
                                                            in1=rsum[:].unsqueeze(2).to_broadcast([128, 2, 64]), op=ALU.mult), reads=[bpO, brsum], writes=[bao])

                    stA(0); stA(1); stB(0); stB(1)
                    dma("pool", lambda e: e.dma_start(out=M[ti * 128:(ti + 1) * 128, 512:768], in_=ao[:]), reads=[bao])

                for t in range(2):
                    prep(t, cQKV[t], cqT[t], ckT[t], None)
                cblocks = [(ckT[0], cQKV[0], None), (ckT[1], cQKV[1], None)]
                if need_ctx:
                    for t in range(2):
                        attend(cqT[t], cblocks, t, aos[t % 2])
                prep(2, QKV[0], qT[0], kT[0], 0)
                for i in range(NL):
                    if i + 1 < NL:
                        s = (i + 1) % RING
                        prep(i + 3, QKV[s], qT[s], kT[s], (i + 1) % 2)
                    blocks = []
                    if i > 0:
                        blocks.append((kT[(i - 1) % RING], QKV[(i - 1) % RING], MP))
                    blocks.append((kT[i % RING], QKV[i % RING], None))
                    if i + 1 < NL:
                        blocks.append((kT[(i + 1) % RING], QKV[(i + 1) % RING], MN))
                    blocks += cblocks
                    attend(qT[i % RING], blocks, i + 2, aos[i % 2])
                    if l == 0:
                        a0, a1 = (i * NBLK) // NL, ((i + 1) * NBLK) // NL
                        for a in range(a0, a1, 8):
                            ae = min(a1, a + 8)
                            dma("sp", lambda e: e.dma_start(out=XS.rearrange("(a p) d -> p a d", p=128)[:, a:ae, :], in_=ZT[:, 0:(ae - a) * D].rearrange("p (a d) -> p a d", d=D)), reads=[bZT])
            P.barrier(new_epoch=False)
            if debug == "p2c":
                break
            with ExitStack() as es:
                sb, ps = pools(es)
                POSZ, bPOSZ = load(es, "POSZ", cst["POSZ"], [128, 4, 2]); POSXI, bPOSXI = load(es, "POSXI", cst["POSXI"], [128, 128])
                PFt, bPF = load(es, "PFt", cst["PF"], [128, 128]); MFt, bMF = load(es, "MFt", cst["MF"], [128, 128])
                PBKt, bPBK = load(es, "PBKt", cst["PBK"], [128, 128]); MBt, bMB = load(es, "MBt", cst["MB"], [128, 128])
                LGX, bLGX = sb("LGX", [128, 4]); LG, bLG = sb("LG", [128, 2, 4])
                dma("sp", lambda e: e.dma_start(out=LGX[0:64, :], in_=ret_decay[l, 0].partition_broadcast(64)), writes=[bLGX])
                dma("sp", lambda e: e.dma_start(out=LGX[64:128, :], in_=ret_decay[l, 1].partition_broadcast(64)), writes=[bLGX])
                dma("sp", lambda e: e.dma_start(out=LG[:].rearrange("p a b -> p (a b)"), in_=ret_decay[l].rearrange("a b -> (a b)").partition_broadcast(128)), writes=[bLG])
                for (Tt, bT) in ((LGX, bLGX), (LG, bLG)):
                    op("act", lambda e: e.activation(out=Tt[:], in_=Tt[:], func=AF.Exp), reads=[bT], writes=[bT])
                    op("dve", lambda e: e.tensor_scalar(out=Tt[:], in0=Tt[:], scalar1=-1.0, scalar2=None, op0=ALU.mult), reads=[bT], writes=[bT])
                ZETA, bZETA = sb("ZETA", [128, 4, 2]); XI, bXI = sb("XI", [128, 4, 128]); GC, bGC = sb("GC", [128, 4]); DS, bDS = sb("DS", [128, 4, 128])
                e1, be1 = sb("e1", [128, 128]); e2, be2 = sb("e2", [128, 128])
                op("dve", lambda e: e.tensor_tensor(out=ZETA[:], in0=POSZ[:], in1=LG[:].rearrange("p d h -> p h d"), op=ALU.mult), reads=[bPOSZ, bLG], writes=[bZETA])
                op("act", lambda e: e.activation(out=ZETA[:], in_=ZETA[:], func=AF.Exp), reads=[bZETA], writes=[bZETA])
                op("dve", lambda e: e.tensor_scalar(out=ZETA[:], in0=ZETA[:], scalar1=0.125, scalar2=None, op0=ALU.mult), reads=[bZETA], writes=[bZETA])
                op("act", lambda e: e.activation(out=GC[:], in_=LGX[:], func=AF.Exp, scale=128.0), reads=[bLGX], writes=[bGC])
                for h in range(4):
                    op("act", lambda e: e.activation(out=XI[:, h, :], in_=POSXI[:], func=AF.Exp, scale=LGX[:, h:h + 1]), reads=[bPOSXI, bLGX], writes=[bXI])
                    op("act", lambda e: e.activation(out=e1[:], in_=PFt[:], func=AF.Exp, scale=LG[:, 0, h:h + 1]), reads=[bPF, bLG], writes=[be1])
                    op("dve", lambda e: e.tensor_tensor(out=e1[:], in0=e1[:], in1=MFt[:], op=ALU.mult), reads=[be1, bMF], writes=[be1])
                    op("act", lambda e: e.activation(out=e2[:], in_=PBKt[:], func=AF.Exp, scale=LG[:, 1, h:h + 1]), reads=[bPBK, bLG], writes=[be2])
                    op("dve", lambda e: e.tensor_tensor(out=e2[:], in0=e2[:], in1=MBt[:], op=ALU.mult), reads=[be2, bMB], writes=[be2])
                    op("dve", lambda e: e.tensor_tensor(out=e1[:], in0=e1[:], in1=e2[:], op=ALU.add), reads=[be1, be2], writes=[be1])
                    op("dve", lambda e: e.tensor_scalar(out=DS[:, h, :], in0=e1[:], scalar1=0.125, scalar2=None, op0=ALU.mult), reads=[be1], writes=[bDS])
                RB, _ = sb("RB", [64, NT, 4, 64]); bRB = [Buf() for _ in range(NT)]
                RBB, _ = sb("RBB", [64, NT, 4, 64])
                GCB, bGCB = sb("GCB", [64, 4]); XIB, bXIB = sb("XIB", [64, 4, 128])
                PXB, bPXB = load(es, "PXB", cst["POSXI"][64:128, :], [64, 128])
                op("act", lambda e: e.activation(out=GCB[:], in_=LG[0:64, 1, :], func=AF.Exp, scale=128.0), reads=[bLG], writes=[bGCB])
                for h in range(4):
                    op("act", lambda e: e.activation(out=XIB[:, h, :], in_=PXB[:], func=AF.Exp, scale=LG[0:64, 1, h:h + 1]), reads=[bPXB, bLG], writes=[bXIB])
                QXBs = [sb(f"QXB{i}", [64, 128]) for i in range(2)]; kTrs = [sb(f"kTr{i}", [64, 128]) for i in range(2)]
                Rts = [sb(f"Rt{i}", [128, 1024]) for i in range(2)]
                KZ, bKZ = sb("KZ", [128, 4, 2, 64]); RQ2, bRQ2 = sb("RQ2", [128, 4, 2, 64])
                QXs = [sb(f"QX{i}", [128, 128]) for i in range(2)]; qkTs = [sb(f"qkT{i}", [64, 256]) for i in range(2)]; SDs = [sb(f"SD{i}", [128, 128]) for i in range(2)]
                s1, bs1 = sb("s1", [128, 4]); s2, bs2 = sb("s2", [128, 4]); msq, bmsq = sb("msq", [128, 4])
                sq, bsq = sb("sq", [128, 256]); gn, bgn = sb("gn", [128, 4, 64]); sg, bsg = sb("sg", [128, 256])
                st4, bst4 = sb("st4", [128, 4, 6]); mv4, bmv4 = sb("mv4", [128, 4, 2])
                ros = [sb(f"ro{i}", [128, 256]) for i in range(2)]
                pKV, bpKV = ps("pKV", [64, 512]); pTRq, bpTRq = ps("pTRq", [64, 512]); pTRk, bpTRk = ps("pTRk", [64, 512])
                pSTa, bpSTa = ps("pSTa", [128, 512]); pOr, bpOr = ps("pOr", [128, 256])
                qTas = [sb(f"qTa{i}", [64, 4, 128]) for i in range(2)]; kTas = [sb(f"kTa{i}", [64, 4, 128]) for i in range(2)]
                QXas = [sb(f"QXa{i}", [64, 4, 128]) for i in range(2)]; QXBas = [sb(f"QXBa{i}", [64, 4, 128]) for i in range(2)]
                SDas = [sb(f"SDa{i}", [128, 4, 128]) for i in range(2)]
                op("pool", lambda e: e.memset(RBB[:, 1, :, :], 0.0), writes=[bRB[1]])
                op("pool", lambda e: e.memset(RB[:, 0, :, :], 0.0), writes=[bRB[0]])
                order_b = [1, 0] + list(range(NT - 1, 1, -1))
                it = 0

                def kv_states(Rt, bRt):
                    op("dve", lambda e: e.tensor_tensor(out=KZ[:], in0=Rt[:, 256:512].rearrange("p (h d) -> p h d", h=4).unsqueeze(2).to_broadcast([128, 4, 2, 64]),
                                                        in1=ZETA[:].unsqueeze(3).to_broadcast([128, 4, 2, 64]), op=ALU.mult), reads=[bRt, bZETA], writes=[bKZ])
                    for h in range(4):
                        for a in range(2):
                            op("pe", lambda e: e.matmul(pKV[0:64, (a * 4 + h) * 64:(a * 4 + h + 1) * 64], lhsT=KZ[:, h, a, :], rhs=Rt[:, 512 + h * 64:512 + (h + 1) * 64],
                                                        start=True, stop=True), reads=[bKZ, bRt], writes=[bpKV])

                for idx, ti in enumerate(order_b):
                    Rt, bRt = Rts[it % 2]; it += 1
                    dma("sp", lambda e: e.dma_start(out=Rt[:, 256:768], in_=U[ti * 128:(ti + 1) * 128, 1280:1792]), writes=[bRt])
                    kv_states(Rt, bRt)
                    if idx + 1 < NT:
                        nxt = order_b[idx + 1]
                        for h in range(4):
                            op("dve", lambda e: e.scalar_tensor_tensor(out=RBB[:, nxt, h, :], in0=RBB[:, ti, h, :], scalar=GCB[:, h:h + 1],
                                                                       in1=pKV[0:64, (4 + h) * 64:(5 + h) * 64], op0=ALU.mult, op1=ALU.add),
                               reads=[bRB[ti], bGCB, bpKV], writes=[bRB[nxt]])
                for ti in range(NT):
                    want_out = ((ti >= 2) or need_ctx) and not os.environ.get('RET_SKIP_OUT')
                    Rt, bRt = Rts[it % 2]; ro, bro = ros[it % 2]; it += 1
                    dma("sp", lambda e: e.dma_start(out=Rt[:], in_=U[ti * 128:(ti + 1) * 128, 1024:2048]), writes=[bRt])
                    kv_states(Rt, bRt)
                    if want_out:
                        (qTa, bqTa), (kTa, bkTa), (QXa, bQXa), (QXBa, bQXBa), (SDa, bSDa) = qTas[it % 2], kTas[it % 2], QXas[it % 2], QXBas[it % 2], SDas[it % 2]
                        for h in range(4):
                            op("pe", lambda e: e.transpose(out=pTRq[:, h * 128:(h + 1) * 128], in_=Rt[:, h * 64:(h + 1) * 64], identity=ID[:]), reads=[bRt, bID], writes=[bpTRq])
                            op("pe", lambda e: e.transpose(out=pTRk[:, h * 128:(h + 1) * 128], in_=Rt[:, 256 + h * 64:256 + (h + 1) * 64], identity=ID[:]), reads=[bRt, bID], writes=[bpTRk])
                        op("act", lambda e: e.copy(out=qTa[:].rearrange("p a b -> p (a b)"), in_=pTRq[:]), reads=[bpTRq], writes=[bqTa])
                        op("act", lambda e: e.copy(out=kTa[:].rearrange("p a b -> p (a b)"), in_=pTRk[:]), reads=[bpTRk], writes=[bkTa])
                        op("dve", lambda e: e.tensor_tensor(out=QXa[:], in0=qTa[:], in1=XI[0:64, :, :], op=ALU.mult), reads=[bqTa, bXI], writes=[bQXa])
                        op("dve", lambda e: e.tensor_tensor(out=QXBa[:], in0=qTa[:], in1=XIB[:], op=ALU.mult), reads=[bqTa, bXIB], writes=[bQXBa])
                        for h in range(4):
                            op("pe", lambda e: e.matmul(pSTa[:, h * 128:(h + 1) * 128], lhsT=kTa[:, h, :], rhs=qTa[:, h, :], start=True, stop=True), reads=[bkTa, bqTa], writes=[bpSTa])
                        op("dve", lambda e: e.tensor_tensor(out=SDa[:].rearrange("p a b -> p (a b)"), in0=pSTa[:], in1=DS[:].rearrange("p a b -> p (a b)"), op=ALU.mult), reads=[bpSTa, bDS], writes=[bSDa])
                        for h in range(4):
                            op("pe", lambda e: e.matmul(pOr[:, h * 64:(h + 1) * 64], lhsT=SDa[:, h, :], rhs=Rt[:, 512 + h * 64:512 + (h + 1) * 64], start=True, stop=False), reads=[bSDa, bRt], writes=[bpOr])
                            op("pe", lambda e: e.matmul(pOr[:, h * 64:(h + 1) * 64], lhsT=QXa[:, h, :], rhs=RB[:, ti, h, :], start=False, stop=False), reads=[bQXa, bRB[ti]], writes=[bpOr])
                            op("pe", lambda e: e.matmul(pOr[:, h * 64:(h + 1) * 64], lhsT=QXBa[:, h, :], rhs=RBB[:, ti, h, :], start=False, stop=True), reads=[bQXBa, bRB[ti]], writes=[bpOr])
                    if ti + 1 < NT:
                        for h in range(4):
                            op("dve", lambda e: e.scalar_tensor_tensor(out=RB[:, ti + 1, h, :], in0=RB[:, ti, h, :], scalar=GC[0:64, h:h + 1],
                                                                       in1=pKV[0:64, h * 64:(h + 1) * 64], op0=ALU.mult, op1=ALU.add),
                               reads=[bRB[ti], bGC, bpKV], writes=[bRB[ti + 1]])
                    if want_out and os.environ.get('RET_NO_GN'):
                        op("act", lambda e: e.copy(out=ro[:], in_=pOr[:]), reads=[bpOr], writes=[bro])
                        dma("pool", lambda e: e.dma_start(out=M[ti * 128:(ti + 1) * 128, 768:1024], in_=ro[:]), reads=[bro])
                    elif want_out:
                        op("act", lambda e: e.copy(out=sq[:], in_=pOr[:]), reads=[bpOr], writes=[bsq])
                        for h in range(4):
                            op("dve", lambda e: e.bn_stats(out=st4[:, h, :], in_=sq[:, h * 64:(h + 1) * 64]), reads=[bsq], writes=[bst4])
                        for h in range(4):
                            op("dve", lambda e: e.bn_aggr(out=mv4[:, h, :], in_=st4[:, h, :]), reads=[bst4], writes=[bmv4])
                        for h in range(4):
                            op("act", lambda e: e.activation(out=s2[:, h:h + 1], in_=mv4[:, h, 1:2], func=AF.Sqrt, bias=EPS, scale=1.0), reads=[bmv4], writes=[bs2])
                        op("dve", lambda e: e.reciprocal(out=s1[:], in_=s2[:]), reads=[bs2], writes=[bs1])
                        for h in range(4):
                            op("dve", lambda e: e.tensor_scalar(out=gn[:, h, :], in0=sq[:, h * 64:(h + 1) * 64], scalar1=mv4[:, h, 0:1], scalar2=s1[:, h:h + 1], op0=ALU.subtract, op1=ALU.mult),
                               reads=[bsq, bmv4, bs1], writes=[bgn])
                        op("act", lambda e: e.activation(out=sg[:], in_=Rt[:, 768:1024], func=AF.Silu), reads=[bRt], writes=[bsg])
                        op("dve", lambda e: e.tensor_tensor(out=ro[:], in0=gn[:].rearrange("p h d -> p (h d)"), in1=sg[:], op=ALU.mult), reads=[bgn, bsg], writes=[bro])
                        dma("pool", lambda e: e.dma_start(out=M[ti * 128:(ti + 1) * 128, 768:1024], in_=ro[:]), reads=[bro])
            P.barrier()
            if debug == "p2":
                break
            tiles3 = list(range(NT)) if need_ctx else list(range(2, NT))
            with ExitStack() as es:
                sb, ps = pools(es)
                WO, bWO = load(es, "WO", w_out[l].rearrange("(k p) n -> p k n", p=128), [128, 8, D])
                RW, bRW = load(es, "RW", router_w[l].rearrange("(k p) n -> p k n", p=128), [128, 8, NEXP])
                RBi, bRBi = load(es, "RBi", bc(router_b[l]), [128, NEXP])
                G1 = [modv(es, "G1L", 0, 2), modv(es, "G1C", 1, 2)]
                SC2 = [modv(es, "SC2L", 0, 4), modv(es, "SC2C", 1, 4)]
                SH2 = [modv(es, "SH2L", 0, 3), modv(es, "SH2C", 1, 3)]
                LNG = load(es, "LNG", bc(ln_g[l, 0]), [128, D]); LNB = load(es, "LNB", bc(ln_b[l, 0]), [128, D])
                mts = [sb(f"mt{i}", [128, D]) for i in range(2)]; xts = [sb(f"x3t{i}", [128, D]) for i in range(2)]
                mTs = [sb(f"mT{i}", [128, 8, 128]) for i in range(2)]; t3s = [sb(f"t3{i}", [128, D]) for i in range(2)]; xns = [sb(f"xn{i}", [128, D]) for i in range(2)]
                h2s = [sb(f"h2t{i}", [128, D]) for i in range(2)]; h2Ts = [sb(f"h2T{i}", [128, 8, 128]) for i in range(2)]
                bpYn = [Buf() for _ in range(2)]
                tmp = ln_tmp(es, "3")
                pT, bpT = ps("pT3", [128, 1024]); pY, bpY = ps("pY3", [128, 1024]); pL, bpL = ps("pL", [128, NEXP])
                pT2, bpT2 = ps("pT3b", [128, 1024])

                def front(ii):
                    i = tiles3[ii]
                    ck = 1 if i < 2 else 0
                    Mt, bMt = mts[ii % 2]; Xt, bXt = xts[ii % 2]; h2, bh2 = h2s[ii % 2]
                    (mT, bmT), (t3, bt3), (xn, bxn) = mTs[ii % 2], t3s[ii % 2], xns[ii % 2]
                    dma("sp", lambda e: e.dma_start(out=Mt[:], in_=M[i * 128:(i + 1) * 128, :]), writes=[bMt])
                    dma("sp", lambda e: e.dma_start(out=Xt[:], in_=xsrc[i * 128:(i + 1) * 128, :]), writes=[bXt])
                    transpose8(Mt, bMt, pT, bpT, mT, bmT)
                    for n in range(2):
                        for k in range(8):
                            op("pe", lambda e: e.matmul(pY[:, n * 512:(n + 1) * 512], lhsT=mT[:, k, :], rhs=WO[:, k, n * 512:(n + 1) * 512], start=(k == 0), stop=(k == 7)),
                               reads=[bmT, bWO], writes=[bpYn[n]])
                        op("dve", lambda e: e.tensor_tensor(out=t3[:, n * 512:(n + 1) * 512], in0=pY[:, n * 512:(n + 1) * 512], in1=G1[ck][0][:, n * 512:(n + 1) * 512], op=ALU.mult),
                           reads=[bpYn[n], G1[ck][1]], writes=[bt3])
                    op("dve", lambda e: e.scalar_tensor_tensor(out=t3[:], in0=Xt[:], scalar=ALPHA, in1=t3[:], op0=ALU.mult, op1=ALU.add), reads=[bXt, bt3], writes=[bt3])
                    layer_norm(es, "3", t3, bt3, xn, bxn, tmp)
                    mul_add(xn, bxn, xn, bxn, LNG[0], LNG[1], LNB[0], LNB[1])
                    dma("pool", lambda e: e.dma_start(out=X[i * 128:(i + 1) * 128, :], in_=xn[:]), reads=[bxn])
                    layer_norm(es, "3", xn, bxn, h2, bh2, tmp)
                    mul_add(h2, bh2, h2, bh2, SC2[ck][0], SC2[ck][1], SH2[ck][0], SH2[ck][1])
                    dma("pool", lambda e: e.dma_start(out=H2[i * 128:(i + 1) * 128, :], in_=h2[:]), reads=[bh2])

                def back(ii):
                    i = tiles3[ii]
                    h2, bh2 = h2s[ii % 2]; h2T, bh2T = h2Ts[ii % 2]
                    transpose8(h2, bh2, pT2, bpT2, h2T, bh2T)
                    for k in range(8):
                        op("pe", lambda e: e.matmul(pL[:], lhsT=h2T[:, k, :], rhs=RW[:, k, :], start=(k == 0), stop=(k == 7)), reads=[bh2T, bRW], writes=[bpL])
                    op("dve", lambda e: e.tensor_tensor(out=LOG[:, i, :], in0=pL[:], in1=RBi[:], op=ALU.add), reads=[bpL, bRBi], writes=[bLOG])

                front(0)
                for ii in range(len(tiles3)):
                    if ii + 1 < len(tiles3):
                        front(ii + 1)
                    back(ii)
            P.barrier()
            if debug == "p3":
                break
            tiles4 = tiles3
            with ExitStack() as es:
                sb, ps = pools(es)
                UTt, bUT = load(es, "UTt", cst["UT"], [128, 128]); ONES, bONES = load(es, "ONESt", cst["ONES"], [128, 128])
                JV, bJV = load(es, "JVt", cst["JV"], [128, NBLK]); KP, bKP = load(es, "KPt", cst["KP"], [128, 8])
                SL4i, bSL4i = sb("SL4i", [128, NT, 4], I32); G4, bG4 = sb("G4", [128, NT, 4])
                IDX, bIDX = sb("IDX", [128, NBLK, 8], I32); BEi, bBEi = sb("BEi", [128, NBLK], I32)
                with ExitStack() as es2:
                    sb2, ps2 = pools(es2)
                    MASK, bMASK = sb2("MASK", [128, NT, NEXP]); GT, bGT = sb2("GT", [128, NT, NEXP]); POS, bPOS = sb2("POS", [128, NT, NEXP])
                    BASE, bBASE = sb2("BASE", [128, NEXP]); t8, bt8 = sb2("t8", [128, 8]); ng, bng = sb2("ng", [128, 1]); ss, bss = sb2("ss", [128, 1])
                    ee, bee = sb2("ee", [128, NEXP]); oh, boh = sb2("oh", [128, NEXP])
                    PADD, bPADD = sb2("PADD", [128, NEXP]); PEND, bPEND = sb2("PEND", [128, NEXP]); PST, bPST = sb2("PST", [128, NEXP])
                    SLOT, bSLOT = sb2("SLOT", [128, NEXP]); s4f, bs4f = sb2("s4f", [128, 8])
                    BE, bBE = sb2("BE", [128, NBLK]); IDXf, bIDXf = sb2("IDXf", [128, NBLK, 8]); ONE32, bONE32 = sb2("ONE32", [128, NEXP])
                    pPos, bpPos = ps2("pPos", [128, NEXP]); pCS, bpCS = ps2("pCS", [128, NEXP])
                    op("pool", lambda e: e.memset(BASE[:], 0.0), writes=[bBASE])
                    op("pool", lambda e: e.memset(ONE32[:], 1.0), writes=[bONE32])
                    for i in tiles4:
                        lg = LOG[:, i, :]
                        op("dve", lambda e: e.max(out=t8[:], in_=lg), reads=[bLOG], writes=[bt8])
                        op("dve", lambda e: e.tensor_scalar(out=MASK[:, i, :], in0=lg, scalar1=t8[:, 3:4], scalar2=None, op0=ALU.is_ge), reads=[bLOG, bt8], writes=[bMASK])
                        op("dve", lambda e: e.tensor_scalar(out=ng[:], in0=t8[:, 0:1], scalar1=-1.0, scalar2=None, op0=ALU.mult), reads=[bt8], writes=[bng])
                        op("act", lambda e: e.activation(out=ee[:], in_=lg, func=AF.Exp, bias=ng[:, 0:1], scale=1.0), reads=[bLOG, bng], writes=[bee])
                        op("dve", lambda e: e.tensor_tensor(out=ee[:], in0=ee[:], in1=MASK[:, i, :], op=ALU.mult), reads=[bee, bMASK], writes=[bee])
                        op("dve", lambda e: e.reduce_sum(out=ss[:], in_=ee[:], axis=AX.X), reads=[bee], writes=[bss])
                        op("dve", lambda e: e.reciprocal(out=ss[:], in_=ss[:]), reads=[bss], writes=[bss])
                        op("dve", lambda e: e.tensor_scalar(out=GT[:, i, :], in0=ee[:], scalar1=ss[:, 0:1], scalar2=None, op0=ALU.mult), reads=[bee, bss], writes=[bGT])
                        op("pe", lambda e: e.matmul(pPos[:], lhsT=UTt[:], rhs=MASK[:, i, :], start=True, stop=True), reads=[bUT, bMASK], writes=[bpPos])
                        op("pe", lambda e: e.matmul(pCS[:], lhsT=ONES[:], rhs=MASK[:, i, :], start=True, stop=True), reads=[bONES, bMASK], writes=[bpCS])
                        op("dve", lambda e: e.tensor_tensor(out=POS[:, i, :], in0=pPos[:], in1=BASE[:], op=ALU.add), reads=[bpPos, bBASE], writes=[bPOS])
                        op("dve", lambda e: e.tensor_tensor(out=BASE[:], in0=pCS[:], in1=BASE[:], op=ALU.add), reads=[bpCS, bBASE], writes=[bBASE])
                    PADi, bPADi = sb2("PADi", [128, NEXP], I32)
                    op("dve", lambda e: e.tensor_scalar(out=PADD[:], in0=BASE[:], scalar1=127.0, scalar2=None, op0=ALU.add), reads=[bBASE], writes=[bPADD])
                    op("dve", lambda e: e.tensor_copy(out=PADi[:], in_=PADD[:]), reads=[bPADD], writes=[bPADi])
                    op("dve", lambda e: e.tensor_single_scalar(out=PADi[:], in_=PADi[:], scalar=7, op=ALU.arith_shift_right), reads=[bPADi], writes=[bPADi])
                    op("dve", lambda e: e.tensor_single_scalar(out=PADi[:], in_=PADi[:], scalar=7, op=ALU.logical_shift_left), reads=[bPADi], writes=[bPADi])
                    op("dve", lambda e: e.tensor_copy(out=PADD[:], in_=PADi[:]), reads=[bPADi], writes=[bPADD])
                    op("dve", lambda e: e.tensor_tensor_scan(out=PEND[:], data0=ONE32[:], data1=PADD[:], initial=0.0, op0=ALU.mult, op1=ALU.add), reads=[bONE32, bPADD], writes=[bPEND])
                    op("dve", lambda e: e.tensor_tensor(out=PST[:], in0=PEND[:], in1=PADD[:], op=ALU.subtract), reads=[bPEND, bPADD], writes=[bPST])
                    op("dve", lambda e: e.tensor_scalar(out=PST[:], in0=PST[:], scalar1=1.0, scalar2=None, op0=ALU.add), reads=[bPST], writes=[bPST])
                    for i in tiles4:
                        op("dve", lambda e: e.tensor_tensor(out=SLOT[:], in0=POS[:, i, :], in1=PST[:], op=ALU.add), reads=[bPOS, bPST], writes=[bSLOT])
                        op("dve", lambda e: e.tensor_tensor(out=SLOT[:], in0=SLOT[:], in1=MASK[:, i, :], op=ALU.mult), reads=[bSLOT, bMASK], writes=[bSLOT])
                        op("dve", lambda e: e.tensor_scalar(out=SLOT[:], in0=SLOT[:], scalar1=-1.0, scalar2=None, op0=ALU.add), reads=[bSLOT], writes=[bSLOT])
                        op("dve", lambda e: e.max(out=s4f[:], in_=SLOT[:]), reads=[bSLOT], writes=[bs4f])
                        op("dve", lambda e: e.tensor_copy(out=SL4i[:, i, :], in_=s4f[:, 0:4]), reads=[bs4f], writes=[bSL4i])
                        for k in range(4):
                            op("dve", lambda e: e.tensor_scalar(out=oh[:], in0=SLOT[:], scalar1=s4f[:, k:k + 1], scalar2=None, op0=ALU.is_equal), reads=[bSLOT, bs4f], writes=[boh])
                            op("dve", lambda e: e.tensor_tensor(out=oh[:], in0=oh[:], in1=GT[:, i, :], op=ALU.mult), reads=[boh, bGT], writes=[boh])
                            op("dve", lambda e: e.reduce_sum(out=G4[:, i, k:k + 1], in_=oh[:], axis=AX.X), reads=[boh], writes=[bG4])
                    op("pool", lambda e: e.memset(BE[:], 0.0), writes=[bBE])
                    for ex in range(NEXP):
                        op("dve", lambda e: e.scalar_tensor_tensor(out=BE[:], in0=JV[:], scalar=PEND[:, ex:ex + 1], in1=BE[:], op0=ALU.is_ge, op1=ALU.add), reads=[bJV, bPEND, bBE], writes=[bBE])
                    op("dve", lambda e: e.tensor_scalar(out=BE[:], in0=BE[:], scalar1=float(NEXP - 1), scalar2=None, op0=ALU.min), reads=[bBE], writes=[bBE])
                    BE2, bBE2 = sb2("BE2", [128, NBLK]); SAME, bSAME = sb2("SAME", [128, NBLK])
                    op("dve", lambda e: e.tensor_copy(out=BE2[:], in_=BE[:]), reads=[bBE], writes=[bBE2])
                    op("pool", lambda e: e.memset(SAME[:], 0.0), writes=[bSAME])
                    op("dve", lambda e: e.tensor_tensor(out=SAME[:, 1:NBLK], in0=BE[:, 1:NBLK], in1=BE2[:, 0:NBLK - 1], op=ALU.is_equal), reads=[bBE, bBE2, bSAME], writes=[bSAME])
                    op("dve", lambda e: e.tensor_scalar(out=SAME[:], in0=SAME[:], scalar1=100000.0, scalar2=None, op0=ALU.mult), reads=[bSAME], writes=[bSAME])
                    op("dve", lambda e: e.tensor_tensor(out=BE2[:], in0=BE2[:], in1=SAME[:], op=ALU.add), reads=[bBE2, bSAME], writes=[bBE2])
                    op("dve", lambda e: e.tensor_copy(out=BEi[:], in_=BE2[:]), reads=[bBE2], writes=[bBEi])
                    op("dve", lambda e: e.scalar_tensor_tensor(out=BE[:], in0=BE[:], scalar=1024.0, in1=SAME[:], op0=ALU.mult, op1=ALU.add), reads=[bBE, bSAME], writes=[bBE])
                    op("dve", lambda e: e.tensor_tensor(out=IDXf[:], in0=BE[:].unsqueeze(2).to_broadcast([128, NBLK, 8]), in1=KP[:].unsqueeze(1).to_broadcast([128, NBLK, 8]), op=ALU.add),
                       reads=[bBE, bKP], writes=[bIDXf])
                    op("dve", lambda e: e.tensor_copy(out=IDX[:], in_=IDXf[:]), reads=[bIDXf], writes=[bIDX])
                P.barrier(new_epoch=False)
                if debug == "p4a":
                    dma("sp", lambda e: e.dma_start(out=DBG[:, 0:NT * 4], in_=G4[:].rearrange("p a b -> p (a b)")), reads=[bG4])
                    dma("sp", lambda e: e.dma_start(out=DBGI[:, 0:NT * 4], in_=SL4i[:].rearrange("p a b -> p (a b)")), reads=[bSL4i])
                    dma("sp", lambda e: e.dma_start(out=DBGI[:, NT * 4:NT * 4 + NBLK], in_=BEi[:]), reads=[bBEi])
                    P.barrier(new_epoch=False)
                    break
                with ExitStack() as es2:
                    sb2, ps2 = pools(es2)
                    hts = [sb2(f"h4t{i}", [128, D]) for i in range(3)]
                    for ii, i in enumerate(tiles4):
                        Ht, bHt = hts[ii % 3]
                        dma("sp", lambda e: e.dma_start(out=Ht[:], in_=H2[i * 128:(i + 1) * 128, :]), writes=[bHt])
                        for k in range(4):
                            dma("pool", lambda e: e.indirect_dma_start(out=XS[:, :], out_offset=bass.IndirectOffsetOnAxis(ap=SL4i[:, i, k:k + 1], axis=0), in_=Ht[:], in_offset=None), reads=[bHt, bSL4i])
                P.barrier(new_epoch=False)
                if debug == "p4b":
                    break
                with ExitStack() as es2:
                    sb2, ps2 = pools(es2)
                    W1, _ = sb2("W1", [128, 8, 2048]); W2, _ = sb2("W2", [128, 8, D])
                    bW1 = [Buf() for _ in range(8)]; bW2 = [Buf() for _ in range(8)]
                    B1, bB1 = sb2("B1", [128, 2048]); B2, bB2 = sb2("B2", [128, D])
                    xss = [sb2(f"xs{i}", [128, D]) for i in range(2)]; xTs = [sb2(f"xT4_{i}", [128, 8, 128]) for i in range(2)]
                    ubs = [sb2(f"ub{i}", [128, 1024]) for i in range(2)]; glus = [sb2(f"glu{i}", [128, 512]) for i in range(2)]
                    lins = [sb2(f"lin{i}", [128, 512]) for i in range(2)]; sigs = [sb2(f"sig{i}", [128, 512]) for i in range(2)]
                    aTs = [sb2(f"aT4_{i}", [128, 4, 128]) for i in range(2)]; ys = [sb2(f"ys{i}", [128, D]) for i in range(2)]
                    pT, bpT = ps2("pT4", [128, 1024]); pUs = [ps2(f"pU4_{i}", [128, 1024]) for i in range(2)]; pY, bpY = ps2("pY4", [128, 1024])
                    w1v = exp_w1.rearrange("l e k n -> (l e k) n"); w2v = exp_w2.rearrange("l e k n -> (l e k) n")
                    b1v = exp_b1.rearrange("l e n -> (l e) n"); b2v = exp_b2.rearrange("l e n -> (l e) n")
                    regW = nc.gpsimd.to_reg(NEXP * D - 1); regB = nc.gpsimd.to_reg(NEXP - 1)

                    def gW1(j):
                        for k in range(8):
                            dma("pool", lambda e: e.indirect_dma_start(out=W1[:, k, :], out_offset=None, in_=w1v, in_offset=bass.IndirectOffsetOnAxis(ap=IDX[:, j, k:k + 1], axis=0),
                                                                       element_offset=l * NEXP * D * 2048, bounds_check=regW, oob_is_err=False), reads=[bIDX], writes=[bW1[k]])

                    def gB1(j):
                        dma("pool", lambda e: e.indirect_dma_start(out=B1[:], out_offset=None, in_=b1v, in_offset=bass.IndirectOffsetOnAxis(ap=BEi[:, j:j + 1], axis=0),
                                                                   element_offset=l * NEXP * 2048, bounds_check=regB, oob_is_err=False), reads=[bBEi], writes=[bB1])

                    def gW2(j):
                        for k in range(8):
                            dma("pool", lambda e: e.indirect_dma_start(out=W2[:, k, :], out_offset=None, in_=w2v, in_offset=bass.IndirectOffsetOnAxis(ap=IDX[:, j, k:k + 1], axis=0),
                                                                       element_offset=l * NEXP * D * D, bounds_check=regW, oob_is_err=False), reads=[bIDX], writes=[bW2[k]])
                        dma("pool", lambda e: e.indirect_dma_start(out=B2[:], out_offset=None, in_=b2v, in_offset=bass.IndirectOffsetOnAxis(ap=BEi[:, j:j + 1], axis=0),
                                                                   element_offset=l * NEXP * D, bounds_check=regB, oob_is_err=False), reads=[bBEi], writes=[bB2])

                    def ldx(j):
                        Xs, bXs = xss[j % 2]
                        dma("sp", lambda e: e.dma_start(out=Xs[:], in_=XS[j * 128:(j + 1) * 128, :]), writes=[bXs])

                    def tx(j):
                        Xs, bXs = xss[j % 2]; xT, bxT = xTs[j % 2]
                        transpose8(Xs, bXs, pT, bpT, xT, bxT)

                    def swiglu(hf):
                        pU, bpU = pUs[hf]; ub, bub = ubs[hf]; glu, bglu = glus[hf]; lin, blin = lins[hf]; sig, bsig = sigs[hf]
                        op("dve", lambda e: e.tensor_tensor(out=ub[:], in0=pU[:], in1=B1[:, hf * 1024:(hf + 1) * 1024], op=ALU.add), reads=[bpU, bB1], writes=[bub])
                        ubv = ub[:].rearrange("p (f t) -> p f t", t=2)
                        op("dve", lambda e: e.tensor_scalar(out=glu[:], in0=ubv[:, :, 0], scalar1=7.0, scalar2=None, op0=ALU.min), reads=[bub], writes=[bglu])
                        op("dve", lambda e: e.tensor_scalar(out=lin[:], in0=ubv[:, :, 1], scalar1=7.0, scalar2=-7.0, op0=ALU.min, op1=ALU.max), reads=[bub], writes=[blin])
                        op("act", lambda e: e.activation(out=sig[:], in_=glu[:], func=AF.Sigmoid, scale=1.702), reads=[bglu], writes=[bsig])
                        op("dve", lambda e: e.tensor_tensor(out=glu[:], in0=glu[:], in1=sig[:], op=ALU.mult), reads=[bglu, bsig], writes=[bglu])
                        op("dve", lambda e: e.scalar_tensor_tensor(out=lin[:], in0=lin[:], scalar=1.0, in1=glu[:], op0=ALU.add, op1=ALU.mult), reads=[blin, bglu], writes=[blin])

                    def ta(hf):
                        lin, blin = lins[hf]; aT, baT = aTs[hf]
                        for k in range(4):
                            op("pe", lambda e: e.transpose(out=pT[:, k * 128:(k + 1) * 128], in_=lin[:, k * 128:(k + 1) * 128], identity=ID[:]), reads=[blin, bID], writes=[bpT])
                        op("act", lambda e: e.copy(out=aT[:].rearrange("p a b -> p (a b)"), in_=pT[:, 0:512]), reads=[bpT], writes=[baT])

                    NB = len(tiles4) * 4 + NEXP
                    gW1(0); gB1(0); gW2(0); ldx(0)
                    if NB > 1:
                        ldx(1)
                    tx(0)
                    for j in range(NB):
                        xT, bxT = xTs[j % 2]; Ys, bYs = ys[j % 2]
                        for hf in range(2):
                            pU, bpU = pUs[hf]
                            for n in range(2):
                                for k in range(8):
                                    op("pe", lambda e: e.matmul(pU[:, n * 512:(n + 1) * 512], lhsT=xT[:, k, :], rhs=W1[:, k, (hf * 2 + n) * 512:(hf * 2 + n + 1) * 512],
                                                                start=(k == 0), stop=(k == 7)), reads=[bxT, bW1[k]], writes=[bpU])
                        if j + 1 < NB:
                            gW1(j + 1)
                            tx(j + 1)
                        if j + 2 < NB:
                            ldx(j + 2)
                        swiglu(0)
                        ta(0)
                        for n in range(2):
                            for k in range(4):
                                op("pe", lambda e: e.matmul(pY[:, n * 512:(n + 1) * 512], lhsT=aTs[0][0][:, k, :], rhs=W2[:, k, n * 512:(n + 1) * 512], start=(k == 0), stop=False),
                                   reads=[aTs[0][1], bW2[k]], writes=[bpY])
                        swiglu(1)
                        if j + 1 < NB:
                            gB1(j + 1)
                        ta(1)
                        for n in range(2):
                            for k in range(4):
                                op("pe", lambda e: e.matmul(pY[:, n * 512:(n + 1) * 512], lhsT=aTs[1][0][:, k, :], rhs=W2[:, 4 + k, n * 512:(n + 1) * 512], start=False, stop=(k == 3)),
                                   reads=[aTs[1][1], bW2[4 + k]], writes=[bpY])
                        op("dve", lambda e: e.tensor_tensor(out=Ys[:], in0=pY[:], in1=B2[:], op=ALU.add), reads=[bpY, bB2], writes=[bYs])
                        if j + 1 < NB:
                            gW2(j + 1)
                        dma("sp", lambda e: e.dma_start(out=YS[j * 128:(j + 1) * 128, :], in_=Ys[:]), reads=[bYs])
                P.barrier(new_epoch=False)
                if debug == "p4c":
                    dma("sp", lambda e: e.dma_start(out=DBGI[:, 0:NT * 4], in_=SL4i[:].rearrange("p a b -> p (a b)")), reads=[bSL4i])
                    P.barrier(new_epoch=False)
                    break
                with ExitStack() as es2:
                    sb2, ps2 = pools(es2)
                    G2 = [modv(es2, "G2L", 0, 5), modv(es2, "G2C", 1, 5)]
                    LNG = load(es2, "LNG2", bc(ln_g[l, 1]), [128, D]); LNB = load(es2, "LNB2", bc(ln_b[l, 1]), [128, D])
                    ygs = [[sb2(f"yg{i}_{k}", [128, D]) for k in range(4)] for i in range(2)]
                    xts = [sb2(f"x5t{i}", [128, D]) for i in range(2)]
                    f, bf = sb2("f5", [128, D]); xo = [sb2(f"xo{i}", [128, D]) for i in range(2)]
                    tmp = ln_tmp(es2, "5")
                    for ii, i in enumerate(tiles4):
                        ck = 1 if i < 2 else 0
                        YG = ygs[ii % 2]; Xt, bXt = xts[ii % 2]; Xo, bXo = xo[ii % 2]
                        for k in range(4):
                            dma("pool", lambda e: e.indirect_dma_start(out=YG[k][0][:], out_offset=None, in_=YS[:, :], in_offset=bass.IndirectOffsetOnAxis(ap=SL4i[:, i, k:k + 1], axis=0)),
                                reads=[bSL4i], writes=[YG[k][1]])
                        dma("sp", lambda e: e.dma_start(out=Xt[:], in_=X[i * 128:(i + 1) * 128, :]), writes=[bXt])
                        op("dve", lambda e: e.tensor_scalar(out=f[:], in0=YG[0][0][:], scalar1=G4[:, i, 0:1], scalar2=None, op0=ALU.mult), reads=[YG[0][1], bG4], writes=[bf])
                        for k in range(1, 4):
                            op("dve", lambda e: e.scalar_tensor_tensor(out=f[:], in0=YG[k][0][:], scalar=G4[:, i, k:k + 1], in1=f[:], op0=ALU.mult, op1=ALU.add), reads=[YG[k][1], bG4, bf], writes=[bf])
                        op("dve", lambda e: e.tensor_tensor(out=f[:], in0=f[:], in1=G2[ck][0][:], op=ALU.mult), reads=[bf, G2[ck][1]], writes=[bf])
                        op("dve", lambda e: e.scalar_tensor_tensor(out=f[:], in0=Xt[:], scalar=ALPHA, in1=f[:], op0=ALU.mult, op1=ALU.add), reads=[bXt, bf], writes=[bf])
                        layer_norm(es2, "5", f, bf, Xo, bXo, tmp)
                        mul_add(Xo, bXo, Xo, bXo, LNG[0], LNG[1], LNB[0], LNB[1])
                        if l == 1:
                            dma("sp", lambda e: e.dma_start(out=out[(i - 2) * 128:(i - 1) * 128, :], in_=Xo[:]), reads=[bXo])
                        else:
                            dma("sp", lambda e: e.dma_start(out=X[i * 128:(i + 1) * 128, :], in_=Xo[:]), reads=[bXo])
            P.barrier()
        P.finish()
    return nc


def make_in_map(inp, b, N):
    f = lambda a: np.ascontiguousarray(np.asarray(a, dtype=np.float32))
    m = {}
    m["xin"] = f(np.concatenate([inp["ctx"][b], inp["x"][b]], 0))
    cc = np.stack([np.asarray(inp["c"][b]).reshape(8, 128).T, np.asarray(inp["c_ctx"]).reshape(8, 128).T], -1)
    m["cc"] = f(cc)
    for k in ("w_mod", "b_mod", "w_in", "pool_w", "pool_scale", "attn_sink", "ret_decay", "w_out", "ln_g", "ln_b",
              "router_w", "router_b", "exp_w1", "exp_b1", "exp_w2", "exp_b2"):
        m[k] = f(inp[k])
    hc = host_consts(N)
    NT = N // 128 + 2
    NBLK = (NT * 128 * 4) // 128 + NEXP
    hc["JV"] = (np.arange(NBLK, dtype=np.float32) * 128)[None, :].repeat(128, 0)
    for k, v in hc.items():
        m["c_" + k] = f(v)
    return m


_CACHE = {}


def kernel(**inputs):
    inp = {k: np.asarray(v) for k, v in inputs.items()}
    B, N, _ = inp["x"].shape
    if N not in _CACHE:
        _CACHE[N] = build_program(N)
    nc = _CACHE[N]
    n_cores = 8
    maps = [make_in_map(inp, b, N) for b in range(B)]
    in_maps = [maps[i % B] for i in range(n_cores)]
    res = run_bass_kernel_spmd(nc, in_maps, core_ids=list(range(n_cores)))
    return np.stack([np.asarray(res.results[b]["out"], dtype=np.float32) for b in range(B)], 0)
```

```python
import numpy as np
import os
from contextlib import ExitStack
import concourse.bass as bass
import concourse.mybir as mybir
from concourse.bass_utils import run_bass_kernel_spmd

F32 = mybir.dt.float32
I32 = mybir.dt.int32
AF = mybir.ActivationFunctionType
ALU = mybir.AluOpType
AX = mybir.AxisListType


class Buf:
    __slots__ = ("w", "r")

    def __init__(self):
        self.w = None
        self.r = {}


class Prog:
    ENGS = ("pe", "act", "pool", "dve", "sp")

    def __init__(self, nc, es, ring=12):
        self.nc = nc
        self.es = es
        self.eng = {"pe": nc.tensor, "act": nc.scalar, "pool": nc.gpsimd, "dve": nc.vector, "sp": nc.sync}
        self.sem = {}
        self.epoch = 0
        self.nsem = 0
        self._new_epoch_sems()
        self.ring = {}
        self.rr = {}
        for q in ("sp", "pool", "act"):
            self.ring[q] = []
            for i in range(ring):
                key = ("dma", q, i)
                self.sem[key] = self._mk(f"d_{q}_{i}")
                self.ring[q].append([key, 0])
            self.rr[q] = 0

    def _mk(self, name):
        self.nsem += 1
        return self.es.enter_context(self.nc.semaphore(name))

    def _new_epoch_sems(self):
        self.cnt = {}
        self.waited = {e: {} for e in self.ENGS}
        for e in self.ENGS:
            key = ("eng", e, self.epoch)
            self.sem[key] = self._mk(f"e_{e}_{self.epoch}")
            self.cnt[e] = 0

    def ekey(self, e):
        return ("eng", e, self.epoch)

    def _deps(self, reads, writes):
        deps = []
        for b in reads:
            if b.w is not None:
                deps.append(b.w)
        for b in writes:
            if b.w is not None:
                deps.append(b.w)
            deps.extend(b.r.values())
        return deps

    def _waits(self, e, deps):
        for (s, v) in deps:
            if s[0] == "eng":
                if s[2] != self.epoch:
                    continue
                if s[1] == "pe" and e == "pe":
                    continue
            if self.waited[e].get(s, 0) >= v:
                continue
            self.waited[e][s] = v
            self.eng[e].wait_ge(self.sem[s], v)

    def _mark(self, tok, reads, writes):
        for b in reads:
            b.r[tok[0]] = tok
        for b in writes:
            b.w = tok
            b.r = {}

    def op(self, e, fn, reads=(), writes=()):
        self._waits(e, self._deps(reads, writes))
        self.cnt[e] += 1
        k = self.ekey(e)
        tok = (k, self.cnt[e])
        fn(self.eng[e]).then_inc(self.sem[k], 1)
        self._mark(tok, reads, writes)
        return tok

    def dma(self, q, fn, reads=(), writes=(), serial=False):
        ring = self.ring[q]
        i = self.rr[q]
        self.rr[q] = (i + 1) % len(ring)
        key, n = ring[i]
        deps = self._deps(reads, writes)
        if serial and getattr(self, "last_serial", None) is not None:
            deps.append(self.last_serial)
        if n > 0:
            deps.append((key, 16 * n))
        self._waits(q, deps)
        ring[i][1] = n + 1
        tok = (key, 16 * (n + 1))
        fn(self.eng[q]).then_inc(self.sem[key], 16)
        self._mark(tok, reads, writes)
        if serial:
            self.last_serial = tok
        return tok

    def barrier(self, new_epoch=True):
        toks = [(self.ekey(e), self.cnt[e]) for e in self.ENGS if self.cnt[e] > 0]
        for q in self.ring:
            for key, n in self.ring[q]:
                if n > 0:
                    toks.append((key, 16 * n))
        for e in self.ENGS:
            saved = dict(self.waited[e])
            for (s, v) in toks:
                if s[0] == "eng" and s[1] == e:
                    pass
                if self.waited[e].get(s, 0) >= v:
                    continue
                self.waited[e][s] = v
                self.eng[e].wait_ge(self.sem[s], v)
        if new_epoch:
            self.epoch += 1
            dma_waited = {e: {s: v for s, v in self.waited[e].items() if s[0] == "dma"} for e in self.ENGS}
            self._new_epoch_sems()
            self.waited = dma_waited

    def finish(self):
        self.barrier(new_epoch=False)


D = 1024
CTX = 256
ALPHA = 4 ** 0.25
EPS = 1e-6
NEG = -1e30
NEXP = 32


def host_consts(N):
    f64 = np.float64
    c = {}
    c["ident"] = np.eye(128, dtype=np.float32)
    cc = np.arange(64)
    ang = 2 * np.pi * np.outer(cc, cc) / 64
    BC = np.zeros((128, 128), f64); BS = np.zeros((128, 128), f64)
    for g in range(2):
        BC[g * 64:(g + 1) * 64, g * 64:(g + 1) * 64] = np.cos(ang)
        BS[g * 64:(g + 1) * 64, g * 64:(g + 1) * 64] = np.sin(ang)
    c["BC"] = BC.astype(np.float32); c["BS"] = BS.astype(np.float32)
    for nm, Nn in (("L", N), ("C", CTX)):
        N1 = Nn // 128
        a1 = 2 * np.pi * np.outer(np.arange(N1), np.arange(N1)) / N1
        C1, S1 = np.cos(a1), np.sin(a1)
        c["FAP" + nm] = np.concatenate([C1, S1], 1).astype(np.float32)
        c["FAQ" + nm] = np.concatenate([-S1, C1], 1).astype(np.float32)
        at = 2 * np.pi * np.outer(np.arange(128), np.arange(N1)) / Nn
        c["TWC" + nm] = np.cos(at).astype(np.float32)
        c["TWS" + nm] = np.sin(at).astype(np.float32)
        a2 = 2 * np.pi * np.outer(np.arange(128), np.arange(128)) / 128
        sc = 1.0 / np.sqrt(Nn * 64.0)
        c["FBC" + nm] = (np.cos(a2) * sc).astype(np.float32)
        c["FBS" + nm] = (-np.sin(a2) * sc).astype(np.float32)
    PB = np.zeros((128, 20, 128), f64)
    for g, w in enumerate((2, 4, 8, 16)):
        h = w // 2
        for t in range(128):
            for s in range(t - h, t + h):
                if 0 <= s < 128:
                    PB[s, g * 5 + 0, t] += 1.0 / w
                    cnt_first = min(t + h, 10 ** 9) - max(t - h, 0)
                    PB[s, g * 5 + 1, t] += 1.0 / cnt_first
                    cnt_last = min(t + h, 128) - (t - h)
                    PB[s, g * 5 + 2, t] += 1.0 / cnt_last
                elif s < 0:
                    PB[s + 128, g * 5 + 3, t] += 1.0 / w
                else:
                    PB[s - 128, g * 5 + 4, t] += 1.0 / w
            for k in range(3):
                PB[t, g * 5 + k, t] -= 1.0
    c["PB"] = PB.astype(np.float32)
    inv = 1.0 / (10000.0 ** (np.arange(0, 32, 2, dtype=np.float32) / np.float32(32)))
    t = np.arange(N)
    row = (t // 64).astype(np.float32); col = (t % 64).astype(np.float32)
    angr = (row[:, None] * inv[None, :]).astype(np.float32)
    angc = (col[:, None] * inv[None, :]).astype(np.float32)
    c["ROPC"] = np.concatenate([np.cos(angr.astype(f64)), np.cos(angc.astype(f64))], 1).astype(np.float32)
    c["ROPS"] = np.concatenate([np.sin(angr.astype(f64)), np.sin(angc.astype(f64))], 1).astype(np.float32)
    r = np.arange(128)
    c["MASKP"] = np.where(r[None, :] >= r[:, None], 0.0, NEG).astype(np.float32)
    c["MASKN"] = np.where(r[None, :] <= r[:, None], 0.0, NEG).astype(np.float32)
    posz = np.zeros((128, 4, 2), np.float32)
    posz[:, :, 0] = (127 - r)[:, None]; posz[:, :, 1] = r[:, None]
    c["POSZ"] = posz
    posxi = np.zeros((128, 128), np.float32)
    posxi[0:64, :] = (r + 1)[None, :]; posxi[64:128, :] = (128 - r)[None, :]
    c["POSXI"] = posxi
    dq = r[None, :] - r[:, None]
    c["PF"] = np.maximum(dq, 0).astype(np.float32); c["MF"] = (dq >= 0).astype(np.float32)
    c["PBK"] = np.maximum(-dq, 0).astype(np.float32); c["MB"] = (dq <= 0).astype(np.float32)
    c["UT"] = (r[:, None] < r[None, :]).astype(np.float32)
    c["ONES"] = np.ones((128, 128), np.float32)
    kp = (np.arange(8)[None, :] * 128 + r[:, None]).astype(np.float32)
    c["KP"] = kp
    return c


def build_program(N, debug=False):
    NL = N // 128
    NT = NL + 2
    T = NT * 128
    NBLK = (T * 4) // 128 + NEXP
    PR = NBLK * 128
    nc = bass.Bass("TRN2", target_bir_lowering=False)
    dr = lambda name, shape, dt=F32, kind="ExternalInput": nc.dram_tensor(name, shape, dt, kind=kind).ap()
    xin = dr("xin", [T, D])
    cc_in = dr("cc", [128, 8, 2])
    w_mod = dr("w_mod", [2, D, 6 * D]); b_mod = dr("b_mod", [2, 6 * D])
    w_in = dr("w_in", [2, D, 2048]); pool_w = dr("pool_w", [2, 4, 64, 64]); pool_scale = dr("pool_scale", [2, 256])
    attn_sink = dr("attn_sink", [2, 4]); ret_decay = dr("ret_decay", [2, 2, 4]); w_out = dr("w_out", [2, D, D])
    ln_g = dr("ln_g", [2, 2, D]); ln_b = dr("ln_b", [2, 2, D])
    router_w = dr("router_w", [2, D, NEXP]); router_b = dr("router_b", [2, NEXP])
    exp_w1 = dr("exp_w1", [2, NEXP, D, 2048]); exp_b1 = dr("exp_b1", [2, NEXP, 2048])
    exp_w2 = dr("exp_w2", [2, NEXP, D, D]); exp_b2 = dr("exp_b2", [2, NEXP, D])
    hc = host_consts(N)
    jv = (np.arange(NBLK, dtype=np.float32) * 128)[None, :].repeat(128, 0)
    hc["JV"] = jv
    cst = {k: dr("c_" + k, list(v.shape)) for k, v in hc.items()}
    out = dr("out", [N, D], kind="ExternalOutput")
    ik = "ExternalOutput" if debug else "Internal"
    X = dr("X", [T, D], kind=ik)
    U = dr("U", [T, 2048], kind=ik)
    PTD = dr("PTD", [256, T], kind=ik); QTD = dr("QTD", [256, T], kind=ik)
    M = dr("M", [T, D], kind=ik)
    H2 = dr("H2", [T, D], kind=ik)
    MODS = dr("MODS", [2, 6 * D], kind=ik)
    XS = dr("XS", [PR, D], kind=ik); YS = dr("YS", [PR, D], kind=ik)
    DBG = dr("DBG", [128, 4096], kind=ik); DBGI = dr("DBGI", [128, 4096], I32, kind=ik)

    with ExitStack() as es0:
        P = Prog(nc, es0)
        op, dma = P.op, P.dma

        uid = [0]

        def pools(es):
            def sb(name, shape, dt=F32):
                uid[0] += 1
                return (es.enter_context(nc.sbuf_tensor(f"{name}_{uid[0]}", shape, dt)), Buf())

            def ps(name, shape, dt=F32):
                uid[0] += 1
                free = ((shape[1] + 511) // 512) * 512
                t = es.enter_context(nc.psum_tensor(f"{name}_{uid[0]}", [shape[0], free], dt))
                return (t[:, 0:shape[1]], Buf())
            return sb, ps

        def load(es, name, src, shape, q="sp"):
            t, b = pools(es)[0](name, shape)
            dma(q, lambda e: e.dma_start(out=t[:], in_=src), writes=[b])
            return t, b

        def bc(v):
            return v.partition_broadcast(128)

        sb0, _ = pools(es0)
        ID, bID = load(es0, "ID", cst["ident"], [128, 128])
        LOG, bLOG = sb0("LOG", [128, NT, NEXP])

        def layer_norm(es, tag, src, bsrc, dst, bdst, tmp):
            st, bst = tmp["st"]; mv, bmv = tmp["mv"]; rs, brs = tmp["rs"]
            for j in range(2):
                op("dve", lambda e: e.bn_stats(out=st[:, j, :], in_=src[:, j * 512:(j + 1) * 512]), reads=[bsrc], writes=[bst])
            op("dve", lambda e: e.bn_aggr(out=mv[:], in_=st[:].rearrange("p a b -> p (a b)")), reads=[bst], writes=[bmv])
            op("act", lambda e: e.activation(out=rs[:], in_=mv[:, 1:2], func=AF.Sqrt, bias=EPS, scale=1.0), reads=[bmv], writes=[brs])
            op("dve", lambda e: e.reciprocal(out=rs[:], in_=rs[:]), reads=[brs], writes=[brs])
            op("dve", lambda e: e.tensor_scalar(out=dst[:], in0=src[:], scalar1=mv[:, 0:1], scalar2=rs[:, 0:1], op0=ALU.subtract, op1=ALU.mult),
               reads=[bsrc, bmv, brs], writes=[bdst])

        def ln_tmp(es, tag):
            sb, _ = pools(es)
            return {"st": sb("st" + tag, [128, 2, 6]), "mv": sb("mv" + tag, [128, 2]), "rs": sb("rs" + tag, [128, 1])}

        def transpose8(src, bsrc, pT, bpT, dstT, bdstT, eng="act"):
            for k in range(8):
                op("pe", lambda e: e.transpose(out=pT[:, k * 128:(k + 1) * 128], in_=src[:, k * 128:(k + 1) * 128], identity=ID[:]),
                   reads=[bsrc, bID], writes=[bpT])
            if eng == "act":
                op("act", lambda e: e.copy(out=dstT[:].rearrange("p a b -> p (a b)"), in_=pT[:]), reads=[bpT], writes=[bdstT])
            else:
                op("dve", lambda e: e.tensor_copy(out=dstT[:].rearrange("p a b -> p (a b)"), in_=pT[:]), reads=[bpT], writes=[bdstT])

        def mul_add(dst, bdst, src, bsrc, A, bA, B, bB):
            op("dve", lambda e: e.tensor_tensor(out=dst[:], in0=src[:], in1=A[:], op=ALU.mult), reads=[bsrc, bA], writes=[bdst])
            op("dve", lambda e: e.tensor_tensor(out=dst[:], in0=dst[:], in1=B[:], op=ALU.add), reads=[bdst, bB], writes=[bdst])

        for l in range(2):
            need_ctx = (l == 0)
            xsrc = xin if l == 0 else X
            with ExitStack() as es:
                sb, ps = pools(es)
                CC, bCC = load(es, "CC", cc_in, [128, 8, 2])
                op("act", lambda e: e.activation(out=CC[:], in_=CC[:], func=AF.Silu), reads=[bCC], writes=[bCC])
                BM, bBM = sb("BM", [2, 6 * D])
                dma("sp", lambda e: e.dma_start(out=BM[:], in_=b_mod[l].partition_broadcast(2)), writes=[bBM])
                MO, bMO = sb("MO", [2, 6 * D])
                WM = [sb(f"WM{i}", [128, 3072]) for i in range(2)]
                pM, bpM = ps("pM", [2, 3072])
                it = 0
                for half in range(2):
                    for kc in range(8):
                        W, bW = WM[it % 2]; it += 1
                        dma("sp", lambda e: e.dma_start(out=W[:], in_=w_mod[l, kc * 128:(kc + 1) * 128, half * 3072:(half + 1) * 3072]), writes=[bW])
                        for n in range(6):
                            op("pe", lambda e: e.matmul(pM[:, n * 512:(n + 1) * 512], lhsT=CC[:, kc, :], rhs=W[:, n * 512:(n + 1) * 512],
                                                        start=(kc == 0), stop=(kc == 7)), reads=[bCC, bW], writes=[bpM])
                    op("dve", lambda e: e.tensor_tensor(out=MO[:, half * 3072:(half + 1) * 3072], in0=pM[:], in1=BM[:, half * 3072:(half + 1) * 3072], op=ALU.add),
                       reads=[bpM, bBM], writes=[bMO])
                for s in (1, 4):
                    op("dve", lambda e: e.tensor_scalar(out=MO[:, s * D:(s + 1) * D], in0=MO[:, s * D:(s + 1) * D], scalar1=1.0, scalar2=None, op0=ALU.add),
                       reads=[bMO], writes=[bMO])
                dma("sp", lambda e: e.dma_start(out=MODS, in_=MO[:]), reads=[bMO])
            P.barrier()

            def modv(es, name, which, slot):
                return load(es, name, bc(MODS[which, slot * D:(slot + 1) * D]), [128, D])

            with ExitStack() as es:
                sb, ps = pools(es)
                WI, bWI = load(es, "WI", w_in[l].rearrange("(k p) n -> p k n", p=128), [128, 8, 2048])
                SC = [modv(es, "SC1L", 0, 1), modv(es, "SC1C", 1, 1)]
                SH = [modv(es, "SH1L", 0, 0), modv(es, "SH1C", 1, 0)]
                BCt, bBC = load(es, "BCt", cst["BC"], [128, 128]); BSt, bBS = load(es, "BSt", cst["BS"], [128, 128])
                xt = [sb(f"xt{i}", [128, D]) for i in range(2)]
                hts1 = [sb(f"ht{i}", [128, D]) for i in range(2)]; hTs1 = [sb(f"hT{i}", [128, 8, 128]) for i in range(2)]
                ut = [sb(f"ut{i}", [128, 2048]) for i in range(2)]
                aT, baT = sb("aT", [128, 2, 128]); pq = [sb(f"pq{i}", [128, 4, 128]) for i in range(2)]
                tmp = ln_tmp(es, "1")
                pT, bpT = ps("pT", [128, 1024]); pU, bpU = ps("pU", [128, 2048])
                bpUn = [Buf() for _ in range(4)]; bUtn = [[Buf() for _ in range(4)] for _ in range(2)]
                pA, bpA = ps("pA", [128, 512]); pPQ, bpPQ = ps("pPQ", [128, 512])
                def p1_front(i):
                    ctxk = 1 if i < 2 else 0
                    Xt, bX = xt[i % 2]; Ut, bUt = ut[i % 2]; PQ, bPQ = pq[i % 2]; ht, bht = hts1[i % 2]; hT, bhT = hTs1[i % 2]
                    dma("sp", lambda e: e.dma_start(out=Xt[:], in_=xsrc[i * 128:(i + 1) * 128, :]), writes=[bX])
                    layer_norm(es, "1", Xt, bX, ht, bht, tmp)
                    mul_add(ht, bht, ht, bht, SC[ctxk][0], SC[ctxk][1], SH[ctxk][0], SH[ctxk][1])
                    transpose8(ht, bht, pT, bpT, hT, bhT)
                def p1_mid(i):
                    ctxk = 1 if i < 2 else 0
                    Xt, bX = xt[i % 2]; Ut, bUt = ut[i % 2]; PQ, bPQ = pq[i % 2]; ht, bht = hts1[i % 2]; hT, bhT = hTs1[i % 2]
                    for n in range(4):
                        for k in range(8):
                            op("pe", lambda e: e.matmul(pU[:, n * 512:(n + 1) * 512], lhsT=hT[:, k, :], rhs=WI[:, k, n * 512:(n + 1) * 512],
                                                        start=(k == 0), stop=(k == 7)), reads=[bhT, bWI], writes=[bpUn[n]])
                        if n % 2 == 0:
                            op("act", lambda e: e.copy(out=Ut[:, n * 512:(n + 1) * 512], in_=pU[:, n * 512:(n + 1) * 512]), reads=[bpUn[n]], writes=[bUtn[i % 2][n]])
                        else:
                            op("dve", lambda e: e.tensor_copy(out=Ut[:, n * 512:(n + 1) * 512], in_=pU[:, n * 512:(n + 1) * 512]), reads=[bpUn[n]], writes=[bUtn[i % 2][n]])
                    dma("pool", lambda e: e.dma_start(out=U[i * 128:(i + 1) * 128, :], in_=Ut[:]), reads=bUtn[i % 2])
                def p1_dft(i):
                    ctxk = 1 if i < 2 else 0
                    Xt, bX = xt[i % 2]; Ut, bUt = ut[i % 2]; PQ, bPQ = pq[i % 2]; ht, bht = hts1[i % 2]; hT, bhT = hTs1[i % 2]
                    for kc in range(2):
                        op("pe", lambda e: e.transpose(out=pA[:, kc * 128:(kc + 1) * 128], in_=Ut[:, kc * 128:(kc + 1) * 128], identity=ID[:]),
                           reads=[bUtn[i % 2][0], bID], writes=[bpA])
                    op("dve", lambda e: e.tensor_copy(out=aT[:].rearrange("p a b -> p (a b)"), in_=pA[:, 0:256]), reads=[bpA], writes=[baT])
                    for kc in range(2):
                        op("pe", lambda e: e.matmul(pPQ[:, kc * 128:(kc + 1) * 128], lhsT=BCt[:], rhs=aT[:, kc, :], start=True, stop=True),
                           reads=[baT, bBC], writes=[bpPQ])
                        op("pe", lambda e: e.matmul(pPQ[:, (2 + kc) * 128:(3 + kc) * 128], lhsT=BSt[:], rhs=aT[:, kc, :], start=True, stop=True),
                           reads=[baT, bBS], writes=[bpPQ])
                    op("dve", lambda e: e.tensor_copy(out=PQ[:].rearrange("p a b -> p (a b)"), in_=pPQ[:]), reads=[bpPQ], writes=[bPQ])
                    dma("pool", lambda e: e.dma_start(out=PTD[:, i * 128:(i + 1) * 128].rearrange("(k p) t -> p k t", p=128), in_=PQ[:, 0:2, :]), reads=[bPQ])
                    dma("pool", lambda e: e.dma_start(out=QTD[:, i * 128:(i + 1) * 128].rearrange("(k p) t -> p k t", p=128), in_=PQ[:, 2:4, :]), reads=[bPQ])
                p1_front(0)
                for i in range(NT):
                    p1_mid(i)
                    if i + 1 < NT:
                        p1_front(i + 1)
                    p1_dft(i)
            P.barrier()
            if debug and l == 0 and debug == "p1":
                break
            seqs = [("L", 2, NL)] + ([("C", 0, 2)] if need_ctx else [])
            for (nm, t0, N1) in seqs:
                off = t0 * 128
                with ExitStack() as es:
                    sb, ps = pools(es)
                    FAP, bFAP = load(es, "FAP", cst["FAP" + nm], [N1, 2 * N1]); FAQ, bFAQ = load(es, "FAQ", cst["FAQ" + nm], [N1, 2 * N1])
                    TWC, bTWC = load(es, "TWC", cst["TWC" + nm], [128, N1]); TWS, bTWS = load(es, "TWS", cst["TWS" + nm], [128, N1])
                    FBC, bFBC = load(es, "FBC", cst["FBC" + nm], [128, 128]); FBS, bFBS = load(es, "FBS", cst["FBS" + nm], [128, 128])
                    PTs, bPTs = sb("PTs", [N1, 64, 128]); QTs, bQTs = sb("QTs", [N1, 64, 128])
                    At, bAt = sb("At", [128, 64, N1]); Bt, bBt = sb("Bt", [128, 64, N1])
                    cpa = min(64, 512 // (2 * N1)); cpb = min(64, 512 // N1)
                    tt = [sb(f"ft{i}", [128, cpa, N1]) for i in range(4)]
                    Yt, bYt = sb("Yt", [128, N1, 64])
                    pZ = [ps(f"pZ{i}", [128, 512]) for i in range(2)]; pY = [ps(f"pY{i}", [128, 512]) for i in range(2)]
                    for g in range(4):
                        dma("sp", lambda e: e.dma_start(out=PTs[:], in_=PTD[g * 64:(g + 1) * 64, off:off + 128 * N1].rearrange("c (a b) -> a c b", b=128)), writes=[bPTs])
                        dma("sp", lambda e: e.dma_start(out=QTs[:], in_=QTD[g * 64:(g + 1) * 64, off:off + 128 * N1].rearrange("c (a b) -> a c b", b=128)), writes=[bQTs])
                        for ci0, c0 in enumerate(range(0, 64, cpa)):
                            Z, bZ = pZ[ci0 % 2]
                            for ci in range(cpa):
                                c = c0 + ci
                                op("pe", lambda e: e.matmul(Z[:, ci * 2 * N1:(ci + 1) * 2 * N1], lhsT=PTs[:, c, :], rhs=FAP[:], start=True, stop=False), reads=[bPTs, bFAP], writes=[bZ])
                                op("pe", lambda e: e.matmul(Z[:, ci * 2 * N1:(ci + 1) * 2 * N1], lhsT=QTs[:, c, :], rhs=FAQ[:], start=False, stop=True), reads=[bQTs, bFAQ], writes=[bZ])
                            Zv = Z[:, 0:cpa * 2 * N1].rearrange("p (c t k) -> p c t k", t=2, k=N1)
                            Wv = Zv[:, :, 0, :]; Vv = Zv[:, :, 1, :]
                            cb = TWC[:].unsqueeze(1).to_broadcast([128, cpa, N1]); sbb = TWS[:].unsqueeze(1).to_broadcast([128, cpa, N1])
                            (t1, b1), (t2, b2), (t3, b3), (t4, b4) = tt
                            op("dve", lambda e: e.tensor_tensor(out=t1[:], in0=Wv, in1=cb, op=ALU.mult), reads=[bZ, bTWC], writes=[b1])
                            op("dve", lambda e: e.tensor_tensor(out=t2[:], in0=Vv, in1=sbb, op=ALU.mult), reads=[bZ, bTWS], writes=[b2])
                            op("dve", lambda e: e.tensor_tensor(out=t3[:], in0=Wv, in1=sbb, op=ALU.mult), reads=[bZ, bTWS], writes=[b3])
                            op("dve", lambda e: e.tensor_tensor(out=t4[:], in0=Vv, in1=cb, op=ALU.mult), reads=[bZ, bTWC], writes=[b4])
                            op("pool", lambda e: e.tensor_tensor(out=At[:, c0:c0 + cpa, :], in0=t1[:], in1=t2[:], op=ALU.subtract), reads=[b1, b2], writes=[bAt])
                            op("pool", lambda e: e.tensor_tensor(out=Bt[:, c0:c0 + cpa, :], in0=t3[:], in1=t4[:], op=ALU.add), reads=[b3, b4], writes=[bBt])
                        for ci0, c0 in enumerate(range(0, 64, cpb)):
                            Y, bY = pY[ci0 % 2]
                            ncol = cpb * N1
                            op("pe", lambda e: e.matmul(Y[:, 0:ncol], lhsT=FBC[:], rhs=At[:, c0:c0 + cpb, :].rearrange("p c k -> p (c k)"), start=True, stop=False), reads=[bAt, bFBC], writes=[bY])
                            op("pe", lambda e: e.matmul(Y[:, 0:ncol], lhsT=FBS[:], rhs=Bt[:, c0:c0 + cpb, :].rearrange("p c k -> p (c k)"), start=False, stop=True), reads=[bBt, bFBS], writes=[bY])
                            op("act", lambda e: e.copy(out=Yt[:, :, c0:c0 + cpb].rearrange("p k c -> p c k"), in_=Y[:, 0:ncol].rearrange("p (c k) -> p c k", k=N1)), reads=[bY], writes=[bYt])
                        dma("pool", lambda e: e.dma_start(out=M[off:off + 128 * N1, g * 64:(g + 1) * 64].rearrange("(a b) c -> a b c", b=N1), in_=Yt[:]), reads=[bYt])
                P.barrier(new_epoch=False)
            if debug == "p2a":
                break
            with ExitStack() as es:
                sb, ps = pools(es)
                PBt, bPBt = load(es, "PBt", cst["PB"], [128, 20, 128])
                PW, bPW = load(es, "PW", pool_w[l].rearrange("g c d -> c g d"), [64, 4, 64])
                PSC, bPSC = load(es, "PSC", bc(pool_scale[l]), [128, 256])
                bt = [[sb(f"bt{i}_{j}", [128, 256]) for j in range(3)] for i in range(2)]
                pooledT, bpooledT = sb("pooledT", [64, 4, 128])
                ots = [sb(f"pot{i}", [128, 256]) for i in range(2)]
                pP, bpP = ps("pP", [64, 512]); pO, bpO = ps("pO", [128, 256])
                it = 0
                for (nm, t0, nt) in seqs:
                    for i in range(nt):
                        B3 = bt[it % 2]; ot, bot = ots[it % 2]; it += 1
                        for j, ti in enumerate((i - 1, i, i + 1)):
                            if 0 <= ti < nt:
                                dma("sp", lambda e: e.dma_start(out=B3[j][0][:], in_=U[(t0 + ti) * 128:(t0 + ti + 1) * 128, 256:512]), writes=[B3[j][1]])
                        kind = 1 if i == 0 else (2 if i == nt - 1 else 0)
                        terms = [(1, kind)] + ([(0, 3)] if i > 0 else []) + ([(2, 4)] if i < nt - 1 else [])
                        for g in range(4):
                            for ti_, (j, kd) in enumerate(terms):
                                op("pe", lambda e: e.matmul(pP[:, g * 128:(g + 1) * 128], lhsT=B3[j][0][:, g * 64:(g + 1) * 64], rhs=PBt[:, g * 5 + kd, :],
                                                            start=(ti_ == 0), stop=(ti_ == len(terms) - 1)), reads=[B3[j][1], bPBt], writes=[bpP])
                        op("act", lambda e: e.copy(out=pooledT[:].rearrange("p a b -> p (a b)"), in_=pP[:]), reads=[bpP], writes=[bpooledT])
                        for g in range(4):
                            op("pe", lambda e: e.matmul(pO[:, g * 64:(g + 1) * 64], lhsT=pooledT[:, g, :], rhs=PW[:, g, :], start=True, stop=True), reads=[bpooledT, bPW], writes=[bpO])
                        op("dve", lambda e: e.tensor_tensor(out=ot[:], in0=pO[:], in1=PSC[:], op=ALU.mult), reads=[bpO, bPSC], writes=[bot])
                        dma("pool", lambda e: e.dma_start(out=M[(t0 + i) * 128:(t0 + i + 1) * 128, 256:512], in_=ot[:]), reads=[bot])
            P.barrier(new_epoch=False)
            if debug == "p2b":
                break
            with ExitStack() as es:
                sb, ps = pools(es)
                MP = load(es, "MP", cst["MASKP"], [128, 128]); MN = load(es, "MN", cst["MASKN"], [128, 128])
                SINK, bSINK = load(es, "SINK", bc(attn_sink[l]), [128, 4])
                NSINK, bNSINK = sb("NSINK", [128, 4])
                op("dve", lambda e: e.tensor_scalar(out=NSINK[:], in0=SINK[:], scalar1=-1.0, scalar2=None, op0=ALU.mult), reads=[bSINK], writes=[bNSINK])
                RING = 4
                QKV = [sb(f"QKV{i}", [128, 512]) for i in range(RING)]
                qT = [sb(f"qT{i}", [64, 4, 128]) for i in range(RING)]
                kT = [sb(f"kT{i}", [64, 2, 128]) for i in range(RING)]
                cQKV = [sb(f"cQKV{i}", [128, 512]) for i in range(2)]
                cqT = [sb(f"cqT{i}", [64, 4, 128]) for i in range(2)]
                ckT = [sb(f"ckT{i}", [64, 2, 128]) for i in range(2)]
                RC = [sb(f"RC{i}", [128, 32]) for i in range(2)]; RS = [sb(f"RS{i}", [128, 32]) for i in range(2)]
                rtmp, brtmp = sb("rtmp", [128, 6, 2, 16]); rt2, brt2 = sb("rt2", [128, 6, 16]); rt3, brt3 = sb("rt3", [128, 6, 16])
                scs = [sb(f"sc{i}", [128, 2, 5, 128]) for i in range(2)]; prs = [sb(f"pr{i}", [128, 2, 640]) for i in range(2)]; pTss = [sb(f"pTs{i}", [128, 2, 640]) for i in range(2)]
                rmaxs = [sb(f"rmax{i}", [128, 2]) for i in range(2)]; negms = [sb(f"negm{i}", [128, 2]) for i in range(2)]
                rsums = [sb(f"rsum{i}", [128, 2]) for i in range(2)]; esks = [sb(f"esk{i}", [128, 2]) for i in range(2)]
                aos = [sb(f"ao{i}", [128, 256]) for i in range(2)]
                if l == 0:
                    ZT, bZT = sb("ZT", [128, 8192])
                    op("pool", lambda e: e.memset(ZT[:], 0.0), writes=[bZT])
                pS, bpS = ps("pS", [128, 1536]); pPT, bpPT = ps("pPT", [128, 1536]); pO, bpO = ps("pOa", [128, 256])
                pQK, bpQK = pPT[0:64, 0:1024], bpPT

                def prep(ti, QKVt, qTt, kTt, rope_slot):
                    Q, bQ = QKVt
                    dma("sp", lambda e: e.dma_start(out=Q[:], in_=U[ti * 128:(ti + 1) * 128, 512:1024]), writes=[bQ])
                    if rope_slot is not None:
                        (C, bC), (S, bS) = RC[rope_slot], RS[rope_slot]
                        r0 = (ti - 2) * 128
                        dma("sp", lambda e: e.dma_start(out=C[:], in_=cst["ROPC"][r0:r0 + 128, :]), writes=[bC])
                        dma("sp", lambda e: e.dma_start(out=S[:], in_=cst["ROPS"][r0:r0 + 128, :]), writes=[bS])
                        for hf in range(2):
                            xv = Q[:, 0:384].rearrange("p (h f t d) -> p h f t d", h=6, f=2, t=2)[:, :, hf, :, :]
                            cb = C[:, hf * 16:(hf + 1) * 16].unsqueeze(1).unsqueeze(1).to_broadcast([128, 6, 2, 16])
                            sbb = S[:, hf * 16:(hf + 1) * 16].unsqueeze(1).to_broadcast([128, 6, 16])
                            op("dve", lambda e: e.tensor_tensor(out=rtmp[:], in0=xv, in1=cb, op=ALU.mult), reads=[bQ, bC], writes=[brtmp])
                            op("dve", lambda e: e.tensor_tensor(out=rt2[:], in0=xv[:, :, 1, :], in1=sbb, op=ALU.mult), reads=[bQ, bS], writes=[brt2])
                            op("dve", lambda e: e.tensor_tensor(out=rt3[:], in0=xv[:, :, 0, :], in1=sbb, op=ALU.mult), reads=[bQ, bS], writes=[brt3])
                            op("dve", lambda e: e.tensor_tensor(out=xv[:, :, 0, :], in0=rtmp[:, :, 0, :], in1=rt2[:], op=ALU.subtract), reads=[brtmp, brt2], writes=[bQ])
                            op("dve", lambda e: e.tensor_tensor(out=xv[:, :, 1, :], in0=rtmp[:, :, 1, :], in1=rt3[:], op=ALU.add), reads=[brtmp, brt3], writes=[bQ])
                    for h in range(6):
                        op("pe", lambda e: e.transpose(out=pQK[:, h * 128:(h + 1) * 128], in_=Q[:, h * 64:(h + 1) * 64], identity=ID[:]), reads=[bQ, bID], writes=[bpQK])
                    op("act", lambda e: e.copy(out=qTt[0][:].rearrange("p a b -> p (a b)"), in_=pQK[:, 0:512]), reads=[bpQK], writes=[qTt[1]])
                    op("act", lambda e: e.copy(out=kTt[0][:].rearrange("p a b -> p (a b)"), in_=pQK[:, 512:768]), reads=[bpQK], writes=[kTt[1]])

                def attend(qTt, blocks, ti, ao_t):
                    ao, bao = ao_t
                    nb = len(blocks); Wd = nb * 128

                    def stA(hp):
                        (sc, bsc), (pr, bpr) = scs[hp % 2], prs[hp % 2]
                        (rmax, brmax), (negm, bnegm), (rsum, brsum), (esk, besk) = rmaxs[hp % 2], negms[hp % 2], rsums[hp % 2], esks[hp % 2]
                        for g in range(2):
                            for bi, (kTt, Vt, mask) in enumerate(blocks):
                                op("pe", lambda e: e.matmul(pS[:, g * 640 + bi * 128:g * 640 + (bi + 1) * 128], lhsT=qTt[0][:, 2 * hp + g, :], rhs=kTt[0][:, hp, :], start=True, stop=True),
                                   reads=[qTt[1], kTt[1]], writes=[bpS])
                        pSv = pS[:, 0:1280].rearrange("p (g b c) -> p g b c", g=2, b=5)
                        for bi, (kTt, Vt, mask) in enumerate(blocks):
                            if mask is not None:
                                op("dve", lambda e: e.tensor_tensor(out=sc[:, :, bi, :], in0=pSv[:, :, bi, :], in1=mask[0][:].unsqueeze(1).to_broadcast([128, 2, 128]), op=ALU.add),
                                   reads=[bpS, mask[1]], writes=[bsc])
                            else:
                                op("act", lambda e: e.copy(out=sc[:, :, bi, :], in_=pSv[:, :, bi, :]), reads=[bpS], writes=[bsc])
                        for g in range(2):
                            scg = sc[:, g, 0:nb, :].rearrange("p b c -> p (b c)")
                            op("dve", lambda e: e.reduce_max(out=rmax[:, g:g + 1], in_=scg, axis=AX.X), reads=[bsc], writes=[brmax])
                        op("dve", lambda e: e.scalar_tensor_tensor(out=negm[:], in0=rmax[:], scalar=-0.125, in1=NSINK[:, 2 * hp:2 * hp + 2], op0=ALU.mult, op1=ALU.min),
                           reads=[brmax, bNSINK], writes=[bnegm])
                        for g in range(2):
                            scg = sc[:, g, 0:nb, :].rearrange("p b c -> p (b c)")
                            op("act", lambda e: e.activation(out=pr[:, g, 0:Wd], in_=scg, func=AF.Exp, bias=negm[:, g:g + 1], scale=0.125), reads=[bsc, bnegm], writes=[bpr])
                        for g in range(2):
                            op("dve", lambda e: e.reduce_sum(out=rsum[:, g:g + 1], in_=pr[:, g, 0:Wd], axis=AX.X), reads=[bpr], writes=[brsum])
                        op("dve", lambda e: e.tensor_tensor(out=esk[:], in0=SINK[:, 2 * hp:2 * hp + 2], in1=negm[:], op=ALU.add), reads=[bSINK, bnegm], writes=[besk])
                        op("act", lambda e: e.activation(out=esk[:], in_=esk[:], func=AF.Exp), reads=[besk], writes=[besk])
                        op("dve", lambda e: e.tensor_tensor(out=rsum[:], in0=rsum[:], in1=esk[:], op=ALU.add), reads=[brsum, besk], writes=[brsum])
                        op("dve", lambda e: e.reciprocal(out=rsum[:], in_=rsum[:]), reads=[brsum], writes=[brsum])

                    def stB(hp):
                        (pr, bpr), (pTs, bpTs), (rsum, brsum) = prs[hp % 2], pTss[hp % 2], rsums[hp % 2]
                        for g in range(2):
                            for bi in range(nb):
                                op("pe", lambda e: e.transpose(out=pPT[:, g * 640 + bi * 128:g * 640 + (bi + 1) * 128], in_=pr[:, g, bi * 128:(bi + 1) * 128], identity=ID[:]),
                                   reads=[bpr, bID], writes=[bpPT])
                        op("act", lambda e: e.copy(out=pTs[:, :, 0:Wd], in_=pPT[:, 0:1280].rearrange("p (g w) -> p g w", g=2)[:, :, 0:Wd]), reads=[bpPT], writes=[bpTs])
                        for g in range(2):
                            hq = 2 * hp + g
                            for bi, (kTt, Vt, mask) in enumerate(blocks):
                                op("pe", lambda e: e.matmul(pO[:, hq * 64:(hq + 1) * 64], lhsT=pTs[:, g, bi * 128:(bi + 1) * 128], rhs=Vt[0][:, 384 + hp * 64:384 + (hp + 1) * 64],
                                                            start=(bi == 0), stop=(bi == nb - 1)), reads=[bpTs, Vt[1]], writes=[bpO])
                        op("dve", lambda e: e.tensor_tensor(out=ao[:, hp * 128:(hp + 1) * 128].rearrange("p (g d) -> p g d", g=2),
                                                            in0=pO[:, hp * 128:(hp + 1) * 128].rearrange("p (g d) -> p g d", g=2),
                                                            in1=rsum[:].unsqueeze(2).to_broadcast([128, 2, 64]), op=ALU.mult), reads=[bpO, brsum], writes=[bao])

                    stA(0); stA(1); stB(0); stB(1)
                    dma("pool", lambda e: e.dma_start(out=M[ti * 128:(ti + 1) * 128, 512:768], in_=ao[:]), reads=[bao])

                for t in range(2):
                    prep(t, cQKV[t], cqT[t], ckT[t], None)
                cblocks = [(ckT[0], cQKV[0], None), (ckT[1], cQKV[1], None)]
                if need_ctx:
                    for t in range(2):
                        attend(cqT[t], cblocks, t, aos[t % 2])
                prep(2, QKV[0], qT[0], kT[0], 0)
                for i in range(NL):
                    if i + 1 < NL:
                        s = (i + 1) % RING
                        prep(i + 3, QKV[s], qT[s], kT[s], (i + 1) % 2)
                    blocks = []
                    if i > 0:
                        blocks.append((kT[(i - 1) % RING], QKV[(i - 1) % RING], MP))
                    blocks.append((kT[i % RING], QKV[i % RING], None))
                    if i + 1 < NL:
                        blocks.append((kT[(i + 1) % RING], QKV[(i + 1) % RING], MN))
                    blocks += cblocks
                    attend(qT[i % RING], blocks, i + 2, aos[i % 2])
                    if l == 0:
                        a0, a1 = (i * NBLK) // NL, ((i + 1) * NBLK) // NL
                        for a in range(a0, a1, 8):
                            ae = min(a1, a + 8)
                            dma("sp", lambda e: e.dma_start(out=XS.rearrange("(a p) d -> p a d", p=128)[:, a:ae, :], in_=ZT[:, 0:(ae - a) * D].rearrange("p (a d) -> p a d", d=D)), reads=[bZT])
            P.barrier(new_epoch=False)
            if debug == "p2c":
                break
            with ExitStack() as es:
                sb, ps = pools(es)
                POSZ, bPOSZ = load(es, "POSZ", cst["POSZ"], [128, 4, 2]); POSXI, bPOSXI = load(es, "POSXI", cst["POSXI"], [128, 128])
                PFt, bPF = load(es, "PFt", cst["PF"], [128, 128]); MFt, bMF = load(es, "MFt", cst["MF"], [128, 128])
                PBKt, bPBK = load(es, "PBKt", cst["PBK"], [128, 128]); MBt, bMB = load(es, "MBt", cst["MB"], [128, 128])
                LGX, bLGX = sb("LGX", [128, 4]); LG, bLG = sb("LG", [128, 2, 4])
                dma("sp", lambda e: e.dma_start(out=LGX[0:64, :], in_=ret_decay[l, 0].partition_broadcast(64)), writes=[bLGX])
                dma("sp", lambda e: e.dma_start(out=LGX[64:128, :], in_=ret_decay[l, 1].partition_broadcast(64)), writes=[bLGX])
                dma("sp", lambda e: e.dma_start(out=LG[:].rearrange("p a b -> p (a b)"), in_=ret_decay[l].rearrange("a b -> (a b)").partition_broadcast(128)), writes=[bLG])
                for (Tt, bT) in ((LGX, bLGX), (LG, bLG)):
                    op("act", lambda e: e.activation(out=Tt[:], in_=Tt[:], func=AF.Exp), reads=[bT], writes=[bT])
                    op("dve", lambda e: e.tensor_scalar(out=Tt[:], in0=Tt[:], scalar1=-1.0, scalar2=None, op0=ALU.mult), reads=[bT], writes=[bT])
                ZETA, bZETA = sb("ZETA", [128, 4, 2]); XI, bXI = sb("XI", [128, 4, 128]); GC, bGC = sb("GC", [128, 4]); DS, bDS = sb("DS", [128, 4, 128])
                e1, be1 = sb("e1", [128, 128]); e2, be2 = sb("e2", [128, 128])
                op("dve", lambda e: e.tensor_tensor(out=ZETA[:], in0=POSZ[:], in1=LG[:].rearrange("p d h -> p h d"), op=ALU.mult), reads=[bPOSZ, bLG], writes=[bZETA])
                op("act", lambda e: e.activation(out=ZETA[:], in_=ZETA[:], func=AF.Exp), reads=[bZETA], writes=[bZETA])
                op("dve", lambda e: e.tensor_scalar(out=ZETA[:], in0=ZETA[:], scalar1=0.125, scalar2=None, op0=ALU.mult), reads=[bZETA], writes=[bZETA])
                op("act", lambda e: e.activation(out=GC[:], in_=LGX[:], func=AF.Exp, scale=128.0), reads=[bLGX], writes=[bGC])
                for h in range(4):
                    op("act", lambda e: e.activation(out=XI[:, h, :], in_=POSXI[:], func=AF.Exp, scale=LGX[:, h:h + 1]), reads=[bPOSXI, bLGX], writes=[bXI])
                    op("act", lambda e: e.activation(out=e1[:], in_=PFt[:], func=AF.Exp, scale=LG[:, 0, h:h + 1]), reads=[bPF, bLG], writes=[be1])
                    op("dve", lambda e: e.tensor_tensor(out=e1[:], in0=e1[:], in1=MFt[:], op=ALU.mult), reads=[be1, bMF], writes=[be1])
                    op("act", lambda e: e.activation(out=e2[:], in_=PBKt[:], func=AF.Exp, scale=LG[:, 1, h:h + 1]), reads=[bPBK, bLG], writes=[be2])
                    op("dve", lambda e: e.tensor_tensor(out=e2[:], in0=e2[:], in1=MBt[:], op=ALU.mult), reads=[be2, bMB], writes=[be2])
                    op("dve", lambda e: e.tensor_tensor(out=e1[:], in0=e1[:], in1=e2[:], op=ALU.add), reads=[be1, be2], writes=[be1])
                    op("dve", lambda e: e.tensor_scalar(out=DS[:, h, :], in0=e1[:], scalar1=0.125, scalar2=None, op0=ALU.mult), reads=[be1], writes=[bDS])
                RB, _ = sb("RB", [64, NT, 4, 64]); bRB = [Buf() for _ in range(NT)]
                RBB, _ = sb("RBB", [64, NT, 4, 64])
                GCB, bGCB = sb("GCB", [64, 4]); XIB, bXIB = sb("XIB", [64, 4, 128])
                PXB, bPXB = load(es, "PXB", cst["POSXI"][64:128, :], [64, 128])
                op("act", lambda e: e.activation(out=GCB[:], in_=LG[0:64, 1, :], func=AF.Exp, scale=128.0), reads=[bLG], writes=[bGCB])
                for h in range(4):
                    op("act", lambda e: e.activation(out=XIB[:, h, :], in_=PXB[:], func=AF.Exp, scale=LG[0:64, 1, h:h + 1]), reads=[bPXB, bLG], writes=[bXIB])
                QXBs = [sb(f"QXB{i}", [64, 128]) for i in range(2)]; kTrs = [sb(f"kTr{i}", [64, 128]) for i in range(2)]
                Rts = [sb(f"Rt{i}", [128, 1024]) for i in range(2)]
                KZ, bKZ = sb("KZ", [128, 4, 2, 64]); RQ2, bRQ2 = sb("RQ2", [128, 4, 2, 64])
                QXs = [sb(f"QX{i}", [128, 128]) for i in range(2)]; qkTs = [sb(f"qkT{i}", [64, 256]) for i in range(2)]; SDs = [sb(f"SD{i}", [128, 128]) for i in range(2)]
                s1, bs1 = sb("s1", [128, 4]); s2, bs2 = sb("s2", [128, 4]); msq, bmsq = sb("msq", [128, 4])
                sq, bsq = sb("sq", [128, 256]); gn, bgn = sb("gn", [128, 4, 64]); sg, bsg = sb("sg", [128, 256])
                st4, bst4 = sb("st4", [128, 4, 6]); mv4, bmv4 = sb("mv4", [128, 4, 2])
                ros = [sb(f"ro{i}", [128, 256]) for i in range(2)]
                pKV, bpKV = ps("pKV", [64, 512]); pTRq, bpTRq = ps("pTRq", [64, 512]); pTRk, bpTRk = ps("pTRk", [64, 512])
                pSTa, bpSTa = ps("pSTa", [128, 512]); pOr, bpOr = ps("pOr", [128, 256])
                qTas = [sb(f"qTa{i}", [64, 4, 128]) for i in range(2)]; kTas = [sb(f"kTa{i}", [64, 4, 128]) for i in range(2)]
                QXas = [sb(f"QXa{i}", [64, 4, 128]) for i in range(2)]; QXBas = [sb(f"QXBa{i}", [64, 4, 128]) for i in range(2)]
                SDas = [sb(f"SDa{i}", [128, 4, 128]) for i in range(2)]
                op("pool", lambda e: e.memset(RBB[:, 1, :, :], 0.0), writes=[bRB[1]])
                op("pool", lambda e: e.memset(RB[:, 0, :, :], 0.0), writes=[bRB[0]])
                order_b = [1, 0] + list(range(NT - 1, 1, -1))
                it = 0

                def kv_states(Rt, bRt):
                    op("dve", lambda e: e.tensor_tensor(out=KZ[:], in0=Rt[:, 256:512].rearrange("p (h d) -> p h d", h=4).unsqueeze(2).to_broadcast([128, 4, 2, 64]),
                                                        in1=ZETA[:].unsqueeze(3).to_broadcast([128, 4, 2, 64]), op=ALU.mult), reads=[bRt, bZETA], writes=[bKZ])
                    for h in range(4):
                        for a in range(2):
                            op("pe", lambda e: e.matmul(pKV[0:64, (a * 4 + h) * 64:(a * 4 + h + 1) * 64], lhsT=KZ[:, h, a, :], rhs=Rt[:, 512 + h * 64:512 + (h + 1) * 64],
                                                        start=True, stop=True), reads=[bKZ, bRt], writes=[bpKV])

                for idx, ti in enumerate(order_b):
                    Rt, bRt = Rts[it % 2]; it += 1
                    dma("sp", lambda e: e.dma_start(out=Rt[:, 256:768], in_=U[ti * 128:(ti + 1) * 128, 1280:1792]), writes=[bRt])
                    kv_states(Rt, bRt)
                    if idx + 1 < NT:
                        nxt = order_b[idx + 1]
                        for h in range(4):
                            op("dve", lambda e: e.scalar_tensor_tensor(out=RBB[:, nxt, h, :], in0=RBB[:, ti, h, :], scalar=GCB[:, h:h + 1],
                                                                       in1=pKV[0:64, (4 + h) * 64:(5 + h) * 64], op0=ALU.mult, op1=ALU.add),
                               reads=[bRB[ti], bGCB, bpKV], writes=[bRB[nxt]])
                for ti in range(NT):
                    want_out = ((ti >= 2) or need_ctx) and not os.environ.get('RET_SKIP_OUT')
                    Rt, bRt = Rts[it % 2]; ro, bro = ros[it % 2]; it += 1
                    dma("sp", lambda e: e.dma_start(out=Rt[:], in_=U[ti * 128:(ti + 1) * 128, 1024:2048]), writes=[bRt])
                    kv_states(Rt, bRt)
                    if want_out:
                        (qTa, bqTa), (kTa, bkTa), (QXa, bQXa), (QXBa, bQXBa), (SDa, bSDa) = qTas[it % 2], kTas[it % 2], QXas[it % 2], QXBas[it % 2], SDas[it % 2]
                        for h in range(4):
                            op("pe", lambda e: e.transpose(out=pTRq[:, h * 128:(h + 1) * 128], in_=Rt[:, h * 64:(h + 1) * 64], identity=ID[:]), reads=[bRt, bID], writes=[bpTRq])
                            op("pe", lambda e: e.transpose(out=pTRk[:, h * 128:(h + 1) * 128], in_=Rt[:, 256 + h * 64:256 + (h + 1) * 64], identity=ID[:]), reads=[bRt, bID], writes=[bpTRk])
                        op("act", lambda e: e.copy(out=qTa[:].rearrange("p a b -> p (a b)"), in_=pTRq[:]), reads=[bpTRq], writes=[bqTa])
                        op("act", lambda e: e.copy(out=kTa[:].rearrange("p a b -> p (a b)"), in_=pTRk[:]), reads=[bpTRk], writes=[bkTa])
                        op("dve", lambda e: e.tensor_tensor(out=QXa[:], in0=qTa[:], in1=XI[0:64, :, :], op=ALU.mult), reads=[bqTa, bXI], writes=[bQXa])
                        op("dve", lambda e: e.tensor_tensor(out=QXBa[:], in0=qTa[:], in1=XIB[:], op=ALU.mult), reads=[bqTa, bXIB], writes=[bQXBa])
                        for h in range(4):
                            op("pe", lambda e: e.matmul(pSTa[:, h * 128:(h + 1) * 128], lhsT=kTa[:, h, :], rhs=qTa[:, h, :], start=True, stop=True), reads=[bkTa, bqTa], writes=[bpSTa])
                        op("dve", lambda e: e.tensor_tensor(out=SDa[:].rearrange("p a b -> p (a b)"), in0=pSTa[:], in1=DS[:].rearrange("p a b -> p (a b)"), op=ALU.mult), reads=[bpSTa, bDS], writes=[bSDa])
                        for h in range(4):
                            op("pe", lambda e: e.matmul(pOr[:, h * 64:(h + 1) * 64], lhsT=SDa[:, h, :], rhs=Rt[:, 512 + h * 64:512 + (h + 1) * 64], start=True, stop=False), reads=[bSDa, bRt], writes=[bpOr])
                            op("pe", lambda e: e.matmul(pOr[:, h * 64:(h + 1) * 64], lhsT=QXa[:, h, :], rhs=RB[:, ti, h, :], start=False, stop=False), reads=[bQXa, bRB[ti]], writes=[bpOr])
                            op("pe", lambda e: e.matmul(pOr[:, h * 64:(h + 1) * 64], lhsT=QXBa[:, h, :], rhs=RBB[:, ti, h, :], start=False, stop=True), reads=[bQXBa, bRB[ti]], writes=[bpOr])
                    if ti + 1 < NT:
                        for h in range(4):
                            op("dve", lambda e: e.scalar_tensor_tensor(out=RB[:, ti + 1, h, :], in0=RB[:, ti, h, :], scalar=GC[0:64, h:h + 1],
                                                                       in1=pKV[0:64, h * 64:(h + 1) * 64], op0=ALU.mult, op1=ALU.add),
                               reads=[bRB[ti], bGC, bpKV], writes=[bRB[ti + 1]])
                    if want_out and os.environ.get('RET_NO_GN'):
                        op("act", lambda e: e.copy(out=ro[:], in_=pOr[:]), reads=[bpOr], writes=[bro])
                        dma("pool", lambda e: e.dma_start(out=M[ti * 128:(ti + 1) * 128, 768:1024], in_=ro[:]), reads=[bro])
                    elif want_out:
                        op("act", lambda e: e.copy(out=sq[:], in_=pOr[:]), reads=[bpOr], writes=[bsq])
                        for h in range(4):
                            op("dve", lambda e: e.bn_stats(out=st4[:, h, :], in_=sq[:, h * 64:(h + 1) * 64]), reads=[bsq], writes=[bst4])
                        for h in range(4):
                            op("dve", lambda e: e.bn_aggr(out=mv4[:, h, :], in_=st4[:, h, :]), reads=[bst4], writes=[bmv4])
                        for h in range(4):
                            op("act", lambda e: e.activation(out=s2[:, h:h + 1], in_=mv4[:, h, 1:2], func=AF.Sqrt, bias=EPS, scale=1.0), reads=[bmv4], writes=[bs2])
                        op("dve", lambda e: e.reciprocal(out=s1[:], in_=s2[:]), reads=[bs2], writes=[bs1])
                        for h in range(4):
                            op("dve", lambda e: e.tensor_scalar(out=gn[:, h, :], in0=sq[:, h * 64:(h + 1) * 64], scalar1=mv4[:, h, 0:1], scalar2=s1[:, h:h + 1], op0=ALU.subtract, op1=ALU.mult),
                               reads=[bsq, bmv4, bs1], writes=[bgn])
                        op("act", lambda e: e.activation(out=sg[:], in_=Rt[:, 768:1024], func=AF.Silu), reads=[bRt], writes=[bsg])
                        op("dve", lambda e: e.tensor_tensor(out=ro[:], in0=gn[:].rearrange("p h d -> p (h d)"), in1=sg[:], op=ALU.mult), reads=[bgn, bsg], writes=[bro])
                        dma("pool", lambda e: e.dma_start(out=M[ti * 128:(ti + 1) * 128, 768:1024], in_=ro[:]), reads=[bro])
            P.barrier()
            if debug == "p2":
                break
            tiles3 = list(range(NT)) if need_ctx else list(range(2, NT))
            with ExitStack() as es:
                sb, ps = pools(es)
                WO, bWO = load(es, "WO", w_out[l].rearrange("(k p) n -> p k n", p=128), [128, 8, D])
                RW, bRW = load(es, "RW", router_w[l].rearrange("(k p) n -> p k n", p=128), [128, 8, NEXP])
                RBi, bRBi = load(es, "RBi", bc(router_b[l]), [128, NEXP])
                G1 = [modv(es, "G1L", 0, 2), modv(es, "G1C", 1, 2)]
                SC2 = [modv(es, "SC2L", 0, 4), modv(es, "SC2C", 1, 4)]
                SH2 = [modv(es, "SH2L", 0, 3), modv(es, "SH2C", 1, 3)]
                LNG = load(es, "LNG", bc(ln_g[l, 0]), [128, D]); LNB = load(es, "LNB", bc(ln_b[l, 0]), [128, D])
                mts = [sb(f"mt{i}", [128, D]) for i in range(2)]; xts = [sb(f"x3t{i}", [128, D]) for i in range(2)]
                mTs = [sb(f"mT{i}", [128, 8, 128]) for i in range(2)]; t3s = [sb(f"t3{i}", [128, D]) for i in range(2)]; xns = [sb(f"xn{i}", [128, D]) for i in range(2)]
                h2s = [sb(f"h2t{i}", [128, D]) for i in range(2)]; h2Ts = [sb(f"h2T{i}", [128, 8, 128]) for i in range(2)]
                bpYn = [Buf() for _ in range(2)]
                tmp = ln_tmp(es, "3")
                pT, bpT = ps("pT3", [128, 1024]); pY, bpY = ps("pY3", [128, 1024]); pL, bpL = ps("pL", [128, NEXP])
                pT2, bpT2 = ps("pT3b", [128, 1024])

                def front(ii):
                    i = tiles3[ii]
                    ck = 1 if i < 2 else 0
                    Mt, bMt = mts[ii % 2]; Xt, bXt = xts[ii % 2]; h2, bh2 = h2s[ii % 2]
                    (mT, bmT), (t3, bt3), (xn, bxn) = mTs[ii % 2], t3s[ii % 2], xns[ii % 2]
                    dma("sp", lambda e: e.dma_start(out=Mt[:], in_=M[i * 128:(i + 1) * 128, :]), writes=[bMt])
                    dma("sp", lambda e: e.dma_start(out=Xt[:], in_=xsrc[i * 128:(i + 1) * 128, :]), writes=[bXt])
                    transpose8(Mt, bMt, pT, bpT, mT, bmT)
                    for n in range(2):
                        for k in range(8):
                            op("pe", lambda e: e.matmul(pY[:, n * 512:(n + 1) * 512], lhsT=mT[:, k, :], rhs=WO[:, k, n * 512:(n + 1) * 512], start=(k == 0), stop=(k == 7)),
                               reads=[bmT, bWO], writes=[bpYn[n]])
                        op("dve", lambda e: e.tensor_tensor(out=t3[:, n * 512:(n + 1) * 512], in0=pY[:, n * 512:(n + 1) * 512], in1=G1[ck][0][:, n * 512:(n + 1) * 512], op=ALU.mult),
                           reads=[bpYn[n], G1[ck][1]], writes=[bt3])
                    op("dve", lambda e: e.scalar_tensor_tensor(out=t3[:], in0=Xt[:], scalar=ALPHA, in1=t3[:], op0=ALU.mult, op1=ALU.add), reads=[bXt, bt3], writes=[bt3])
                    layer_norm(es, "3", t3, bt3, xn, bxn, tmp)
                    mul_add(xn, bxn, xn, bxn, LNG[0], LNG[1], LNB[0], LNB[1])
                    dma("pool", lambda e: e.dma_start(out=X[i * 128:(i + 1) * 128, :], in_=xn[:]), reads=[bxn])
                    layer_norm(es, "3", xn, bxn, h2, bh2, tmp)
                    mul_add(h2, bh2, h2, bh2, SC2[ck][0], SC2[ck][1], SH2[ck][0], SH2[ck][1])
                    dma("pool", lambda e: e.dma_start(out=H2[i * 128:(i + 1) * 128, :], in_=h2[:]), reads=[bh2])

                def back(ii):
                    i = tiles3[ii]
                    h2, bh2 = h2s[ii % 2]; h2T, bh2T = h2Ts[ii % 2]
                    transpose8(h2, bh2, pT2, bpT2, h2T, bh2T)
                    for k in range(8):
                        op("pe", lambda e: e.matmul(pL[:], lhsT=h2T[:, k, :], rhs=RW[:, k, :], start=(k == 0), stop=(k == 7)), reads=[bh2T, bRW], writes=[bpL])
                    op("dve", lambda e: e.tensor_tensor(out=LOG[:, i, :], in0=pL[:], in1=RBi[:], op=ALU.add), reads=[bpL, bRBi], writes=[bLOG])

                front(0)
                for ii in range(len(tiles3)):
                    if ii + 1 < len(tiles3):
                        front(ii + 1)
                    back(ii)
            P.barrier()
            if debug == "p3":
                break
            tiles4 = tiles3
            with ExitStack() as es:
                sb, ps = pools(es)
                UTt, bUT = load(es, "UTt", cst["UT"], [128, 128]); ONES, bONES = load(es, "ONESt", cst["ONES"], [128, 128])
                JV, bJV = load(es, "JVt", cst["JV"], [128, NBLK]); KP, bKP = load(es, "KPt", cst["KP"], [128, 8])
                SL4i, bSL4i = sb("SL4i", [128, NT, 4], I32); G4, bG4 = sb("G4", [128, NT, 4])
                IDX, bIDX = sb("IDX", [128, NBLK, 8], I32); BEi, bBEi = sb("BEi", [128, NBLK], I32)
                with ExitStack() as es2:
                    sb2, ps2 = pools(es2)
                    MASK, bMASK = sb2("MASK", [128, NT, NEXP]); GT, bGT = sb2("GT", [128, NT, NEXP]); POS, bPOS = sb2("POS", [128, NT, NEXP])
                    BASE, bBASE = sb2("BASE", [128, NEXP]); t8, bt8 = sb2("t8", [128, 8]); ng, bng = sb2("ng", [128, 1]); ss, bss = sb2("ss", [128, 1])
                    ee, bee = sb2("ee", [128, NEXP]); oh, boh = sb2("oh", [128, NEXP])
                    PADD, bPADD = sb2("PADD", [128, NEXP]); PEND, bPEND = sb2("PEND", [128, NEXP]); PST, bPST = sb2("PST", [128, NEXP])
                    SLOT, bSLOT = sb2("SLOT", [128, NEXP]); s4f, bs4f = sb2("s4f", [128, 8])
                    BE, bBE = sb2("BE", [128, NBLK]); IDXf, bIDXf = sb2("IDXf", [128, NBLK, 8]); ONE32, bONE32 = sb2("ONE32", [128, NEXP])
                    pPos, bpPos = ps2("pPos", [128, NEXP]); pCS, bpCS = ps2("pCS", [128, NEXP])
                    op("pool", lambda e: e.memset(BASE[:], 0.0), writes=[bBASE])
                    op("pool", lambda e: e.memset(ONE32[:], 1.0), writes=[bONE32])
                    for i in tiles4:
                        lg = LOG[:, i, :]
                        op("dve", lambda e: e.max(out=t8[:], in_=lg), reads=[bLOG], writes=[bt8])
                        op("dve", lambda e: e.tensor_scalar(out=MASK[:, i, :], in0=lg, scalar1=t8[:, 3:4], scalar2=None, op0=ALU.is_ge), reads=[bLOG, bt8], writes=[bMASK])
                        op("dve", lambda e: e.tensor_scalar(out=ng[:], in0=t8[:, 0:1], scalar1=-1.0, scalar2=None, op0=ALU.mult), reads=[bt8], writes=[bng])
                        op("act", lambda e: e.activation(out=ee[:], in_=lg, func=AF.Exp, bias=ng[:, 0:1], scale=1.0), reads=[bLOG, bng], writes=[bee])
                        op("dve", lambda e: e.tensor_tensor(out=ee[:], in0=ee[:], in1=MASK[:, i, :], op=ALU.mult), reads=[bee, bMASK], writes=[bee])
                        op("dve", lambda e: e.reduce_sum(out=ss[:], in_=ee[:], axis=AX.X), reads=[bee], writes=[bss])
                        op("dve", lambda e: e.reciprocal(out=ss[:], in_=ss[:]), reads=[bss], writes=[bss])
                        op("dve", lambda e: e.tensor_scalar(out=GT[:, i, :], in0=ee[:], scalar1=ss[:, 0:1], scalar2=None, op0=ALU.mult), reads=[bee, bss], writes=[bGT])
                        op("pe", lambda e: e.matmul(pPos[:], lhsT=UTt[:], rhs=MASK[:, i, :], start=True, stop=True), reads=[bUT, bMASK], writes=[bpPos])
                        op("pe", lambda e: e.matmul(pCS[:], lhsT=ONES[:], rhs=MASK[:, i, :], start=True, stop=True), reads=[bONES, bMASK], writes=[bpCS])
                        op("dve", lambda e: e.tensor_tensor(out=POS[:, i, :], in0=pPos[:], in1=BASE[:], op=ALU.add), reads=[bpPos, bBASE], writes=[bPOS])
                        op("dve", lambda e: e.tensor_tensor(out=BASE[:], in0=pCS[:], in1=BASE[:], op=ALU.add), reads=[bpCS, bBASE], writes=[bBASE])
                    PADi, bPADi = sb2("PADi", [128, NEXP], I32)
                    op("dve", lambda e: e.tensor_scalar(out=PADD[:], in0=BASE[:], scalar1=127.0, scalar2=None, op0=ALU.add), reads=[bBASE], writes=[bPADD])
                    op("dve", lambda e: e.tensor_copy(out=PADi[:], in_=PADD[:]), reads=[bPADD], writes=[bPADi])
                    op("dve", lambda e: e.tensor_single_scalar(out=PADi[:], in_=PADi[:], scalar=7, op=ALU.arith_shift_right), reads=[bPADi], writes=[bPADi])
                    op("dve", lambda e: e.tensor_single_scalar(out=PADi[:], in_=PADi[:], scalar=7, op=ALU.logical_shift_left), reads=[bPADi], writes=[bPADi])
                    op("dve", lambda e: e.tensor_copy(out=PADD[:], in_=PADi[:]), reads=[bPADi], writes=[bPADD])
                    op("dve", lambda e: e.tensor_tensor_scan(out=PEND[:], data0=ONE32[:], data1=PADD[:], initial=0.0, op0=ALU.mult, op1=ALU.add), reads=[bONE32, bPADD], writes=[bPEND])
                    op("dve", lambda e: e.tensor_tensor(out=PST[:], in0=PEND[:], in1=PADD[:], op=ALU.subtract), reads=[bPEND, bPADD], writes=[bPST])
                    op("dve", lambda e: e.tensor_scalar(out=PST[:], in0=PST[:], scalar1=1.0, scalar2=None, op0=ALU.add), reads=[bPST], writes=[bPST])
                    for i in tiles4:
                        op("dve", lambda e: e.tensor_tensor(out=SLOT[:], in0=POS[:, i, :], in1=PST[:], op=ALU.add), reads=[bPOS, bPST], writes=[bSLOT])
                        op("dve", lambda e: e.tensor_tensor(out=SLOT[:], in0=SLOT[:], in1=MASK[:, i, :], op=ALU.mult), reads=[bSLOT, bMASK], writes=[bSLOT])
                        op("dve", lambda e: e.tensor_scalar(out=SLOT[:], in0=SLOT[:], scalar1=-1.0, scalar2=None, op0=ALU.add), reads=[bSLOT], writes=[bSLOT])
                        op("dve", lambda e: e.max(out=s4f[:], in_=SLOT[:]), reads=[bSLOT], writes=[bs4f])
                        op("dve", lambda e: e.tensor_copy(out=SL4i[:, i, :], in_=s4f[:, 0:4]), reads=[bs4f], writes=[bSL4i])
                        for k in range(4):
                            op("dve", lambda e: e.tensor_scalar(out=oh[:], in0=SLOT[:], scalar1=s4f[:, k:k + 1], scalar2=None, op0=ALU.is_equal), reads=[bSLOT, bs4f], writes=[boh])
                            op("dve", lambda e: e.tensor_tensor(out=oh[:], in0=oh[:], in1=GT[:, i, :], op=ALU.mult), reads=[boh, bGT], writes=[boh])
                            op("dve", lambda e: e.reduce_sum(out=G4[:, i, k:k + 1], in_=oh[:], axis=AX.X), reads=[boh], writes=[bG4])
                    op("pool", lambda e: e.memset(BE[:], 0.0), writes=[bBE])
                    for ex in range(NEXP):
                        op("dve", lambda e: e.scalar_tensor_tensor(out=BE[:], in0=JV[:], scalar=PEND[:, ex:ex + 1], in1=BE[:], op0=ALU.is_ge, op1=ALU.add), reads=[bJV, bPEND, bBE], writes=[bBE])
                    op("dve", lambda e: e.tensor_scalar(out=BE[:], in0=BE[:], scalar1=float(NEXP - 1), scalar2=None, op0=ALU.min), reads=[bBE], writes=[bBE])
                    BE2, bBE2 = sb2("BE2", [128, NBLK]); SAME, bSAME = sb2("SAME", [128, NBLK])
                    op("dve", lambda e: e.tensor_copy(out=BE2[:], in_=BE[:]), reads=[bBE], writes=[bBE2])
                    op("pool", lambda e: e.memset(SAME[:], 0.0), writes=[bSAME])
                    op("dve", lambda e: e.tensor_tensor(out=SAME[:, 1:NBLK], in0=BE[:, 1:NBLK], in1=BE2[:, 0:NBLK - 1], op=ALU.is_equal), reads=[bBE, bBE2, bSAME], writes=[bSAME])
                    op("dve", lambda e: e.tensor_scalar(out=SAME[:], in0=SAME[:], scalar1=100000.0, scalar2=None, op0=ALU.mult), reads=[bSAME], writes=[bSAME])
                    op("dve", lambda e: e.tensor_tensor(out=BE2[:], in0=BE2[:], in1=SAME[:], op=ALU.add), reads=[bBE2, bSAME], writes=[bBE2])
                    op("dve", lambda e: e.tensor_copy(out=BEi[:], in_=BE2[:]), reads=[bBE2], writes=[bBEi])
                    op("dve", lambda e: e.scalar_tensor_tensor(out=BE[:], in0=BE[:], scalar=1024.0, in1=SAME[:], op0=ALU.mult, op1=ALU.add), reads=[bBE, bSAME], writes=[bBE])
                    op("dve", lambda e: e.tensor_tensor(out=IDXf[:], in0=BE[:].unsqueeze(2).to_broadcast([128, NBLK, 8]), in1=KP[:].unsqueeze(1).to_broadcast([128, NBLK, 8]), op=ALU.add),
                       reads=[bBE, bKP], writes=[bIDXf])
                    op("dve", lambda e: e.tensor_copy(out=IDX[:], in_=IDXf[:]), reads=[bIDXf], writes=[bIDX])
                P.barrier(new_epoch=False)
                if debug == "p4a":
                    dma("sp", lambda e: e.dma_start(out=DBG[:, 0:NT * 4], in_=G4[:].rearrange("p a b -> p (a b)")), reads=[bG4])
                    dma("sp", lambda e: e.dma_start(out=DBGI[:, 0:NT * 4], in_=SL4i[:].rearrange("p a b -> p (a b)")), reads=[bSL4i])
                    dma("sp", lambda e: e.dma_start(out=DBGI[:, NT * 4:NT * 4 + NBLK], in_=BEi[:]), reads=[bBEi])
                    P.barrier(new_epoch=False)
                    break
                with ExitStack() as es2:
                    sb2, ps2 = pools(es2)
                    hts = [sb2(f"h4t{i}", [128, D]) for i in range(3)]
                    for ii, i in enumerate(tiles4):
                        Ht, bHt = hts[ii % 3]
                        dma("sp", lambda e: e.dma_start(out=Ht[:], in_=H2[i * 128:(i + 1) * 128, :]), writes=[bHt])
                        for k in range(4):
                            dma("pool", lambda e: e.indirect_dma_start(out=XS[:, :], out_offset=bass.IndirectOffsetOnAxis(ap=SL4i[:, i, k:k + 1], axis=0), in_=Ht[:], in_offset=None), reads=[bHt, bSL4i])
                P.barrier(new_epoch=False)
                if debug == "p4b":
                    break
                with ExitStack() as es2:
                    sb2, ps2 = pools(es2)
                    W1, _ = sb2("W1", [128, 8, 2048]); W2, _ = sb2("W2", [128, 8, D])
                    bW1 = [Buf() for _ in range(8)]; bW2 = [Buf() for _ in range(8)]
                    B1, bB1 = sb2("B1", [128, 2048]); B2, bB2 = sb2("B2", [128, D])
                    xss = [sb2(f"xs{i}", [128, D]) for i in range(2)]; xTs = [sb2(f"xT4_{i}", [128, 8, 128]) for i in range(2)]
                    ubs = [sb2(f"ub{i}", [128, 1024]) for i in range(2)]; glus = [sb2(f"glu{i}", [128, 512]) for i in range(2)]
                    lins = [sb2(f"lin{i}", [128, 512]) for i in range(2)]; sigs = [sb2(f"sig{i}", [128, 512]) for i in range(2)]
                    aTs = [sb2(f"aT4_{i}", [128, 4, 128]) for i in range(2)]; ys = [sb2(f"ys{i}", [128, D]) for i in range(2)]
                    pT0, bpT0 = ps2("pT4a", [128, 512]); pT1, bpT1 = ps2("pT4x", [128, 512])
                    pUs = [ps2(f"pU4_{i}", [128, 1024]) for i in range(2)]; pY, bpY = ps2("pY4", [128, 1024])
                    bpUh = [[Buf(), Buf()], [Buf(), Buf()]]
                    w1v = exp_w1.rearrange("l e k n -> (l e k) n"); w2v = exp_w2.rearrange("l e k n -> (l e k) n")
                    b1v = exp_b1.rearrange("l e n -> (l e) n"); b2v = exp_b2.rearrange("l e n -> (l e) n")
                    regW = nc.gpsimd.to_reg(NEXP * D - 1); regB = nc.gpsimd.to_reg(NEXP - 1)

                    def gW1(j):
                        for k in range(8):
                            dma("pool", lambda e: e.indirect_dma_start(out=W1[:, k, :], out_offset=None, in_=w1v, in_offset=bass.IndirectOffsetOnAxis(ap=IDX[:, j, k:k + 1], axis=0),
                                                                       element_offset=l * NEXP * D * 2048, bounds_check=regW, oob_is_err=False), reads=[bIDX], writes=[bW1[k]])

                    def gB1(j):
                        dma("pool", lambda e: e.indirect_dma_start(out=B1[:], out_offset=None, in_=b1v, in_offset=bass.IndirectOffsetOnAxis(ap=BEi[:, j:j + 1], axis=0),
                                                                   element_offset=l * NEXP * 2048, bounds_check=regB, oob_is_err=False), reads=[bBEi], writes=[bB1])

                    def gW2(j):
                        for k in range(8):
                            dma("pool", lambda e: e.indirect_dma_start(out=W2[:, k, :], out_offset=None, in_=w2v, in_offset=bass.IndirectOffsetOnAxis(ap=IDX[:, j, k:k + 1], axis=0),
                                                                       element_offset=l * NEXP * D * D, bounds_check=regW, oob_is_err=False), reads=[bIDX], writes=[bW2[k]])
                        dma("pool", lambda e: e.indirect_dma_start(out=B2[:], out_offset=None, in_=b2v, in_offset=bass.IndirectOffsetOnAxis(ap=BEi[:, j:j + 1], axis=0),
                                                                   element_offset=l * NEXP * D, bounds_check=regB, oob_is_err=False), reads=[bBEi], writes=[bB2])

                    def ldx(j):
                        Xs, bXs = xss[j % 2]
                        dma("sp", lambda e: e.dma_start(out=Xs[:], in_=XS[j * 128:(j + 1) * 128, :]), writes=[bXs])

                    bxTh = [[Buf(), Buf()], [Buf(), Buf()]]

                    def txh(j, hh):
                        Xs, bXs = xss[j % 2]; xT, _ = xTs[j % 2]
                        for k in range(4):
                            kk = hh * 4 + k
                            op("pe", lambda e: e.transpose(out=pT1[:, k * 128:(k + 1) * 128], in_=Xs[:, kk * 128:(kk + 1) * 128], identity=ID[:]), reads=[bXs, bID], writes=[bpT1])
                        op("act", lambda e: e.copy(out=xT[:, hh * 4:(hh + 1) * 4, :].rearrange("p a b -> p (a b)"), in_=pT1[:]), reads=[bpT1], writes=[bxTh[j % 2][hh]])

                    def swiglu(hf):
                        pU, bpU = pUs[hf]; ub, bub = ubs[hf]; glu, bglu = glus[hf]; lin, blin = lins[hf]; sig, bsig = sigs[hf]
                        op("dve", lambda e: e.tensor_tensor(out=ub[:], in0=pU[:], in1=B1[:, hf * 1024:(hf + 1) * 1024], op=ALU.add), reads=[bpUh[hf][0], bpUh[hf][1], bB1], writes=[bub])
                        ubv = ub[:].rearrange("p (f t) -> p f t", t=2)
                        op("dve", lambda e: e.tensor_scalar(out=glu[:], in0=ubv[:, :, 0], scalar1=7.0, scalar2=None, op0=ALU.min), reads=[bub], writes=[bglu])
                        op("dve", lambda e: e.tensor_scalar(out=lin[:], in0=ubv[:, :, 1], scalar1=7.0, scalar2=-7.0, op0=ALU.min, op1=ALU.max), reads=[bub], writes=[blin])
                        op("act", lambda e: e.activation(out=sig[:], in_=glu[:], func=AF.Sigmoid, scale=1.702), reads=[bglu], writes=[bsig])
                        op("dve", lambda e: e.tensor_tensor(out=glu[:], in0=glu[:], in1=sig[:], op=ALU.mult), reads=[bglu, bsig], writes=[bglu])
                        op("dve", lambda e: e.scalar_tensor_tensor(out=lin[:], in0=lin[:], scalar=1.0, in1=glu[:], op0=ALU.add, op1=ALU.mult), reads=[blin, bglu], writes=[blin])

                    def ta(hf):
                        lin, blin = lins[hf]; aT, baT = aTs[hf]
                        for k in range(4):
                            op("pe", lambda e: e.transpose(out=pT0[:, k * 128:(k + 1) * 128], in_=lin[:, k * 128:(k + 1) * 128], identity=ID[:]), reads=[blin, bID], writes=[bpT0])
                        op("act", lambda e: e.copy(out=aT[:].rearrange("p a b -> p (a b)"), in_=pT0[:]), reads=[bpT0], writes=[baT])

                    NB = len(tiles4) * 4 + NEXP
                    gW1(0); gB1(0); gW2(0); ldx(0)
                    if NB > 1:
                        ldx(1)
                    txh(0, 0); txh(0, 1)

                    def s1(j, hf, n):
                        xT, _ = xTs[j % 2]; pU, _ = pUs[hf]
                        for k in range(8):
                            op("pe", lambda e: e.matmul(pU[:, n * 512:(n + 1) * 512], lhsT=xT[:, k, :], rhs=W1[:, k, (hf * 2 + n) * 512:(hf * 2 + n + 1) * 512],
                                                        start=(k == 0), stop=(k == 7)), reads=[bxTh[j % 2][k // 4], bW1[k]], writes=[bpUh[hf][n]])

                    def s2(hf):
                        for n in range(2):
                            for k in range(4):
                                op("pe", lambda e: e.matmul(pY[:, n * 512:(n + 1) * 512], lhsT=aTs[hf][0][:, k, :], rhs=W2[:, hf * 4 + k, n * 512:(n + 1) * 512],
                                                            start=(hf == 0 and k == 0), stop=(hf == 1 and k == 3)), reads=[aTs[hf][1], bW2[hf * 4 + k]], writes=[bpY])

                    for j in range(NB):
                        Ys, bYs = ys[j % 2]
                        s1(j, 0, 0); s1(j, 0, 1)
                        swiglu(0)
                        s1(j, 1, 0)
                        ta(0)
                        s1(j, 1, 1)
                        if j + 1 < NB:
                            gW1(j + 1)
                            txh(j + 1, 0)
                        if j + 2 < NB:
                            ldx(j + 2)
                        swiglu(1)
                        if j + 1 < NB:
                            gB1(j + 1)
                        s2(0)
                        ta(1)
                        if j + 1 < NB:
                            txh(j + 1, 1)
                        s2(1)
                        op("dve", lambda e: e.tensor_tensor(out=Ys[:], in0=pY[:], in1=B2[:], op=ALU.add), reads=[bpY, bB2], writes=[bYs])
                        if j + 1 < NB:
                            gW2(j + 1)
                        dma("sp", lambda e: e.dma_start(out=YS[j * 128:(j + 1) * 128, :], in_=Ys[:]), reads=[bYs])
                P.barrier(new_epoch=False)
                if debug == "p4c":
                    dma("sp", lambda e: e.dma_start(out=DBGI[:, 0:NT * 4], in_=SL4i[:].rearrange("p a b -> p (a b)")), reads=[bSL4i])
                    P.barrier(new_epoch=False)
                    break
                with ExitStack() as es2:
                    sb2, ps2 = pools(es2)
                    G2 = [modv(es2, "G2L", 0, 5), modv(es2, "G2C", 1, 5)]
                    LNG = load(es2, "LNG2", bc(ln_g[l, 1]), [128, D]); LNB = load(es2, "LNB2", bc(ln_b[l, 1]), [128, D])
                    ygs = [[sb2(f"yg{i}_{k}", [128, D]) for k in range(4)] for i in range(2)]
                    xts = [sb2(f"x5t{i}", [128, D]) for i in range(2)]
                    f, bf = sb2("f5", [128, D]); xo = [sb2(f"xo{i}", [128, D]) for i in range(2)]
                    tmp = ln_tmp(es2, "5")
                    for ii, i in enumerate(tiles4):
                        ck = 1 if i < 2 else 0
                        YG = ygs[ii % 2]; Xt, bXt = xts[ii % 2]; Xo, bXo = xo[ii % 2]
                        for k in range(4):
                            dma("pool", lambda e: e.indirect_dma_start(out=YG[k][0][:], out_offset=None, in_=YS[:, :], in_offset=bass.IndirectOffsetOnAxis(ap=SL4i[:, i, k:k + 1], axis=0)),
                                reads=[bSL4i], writes=[YG[k][1]])
                        dma("sp", lambda e: e.dma_start(out=Xt[:], in_=X[i * 128:(i + 1) * 128, :]), writes=[bXt])
                        op("dve", lambda e: e.tensor_scalar(out=f[:], in0=YG[0][0][:], scalar1=G4[:, i, 0:1], scalar2=None, op0=ALU.mult), reads=[YG[0][1], bG4], writes=[bf])
                        for k in range(1, 4):
                            op("dve", lambda e: e.scalar_tensor_tensor(out=f[:], in0=YG[k][0][:], scalar=G4[:, i, k:k + 1], in1=f[:], op0=ALU.mult, op1=ALU.add), reads=[YG[k][1], bG4, bf], writes=[bf])
                        op("dve", lambda e: e.tensor_tensor(out=f[:], in0=f[:], in1=G2[ck][0][:], op=ALU.mult), reads=[bf, G2[ck][1]], writes=[bf])
                        op("dve", lambda e: e.scalar_tensor_tensor(out=f[:], in0=Xt[:], scalar=ALPHA, in1=f[:], op0=ALU.mult, op1=ALU.add), reads=[bXt, bf], writes=[bf])
                        layer_norm(es2, "5", f, bf, Xo, bXo, tmp)
                        mul_add(Xo, bXo, Xo, bXo, LNG[0], LNG[1], LNB[0], LNB[1])
                        if l == 1:
                            dma("sp", lambda e: e.dma_start(out=out[(i - 2) * 128:(i - 1) * 128, :], in_=Xo[:]), reads=[bXo])
                        else:
                            dma("sp", lambda e: e.dma_start(out=X[i * 128:(i + 1) * 128, :], in_=Xo[:]), reads=[bXo])
            P.barrier()
        P.finish()
    return nc


def make_in_map(inp, b, N):
    f = lambda a: np.ascontiguousarray(np.asarray(a, dtype=np.float32))
    m = {}
    m["xin"] = f(np.concatenate([inp["ctx"][b], inp["x"][b]], 0))
    cc = np.stack([np.asarray(inp["c"][b]).reshape(8, 128).T, np.asarray(inp["c_ctx"]).reshape(8, 128).T], -1)
    m["cc"] = f(cc)
    for k in ("w_mod", "b_mod", "w_in", "pool_w", "pool_scale", "attn_sink", "ret_decay", "w_out", "ln_g", "ln_b",
              "router_w", "router_b", "exp_w1", "exp_b1", "exp_w2", "exp_b2"):
        m[k] = f(inp[k])
    hc = host_consts(N)
    NT = N // 128 + 2
    NBLK = (NT * 128 * 4) // 128 + NEXP
    hc["JV"] = (np.arange(NBLK, dtype=np.float32) * 128)[None, :].repeat(128, 0)
    for k, v in hc.items():
        m["c_" + k] = f(v)
    return m


_CACHE = {}


def kernel(**inputs):
    inp = {k: np.asarray(v) for k, v in inputs.items()}
    B, N, _ = inp["x"].shape
    if N not in _CACHE:
        _CACHE[N] = build_program(N)
    nc = _CACHE[N]
    n_cores = 8
    maps = [make_in_map(inp, b, N) for b in range(B)]
    in_maps = [maps[i % B] for i in range(n_cores)]
    res = run_bass_kernel_spmd(nc, in_maps, core_ids=list(range(n_cores)))
    return np.stack([np.asarray(res.results[b]["out"], dtype=np.float32) for b in range(B)], 0)
```
